# Optimizing a Trainium2 kernel written in Bass

```python
import jax
import jax.numpy as jnp
from jax import lax
import numpy as np

D_MODEL = 1024
BATCH = 8
SEQ = 4096
DEPTH = 2

GRID_W = 64
CTX_LEN = 256
EPS = 1e-6

MLSTM_HEADS = 4
MLSTM_WIDTH = D_MODEL
MLSTM_HEAD_DIM = MLSTM_WIDTH // MLSTM_HEADS
MLSTM_CHUNK = 128
N_DIRS = 2
N_GATE_COLS = N_DIRS * 2 * MLSTM_HEADS

CONV_CHANNELS = D_MODEL // 2
DW_CONV_SIZE = 31

SG_WIDTH = D_MODEL // 2
SG_GROUPS = 4
SG_GROUP_DIM = SG_WIDTH // SG_GROUPS
SG_CHUNK = 128

N_BRANCHES = 3

N_EXPERTS = 16
EXPERT_FF = 2 * D_MODEL
CAPACITY_FACTOR = 2

OFF_Q = 0
OFF_K = MLSTM_WIDTH
OFF_V = 2 * MLSTM_WIDTH
OFF_GATES = 3 * MLSTM_WIDTH
N_STATE_COLS = OFF_GATES + N_GATE_COLS
OFF_O = N_STATE_COLS
OFF_CONV = OFF_O + MLSTM_WIDTH
OFF_SG = OFF_CONV + 2 * CONV_CHANNELS
OFF_MERGE = OFF_SG + 2 * SG_WIDTH
N_IN = OFF_MERGE + N_BRANCHES * D_MODEL

kernel_name = 'hybrid_mlstm_conformer_sgmlp_ecmoe_prefix_dit'


def rms_norm(x, w):
    xf = x.astype(jnp.float32)
    y = xf * lax.rsqrt(jnp.mean(xf * xf, axis=-1, keepdims=True) + EPS)
    return (y * w.astype(jnp.float32)).astype(x.dtype)


def layer_norm(x, w, b):
    xf = x.astype(jnp.float32)
    xc = xf - jnp.mean(xf, axis=-1, keepdims=True)
    y = xc * lax.rsqrt(jnp.mean(xc * xc, axis=-1, keepdims=True) + EPS)
    return (y * w.astype(jnp.float32) + b.astype(jnp.float32)).astype(x.dtype)


def modulate(h, shift, scale):
    return h * (1 + scale) + shift


def mlstm_scan(q, k, v, ig, lf, state):
    b_, h_, t_, dh = q.shape
    nc = t_ // MLSTM_CHUNK

    def to_chunks(a):
        a = a.reshape(a.shape[:2] + (nc, MLSTM_CHUNK) + a.shape[3:])
        return jnp.moveaxis(a, 2, 0)

    xs = (to_chunks(q), to_chunks(k), to_chunks(v), to_chunks(ig), to_chunks(lf))
    tril = jnp.tril(jnp.ones((MLSTM_CHUNK, MLSTM_CHUNK), dtype=bool))

    def step(carry, inp):
        c_prev, n_prev, m_prev = carry
        qc, kc, vc, ic, fc = inp
        bcum = jnp.cumsum(fc, axis=-1)
        log_d = bcum[..., :, None] - bcum[..., None, :] + ic[..., None, :]
        log_d = jnp.where(tril, log_d, -jnp.inf)
        m_inter = bcum + m_prev[..., None]
        m_t = jnp.maximum(m_inter, jnp.max(log_d, axis=-1))
        dmat = jnp.exp(log_d - m_t[..., None])
        inter = jnp.exp(m_inter - m_t)
        s = jnp.einsum('bhtd,bhsd->bhts', qc, kc) * dmat
        num = jnp.einsum('bhts,bhse->bhte', s, vc) + inter[..., None] * jnp.einsum('bhtd,bhde->bhte', qc, c_prev)
        den = jnp.sum(s, axis=-1) + inter * jnp.einsum('bhtd,bhd->bht', qc, n_prev)
        h = num / jnp.maximum(jnp.abs(den), jnp.exp(-m_t))[..., None]
        b_last = bcum[..., -1]
        g = b_last[..., None] - bcum + ic
        m_new = jnp.maximum(b_last + m_prev, jnp.max(g, axis=-1))
        decay = jnp.exp(b_last + m_prev - m_new)
        w = jnp.exp(g - m_new[..., None])
        c_new = decay[..., None, None] * c_prev + jnp.einsum('bhs,bhsd,bhse->bhde', w, kc, vc)
        n_new = decay[..., None] * n_prev + jnp.einsum('bhs,bhsd->bhd', w, kc)
        return (c_new, n_new, m_new), h

    state, hs = lax.scan(step, state, xs)
    h = jnp.moveaxis(hs, 0, 2).reshape(b_, h_, t_, dh)
    return h, state


def mlstm_inputs(z, gate_b):
    b_, t_, _ = z.shape
    f32 = jnp.float32

    def heads(a):
        return a.reshape(b_, t_, MLSTM_HEADS, MLSTM_HEAD_DIM).transpose(0, 2, 1, 3).astype(f32)

    q = heads(z[..., OFF_Q:OFF_K])
    k = heads(z[..., OFF_K:OFF_V]) * (MLSTM_HEAD_DIM ** -0.5)
    v = heads(z[..., OFF_V:OFF_GATES])
    gates = (z[..., OFF_GATES:N_STATE_COLS] + gate_b).astype(f32)
    gates = gates.reshape(b_, t_, N_DIRS, 2, MLSTM_HEADS).transpose(2, 3, 0, 4, 1)
    return q, k, v, gates[:, 0], jax.nn.log_sigmoid(gates[:, 1])


def mlstm_bidirectional(z_ctx, z_lat, gate_b):
    qc, kc, vc, ic, fc = mlstm_inputs(z_ctx, gate_b)
    ql, kl, vl, il, fl = mlstm_inputs(z_lat, gate_b)
    b_ = qc.shape[0]
    f32 = jnp.float32
    zero = (jnp.zeros((b_, MLSTM_HEADS, MLSTM_HEAD_DIM, MLSTM_HEAD_DIM), f32),
            jnp.zeros((b_, MLSTM_HEADS, MLSTM_HEAD_DIM), f32),
            jnp.zeros((b_, MLSTM_HEADS), f32))
    hc_f, st_f = mlstm_scan(qc, kc, vc, ic[0], fc[0], zero)
    hl_f, _ = mlstm_scan(ql, kl, vl, il[0], fl[0], st_f)
    rev = lambda a: jnp.flip(a, axis=2)
    hc_b, st_b = mlstm_scan(rev(qc), rev(kc), rev(vc), rev(ic[1]), rev(fc[1]), zero)
    hl_b, _ = mlstm_scan(rev(ql), rev(kl), rev(vl), rev(il[1]), rev(fl[1]), st_b)
    return hc_f + rev(hc_b), hl_f + rev(hl_b)


def head_rms_norm(h, w):
    y = h * lax.rsqrt(jnp.mean(h * h, axis=-1, keepdims=True) + EPS)
    b_, h_, t_, dh = h.shape
    return y.transpose(0, 2, 1, 3).reshape(b_, t_, h_ * dh) * w.astype(jnp.float32)


def conformer_conv(zc, dw_w, dw_b, ln_w, ln_b, w_out, on_grid):
    u = zc[..., :CONV_CHANNELS] * jax.nn.sigmoid(zc[..., CONV_CHANNELS:])
    b_, t_, ch = u.shape
    if on_grid:
        rows = t_ // GRID_W
        u = u.reshape(b_ * rows, GRID_W, ch)
    half = DW_CONV_SIZE // 2
    u = lax.conv_general_dilated(u, dw_w[:, None, :].astype(u.dtype), window_strides=(1,),
                                 padding=[(half, half)], dimension_numbers=('NWC', 'WIO', 'NWC'),
                                 feature_group_count=ch) + dw_b
    u = layer_norm(u.reshape(b_, t_, ch), ln_w, ln_b)
    return jax.nn.silu(u) @ w_out


def spatial_gating(zs, ln_w, ln_b, sg_w, sg_b, w_out):
    zs = jax.nn.gelu(zs)
    u, v = zs[..., :SG_WIDTH], zs[..., SG_WIDTH:]
    v = layer_norm(v, ln_w, ln_b)
    b_, t_, _ = v.shape
    nck = t_ // SG_CHUNK
    v = v.reshape(b_, nck, SG_CHUNK, SG_GROUPS, SG_GROUP_DIM)
    v = jnp.einsum('gts,bnsgc->bntgc', sg_w, v) + sg_b.T[:, :, None]
    return (u * v.reshape(b_, t_, SG_WIDTH)) @ w_out


def mixer_output(z, hm, on_grid, mlstm_norm_w, w_mlstm_out, conv_dw_w, conv_dw_b, conv_ln_w, conv_ln_b,
                 w_conv_out, sg_ln_w, sg_ln_b, sg_w, sg_b, w_sg_out, w_o):
    d = D_MODEL
    o = jax.nn.sigmoid(z[..., OFF_O:OFF_CONV])
    y_m = (head_rms_norm(hm, mlstm_norm_w).astype(z.dtype) * o) @ w_mlstm_out
    y_c = conformer_conv(z[..., OFF_CONV:OFF_SG], conv_dw_w, conv_dw_b, conv_ln_w, conv_ln_b, w_conv_out, on_grid)
    y_s = spatial_gating(z[..., OFF_SG:OFF_MERGE], sg_ln_w, sg_ln_b, sg_w, sg_b, w_sg_out)
    gm = jax.nn.sigmoid(z[..., OFF_MERGE:])
    merged = gm[..., :d] * y_m + gm[..., d:2 * d] * y_c + gm[..., 2 * d:] * y_s
    return merged @ w_o


def expert_choice_moe(h, router_w, router_b, w_gate, w_up, w_down):
    b_, t_, _ = h.shape
    cap = CAPACITY_FACTOR * t_ // N_EXPERTS
    logits = (h @ router_w + router_b).astype(jnp.float32)
    aff = jax.nn.softmax(logits, axis=-1)
    g, idx = lax.top_k(jnp.swapaxes(aff, 1, 2), cap)
    bidx = jnp.arange(b_)[:, None, None]
    xe = h[bidx, idx]
    a = jnp.einsum('becd,edf->becf', xe, w_gate)
    u = jnp.einsum('becd,edf->becf', xe, w_up)
    y = jnp.einsum('becf,efd->becd', jax.nn.silu(a) * u, w_down)
    y = y * g[..., None].astype(y.dtype)
    return jnp.zeros_like(h).at[bidx, idx].add(y)


def setup_inputs(seed: int = 0) -> dict:
    key = jax.random.key(seed)
    ks = jax.random.split(key, 32)
    f32 = jnp.float32
    L, D = DEPTH, D_MODEL

    def nrm(k, shape, scale):
        return jax.random.normal(k, shape, f32) * scale

    gate_offset = jnp.array([0.0, 3.0], f32)[None, None, :, None]
    return {
        'x': nrm(ks[0], (BATCH, SEQ, D), 1.0),
        'c': nrm(ks[1], (BATCH, D), 1.0),
        'ctx': nrm(ks[2], (BATCH, CTX_LEN, D), 1.0),
        'c_ctx': nrm(ks[3], (D,), 1.0),
        'ada_w': nrm(ks[4], (L, D, 6 * D), 0.5 * D ** -0.5),
        'ada_b': nrm(ks[5], (L, 6 * D), 0.02),
        'norm1_w': 1.0 + nrm(ks[6], (L, D), 0.05),
        'norm2_w': 1.0 + nrm(ks[7], (L, D), 0.05),
        'w_in': nrm(ks[8], (L, D, N_IN), D ** -0.5),
        'mlstm_gate_b': (nrm(ks[9], (L, N_DIRS, 2, MLSTM_HEADS), 0.1) + gate_offset).reshape(L, N_GATE_COLS),
        'mlstm_norm_w': 1.0 + nrm(ks[10], (L, MLSTM_WIDTH), 0.05),
        'w_mlstm_out': nrm(ks[11], (L, MLSTM_WIDTH, D), MLSTM_WIDTH ** -0.5),
        'conv_dw_w': nrm(ks[12], (L, DW_CONV_SIZE, CONV_CHANNELS), DW_CONV_SIZE ** -0.5),
        'conv_dw_b': nrm(ks[13], (L, CONV_CHANNELS), 0.02),
        'conv_ln_w': 1.0 + nrm(ks[14], (L, CONV_CHANNELS), 0.05),
        'conv_ln_b': nrm(ks[15], (L, CONV_CHANNELS), 0.02),
        'w_conv_out': nrm(ks[16], (L, CONV_CHANNELS, D), CONV_CHANNELS ** -0.5),
        'sg_ln_w': 1.0 + nrm(ks[17], (L, SG_WIDTH), 0.05),
        'sg_ln_b': nrm(ks[18], (L, SG_WIDTH), 0.02),
        'sg_w': nrm(ks[19], (L, SG_GROUPS, SG_CHUNK, SG_CHUNK), SG_CHUNK ** -0.5),
        'sg_b': 1.0 + nrm(ks[20], (L, SG_GROUPS, SG_CHUNK), 0.02),
        'w_sg_out': nrm(ks[21], (L, SG_WIDTH, D), SG_WIDTH ** -0.5),
        'w_o': nrm(ks[22], (L, D, D), D ** -0.5),
        'router_w': nrm(ks[23], (L, D, N_EXPERTS), D ** -0.5),
        'router_b': nrm(ks[24], (L, N_EXPERTS), 0.01),
        'expert_w_gate': nrm(ks[25], (L, N_EXPERTS, D, EXPERT_FF), D ** -0.5),
        'expert_w_up': nrm(ks[26], (L, N_EXPERTS, D, EXPERT_FF), D ** -0.5),
        'expert_w_down': nrm(ks[27], (L, N_EXPERTS, EXPERT_FF, D), EXPERT_FF ** -0.5),
        'final_norm_w': 1.0 + nrm(ks[28], (D,), 0.05),
    }


def reference(x, c, ctx, c_ctx, ada_w, ada_b, norm1_w, norm2_w, w_in, mlstm_gate_b, mlstm_norm_w,
              w_mlstm_out, conv_dw_w, conv_dw_b, conv_ln_w, conv_ln_b, w_conv_out, sg_ln_w, sg_ln_b,
              sg_w, sg_b, w_sg_out, w_o, router_w, router_b, expert_w_gate, expert_w_up, expert_w_down,
              final_norm_w):
    for layer in range(DEPTH):
        need_ctx = layer < DEPTH - 1
        mod_lat = jax.nn.silu(c) @ ada_w[layer] + ada_b[layer]
        mod_ctx = jax.nn.silu(c_ctx) @ ada_w[layer] + ada_b[layer]
        sh1, sc1, g1, sh2, sc2, g2 = jnp.split(mod_lat[:, None, :], 6, axis=-1)
        csh1, csc1, cg1, csh2, csc2, cg2 = jnp.split(mod_ctx, 6, axis=-1)

        h_lat = modulate(rms_norm(x, norm1_w[layer]), sh1, sc1)
        h_ctx = modulate(rms_norm(ctx, norm1_w[layer]), csh1, csc1)
        z_lat = h_lat @ w_in[layer]
        z_ctx = h_ctx @ (w_in[layer] if need_ctx else w_in[layer][:, :N_STATE_COLS])
        hm_ctx, hm_lat = mlstm_bidirectional(z_ctx[..., :N_STATE_COLS], z_lat[..., :N_STATE_COLS],
                                             mlstm_gate_b[layer])
        branch_w = (mlstm_norm_w[layer], w_mlstm_out[layer], conv_dw_w[layer], conv_dw_b[layer],
                    conv_ln_w[layer], conv_ln_b[layer], w_conv_out[layer], sg_ln_w[layer], sg_ln_b[layer],
                    sg_w[layer], sg_b[layer], w_sg_out[layer], w_o[layer])
        y_lat = mixer_output(z_lat, hm_lat, True, *branch_w)
        x = x + g1 * y_lat
        if need_ctx:
            y_ctx = mixer_output(z_ctx, hm_ctx, False, *branch_w)
            ctx = ctx + cg1 * y_ctx

        moe_w = (router_w[layer], router_b[layer], expert_w_gate[layer], expert_w_up[layer], expert_w_down[layer])
        x = x + g2 * expert_choice_moe(modulate(rms_norm(x, norm2_w[layer]), sh2, sc2), *moe_w)
        if need_ctx:
            ctx = ctx + cg2 * expert_choice_moe(modulate(rms_norm(ctx, norm2_w[layer]), csh2, csc2), *moe_w)

    return rms_norm(x, final_norm_w)
```

```python
import numpy as np
from contextlib import ExitStack
import concourse.bass as bass
import concourse.mybir as mybir
from concourse.bass_utils import run_bass_kernel_spmd

F32 = mybir.dt.float32
BF16 = mybir.dt.bfloat16
I32 = mybir.dt.int32
AF = mybir.ActivationFunctionType
ALU = mybir.AluOpType

D = 1024
SEQ = 4096
CTX = 256
DEPTH = 2
NT = 34
NROW = NT * 128
NE = 16
FF = 2048
N_IN = 9232
NST = 3088
EPS = 1e-6
ZR_OG, ZR_U, ZR_ZS, ZR_GM, ZR_W = 0, 1024, 1536, 2560, 5632


class Buf:
    __slots__ = ("t", "name", "writes", "readers", "dsem", "dcnt")

    def __init__(self, t, name):
        self.t = t
        self.name = name
        self.writes = {}
        self.readers = {}
        self.dsem = None
        self.dcnt = 0

    def __getitem__(self, key):
        return self.t[key]


def _merge(d, s):
    for k, v in s.items():
        if d.get(k, 0) < v:
            d[k] = v


class KB:
    def __init__(self, nc):
        self.nc = nc
        self.es = ExitStack()
        self.eng = {"pe": nc.tensor, "dve": nc.vector, "act": nc.scalar, "pool": nc.gpsimd, "sp": nc.sync}
        self.semh = {}
        self.cnt = {}
        self.seen = {k: {} for k in self.eng}
        for k in self.eng:
            self.semh[k] = self.es.enter_context(nc.semaphore("s_" + k))
            self.cnt[k] = 0
        self.dfree = []
        self.dcount = []
        self.nop = 0

    def sbuf(self, name, shape, dtype, es=None):
        self.uid = getattr(self, "uid", 0) + 1
        name = "%s_u%d" % (name, self.uid)
        t = (es or self.es).enter_context(self.nc.sbuf_tensor(name, list(shape), dtype))
        b = Buf(t, name)
        if es is not None:
            es.callback(self._release, b)
        return b

    def _release(self, b):
        if b.dsem is not None:
            self.dfree.append(b.dsem)
            b.dsem = None

    def _dsem_for(self, b):
        if b.dsem is None:
            if self.dfree:
                b.dsem = self.dfree.pop()
            else:
                idx = len(self.dcount)
                h = self.es.enter_context(self.nc.semaphore("dq%d" % idx))
                self.dcount.append(0)
                self.semh[("d", idx)] = h
                b.dsem = idx
        return b.dsem

    def psum(self, name, shape, dtype, es=None):
        t = (es or self.es).enter_context(self.nc.psum_tensor(name, list(shape), dtype))
        return Buf(t, name)

    def dram(self, name, shape, dtype, kind="Internal"):
        t = self.nc.dram_tensor(name, list(shape), dtype, kind=kind)
        return Buf(t.ap(), name)

    def _wait(self, e, need):
        eng = self.eng[e]
        seen = self.seen[e]
        for key, v in need.items():
            if seen.get(key, 0) < v:
                eng.wait_ge(self.semh[key], v)
                seen[key] = v

    def op(self, e, fn, reads=(), writes=()):
        raw = {}
        oth = {}
        for b in reads:
            _merge(raw, b.writes)
        for b in writes:
            _merge(oth, b.readers)
            _merge(oth, b.writes)
        need = dict(raw)
        for key, v in oth.items():
            if key == e:
                continue
            if need.get(key, 0) < v:
                need[key] = v
        if e == "pe":
            need.pop("pe", None)
        self._wait(e, need)
        ins = fn(self.eng[e])
        self.cnt[e] += 1
        ins.then_inc(self.semh[e], 1)
        tok = {e: self.cnt[e]}
        for b in reads:
            _merge(b.readers, tok)
        for b in writes:
            b.writes = dict(tok)
            b.readers = {}
        self.nop += 1
        return ins

    def dma(self, q, fn, reads=(), writes=(), sembuf=None):
        need = {}
        for b in reads:
            _merge(need, b.writes)
        for b in writes:
            _merge(need, b.readers)
            _merge(need, b.writes)
        self._wait(q, need)
        idx = self._dsem_for(sembuf)
        key = ("d", idx)
        ins = fn(self.eng[q])
        self.dcount[idx] += 16
        ins.then_inc(self.semh[key], 16)
        tok = {key: self.dcount[idx]}
        for b in reads:
            _merge(b.readers, tok)
        for b in writes:
            b.writes = dict(tok)
            b.readers = {}
        self.nop += 1
        return ins

    def barrier(self):
        need = {k: c for k, c in self.cnt.items() if c > 0}
        for idx, c in enumerate(self.dcount):
            if c > 0:
                need[("d", idx)] = c
        for e in self.eng:
            self._wait(e, dict(need))

    def finish(self):
        self.barrier()
        self.es.close()


class Ring:
    def __init__(self, bufs):
        self.bufs = bufs
        self.i = 0

    def next(self):
        b = self.bufs[self.i % len(self.bufs)]
        self.i += 1
        return b


def interleave(gens, skew=None):
    gens = list(gens)
    delay = {id(g): (skew[i] if skew else 0) for i, g in enumerate(gens)}
    while gens:
        for g in list(gens):
            if delay[id(g)] > 0:
                delay[id(g)] -= 1
                continue
            try:
                next(g)
            except StopIteration:
                gens.remove(g)


def build_program(layers=DEPTH, upto="all", dbg=False):
    nc = bass.Bass("TRN2", target_bir_lowering=False)
    k = KB(nc)
    ins_ = {}

    def din(name, shape):
        ins_[name] = k.dram(name, shape, F32, kind="ExternalInput")
        return ins_[name]

    x_d = din("x", [SEQ, D]); c_d = din("c", [D]); ctx_d = din("ctx", [CTX, D]); cctx_d = din("c_ctx", [D])
    ada_w_d = din("ada_w", [DEPTH, D, 6 * D]); ada_b_d = din("ada_b", [DEPTH, 6 * D])
    n1_d = din("norm1_w", [DEPTH, D]); n2_d = din("norm2_w", [DEPTH, D])
    w_in_d = din("w_in", [DEPTH, D, N_IN]); gb_d = din("mlstm_gate_b", [DEPTH, 16])
    mnw_d = din("mlstm_norm_w", [DEPTH, D]); wmo_d = din("w_mlstm_out", [DEPTH, D, D])
    cdw_d = din("conv_dw_w", [DEPTH, 31, 512]); cdb_d = din("conv_dw_b", [DEPTH, 512])
    clw_d = din("conv_ln_w", [DEPTH, 512]); clb_d = din("conv_ln_b", [DEPTH, 512])
    wco_d = din("w_conv_out", [DEPTH, 512, D])
    slw_d = din("sg_ln_w", [DEPTH, 512]); slb_d = din("sg_ln_b", [DEPTH, 512])
    sgw_d = din("sg_w", [DEPTH, 4, 128, 128]); sgb_d = din("sg_b", [DEPTH, 4, 128])
    wso_d = din("w_sg_out", [DEPTH, 512, D]); wo_d = din("w_o", [DEPTH, D, D])
    rw_d = din("router_w", [DEPTH, D, NE]); rb_d = din("router_b", [DEPTH, NE])
    wg_d = din("expert_w_gate", [DEPTH, NE, D, FF]); wu_d = din("expert_w_up", [DEPTH, NE, D, FF])
    wd_d = din("expert_w_down", [DEPTH, NE, FF, D]); fnw_d = din("final_norm_w", [D])
    out_d = k.dram("out", [SEQ, D], F32, kind="ExternalOutput")

    sk = "ExternalOutput" if dbg else "Internal"
    XA = k.dram("XA", [NROW, D], F32, kind=sk)
    XB = k.dram("XB", [NROW, D], F32, kind=sk)
    ZQ = k.dram("ZQ", [NT, 128, 3072], BF16, kind=sk)
    GGd = k.dram("GGd", [NT, 128, 32], F32, kind=sk)
    ZR = k.dram("ZR", [NT, 128, ZR_W], BF16, kind=sk)
    HD = [k.dram("HF", [NT, 128, D], F32, kind=sk), k.dram("HB", [NT, 128, D], F32, kind=sk)]
    H2d = k.dram("H2d", [NROW, D], BF16, kind=sk)
    MACC = k.dram("MACC", [NROW, D], F32, kind=sk)
    MODd = k.dram("MODd", [2, 6 * D], F32, kind=sk)
    WSd = k.dram("WSd", [2, 2, D], F32, kind=sk)
    dbg_d = {}

    def V(fn, r=(), w=()):
        return k.op("dve", fn, r, w)

    def A(fn, r=(), w=()):
        return k.op("act", fn, r, w)

    def P(fn, r=(), w=()):
        return k.op("pe", fn, r, w)

    def G(fn, r=(), w=()):
        return k.op("pool", fn, r, w)

    def LD(out_ap, in_ap, buf, q="sp"):
        return k.dma(q, lambda e: e.dma_start(out=out_ap, in_=in_ap), writes=[buf], sembuf=buf)

    def ST(out_ap, in_ap, buf, q="pool"):
        return k.dma(q, lambda e: e.dma_start(out=out_ap, in_=in_ap), reads=[buf], sembuf=buf)

    PB = [k.psum("pb%d" % i, [128, 512], F32) for i in range(8)]

    def pbf(i):
        return PB[i][:].bitcast(BF16)

    ONES32 = k.sbuf("ones32", [128, 128], F32)
    ID32 = k.sbuf("id32", [128, 128], F32)
    IDB = k.sbuf("idb", [128, 128], BF16)
    ONESB = k.sbuf("onesb", [128, 128], BF16)
    U32 = k.sbuf("u32", [128, 128], F32)
    L32 = k.sbuf("l32", [128, 128], F32)
    SUB = k.sbuf("sub", [128, 128], BF16)
    MASK4 = k.sbuf("mask4", [128, 2, 4, 128], BF16)
    MEAN32 = k.sbuf("mean32", [128, 128], F32)
    EPSC = k.sbuf("epsc", [128, 1], F32)
    MHALF = k.sbuf("mhalf", [128, 128], F32)
    XINIT = k.sbuf("xinit", [128, 4], F32)
    PIDX = k.sbuf("pidx", [128, 1], F32)
    PIDXI = k.sbuf("pidxi", [128, 1], I32)
    TMPC = k.sbuf("tmpc", [128, 128], F32)

    G(lambda e: e.memset(ONES32[:], 1.0), w=[ONES32])
    G(lambda e: e.memset(EPSC[:], EPS), w=[EPSC])
    G(lambda e: e.memset(MHALF[:], -0.5), w=[MHALF])
    G(lambda e: e.memset(MEAN32[:], 1.0 / 512.0), w=[MEAN32])
    G(lambda e: e.affine_select(out=ID32[:], in_=ONES32[:], pattern=[[-1, 128]], compare_op=ALU.is_equal, fill=0.0, base=0, channel_multiplier=1), r=[ONES32], w=[ID32])
    G(lambda e: e.affine_select(out=U32[:], in_=ONES32[:], pattern=[[1, 128]], compare_op=ALU.is_ge, fill=0.0, base=0, channel_multiplier=-1), r=[ONES32], w=[U32])
    G(lambda e: e.affine_select(out=L32[:], in_=ONES32[:], pattern=[[-1, 128]], compare_op=ALU.is_ge, fill=0.0, base=0, channel_multiplier=1), r=[ONES32], w=[L32])
    G(lambda e: e.affine_select(out=TMPC[:], in_=ONES32[:], pattern=[[1, 128]], compare_op=ALU.is_gt, fill=0.0, base=0, channel_multiplier=-1), r=[ONES32], w=[TMPC])
    V(lambda e: e.tensor_copy(out=SUB[:], in_=TMPC[:]), r=[TMPC], w=[SUB])
    V(lambda e: e.tensor_copy(out=IDB[:], in_=ID32[:]), r=[ID32], w=[IDB])
    V(lambda e: e.tensor_copy(out=ONESB[:], in_=ONES32[:]), r=[ONES32], w=[ONESB])
    for h in range(4):
        V(lambda e: e.tensor_copy(out=MASK4[:, 0, h, :], in_=U32[:]), r=[U32], w=[MASK4])
        V(lambda e: e.tensor_copy(out=MASK4[:, 1, h, :], in_=L32[:]), r=[L32], w=[MASK4])
    G(lambda e: e.iota(PIDXI[:], pattern=[[0, 1]], base=0, channel_multiplier=1), w=[PIDXI])
    V(lambda e: e.tensor_copy(out=PIDX[:], in_=PIDXI[:]), r=[PIDXI], w=[PIDX])

    k.dma("sp", lambda e: e.dma_start(out=XA[0:CTX, :], in_=ctx_d[:, :]), sembuf=XINIT)
    for i in range(4):
        k.dma("sp", lambda e: e.dma_start(out=XA[CTX + i * 1024:CTX + (i + 1) * 1024, :], in_=x_d[i * 1024:(i + 1) * 1024, :]), sembuf=XINIT)

    CROW = k.sbuf("crow", [16, 128], F32)
    CSB = k.sbuf("csb", [128, 8, 2], BF16)
    LD(CROW[0:8, :], c_d.t.rearrange("(r p) -> r p", p=128), CROW)
    LD(CROW[8:16, :], cctx_d.t.rearrange("(r p) -> r p", p=128), CROW)
    P(lambda e: e.transpose(out=PB[0][:, 0:16], in_=CROW[:, :], identity=ID32[0:16, 0:16]), r=[CROW, ID32], w=[PB[0]])
    for j in range(2):
        A(lambda e: e.activation(out=CSB[:, :, j], in_=PB[0][:, j * 8:(j + 1) * 8], func=AF.Silu), r=[PB[0]], w=[CSB])

    FT = k.sbuf("ft", [128, 4, 8, 2], F32)
    GBR = k.sbuf("gbr", [128, 16], F32)

    def bcast_row(dst, src_ap):
        LD(dst[:], src_ap.partition_broadcast(128), dst)

    xs_cur, xs_oth = XA, XB

    for l in range(layers):
        need_ctx = l < DEPTH - 1
        last = l == DEPTH - 1
        with ExitStack() as es:
            es.enter_context(nc.named_scope('A%d' % l))
            AWR = Ring([k.sbuf("aw%d" % i, [128, 8, 512], BF16, es) for i in range(2)])
            MODROW = k.sbuf("modrow", [2, 6 * D], F32, es)
            WSROW = k.sbuf("wsrow", [2, 2, D], F32, es)
            NROWS = k.sbuf("nrows", [2, 2, D], F32, es)
            for g in range(12):
                aw = AWR.next()
                k.dma("pool", lambda e: e.dma_start(out=aw[:], in_=ada_w_d[l, :, g * 512:(g + 1) * 512].rearrange("(kc p) n -> p kc n", p=128)), writes=[aw], sembuf=aw)
                for kc in range(8):
                    P(lambda e: e.matmul(PB[0][0:2, 0:512], lhsT=CSB[:, kc, :], rhs=aw[:, kc, :], start=(kc == 0), stop=(kc == 7)), r=[CSB, aw], w=[PB[0]])
                V(lambda e: e.tensor_copy(out=MODROW[0:2, g * 512:(g + 1) * 512], in_=PB[0][0:2, 0:512]), r=[PB[0]], w=[MODROW])
            BROW = k.sbuf("brow", [2, 6 * D], F32, es)
            LD(BROW[:], ada_b_d[l, :].partition_broadcast(2), BROW)
            V(lambda e: e.tensor_tensor(out=MODROW[:], in0=MODROW[:], in1=BROW[:], op=ALU.add), r=[MODROW, BROW], w=[MODROW])
            LD(NROWS[:, 0, :], n1_d[l, :].partition_broadcast(2), NROWS)
            LD(NROWS[:, 1, :], n2_d[l, :].partition_broadcast(2), NROWS)
            for w_, sc_set in ((0, 1), (1, 4)):
                V(lambda e: e.tensor_scalar(out=WSROW[:, w_, :], in0=MODROW[:, sc_set * D:(sc_set + 1) * D], scalar1=1.0, scalar2=None, op0=ALU.add), r=[MODROW], w=[WSROW])
                V(lambda e: e.tensor_tensor(out=WSROW[:, w_, :], in0=WSROW[:, w_, :], in1=NROWS[:, w_, :], op=ALU.mult), r=[WSROW, NROWS], w=[WSROW])
            srcs = [(WSROW, lambda c: WSROW[0:2, 0, c * 128:(c + 1) * 128]), (MODROW, lambda c: MODROW[0:2, 0 * D + c * 128:0 * D + (c + 1) * 128]),
                    (WSROW, lambda c: WSROW[0:2, 1, c * 128:(c + 1) * 128]), (MODROW, lambda c: MODROW[0:2, 3 * D + c * 128:3 * D + (c + 1) * 128])]
            for s, (sb, fn) in enumerate(srcs):
                for c in range(8):
                    P(lambda e: e.transpose(out=PB[1][:, (s * 8 + c) * 2:(s * 8 + c) * 2 + 2], in_=fn(c), identity=ID32[0:2, 0:2]), r=[sb, ID32], w=[PB[1]])
            V(lambda e: e.tensor_copy(out=FT[:].rearrange("p s c j -> p (s c j)"), in_=PB[1][:, 0:64]), r=[PB[1]], w=[FT])
            LD(GBR[:], gb_d[l, :].partition_broadcast(128), GBR)
            ST(MODd[:, :], MODROW[:], MODROW)
            ST(WSd[:, :, :], WSROW[:], WSROW)
        k.barrier()

        tiles_all = list(range(NT))
        tiles_out = list(range(NT)) if need_ctx else list(range(2, NT))

        def rsqrt_pool(out_ap, in_ap, scale, w, rbufs, wbuf):
            G(lambda e: e.tensor_scalar(out=out_ap, in0=in_ap, scalar1=scale, scalar2=EPS, op0=ALU.mult, op1=ALU.add), r=rbufs, w=[wbuf])
            G(lambda e: e.tensor_tensor(out=out_ap, in0=out_ap, in1=MHALF[0:out_ap.shape[0], 0:w], op=ALU.pow), r=[wbuf, MHALF], w=[wbuf])

        def rms_to_xn(xt, xn, junk, ssq, rt):
            A(lambda e: e.activation(out=junk[:], in_=xt[:], func=AF.Square, accum_out=ssq[:]), r=[xt], w=[junk, ssq])
            rsqrt_pool(rt[:], ssq[:], 1.0 / D, 1, [ssq], rt)
            V(lambda e: e.tensor_scalar(out=xn[:], in0=xt[:], scalar1=rt[:, 0:1], scalar2=None, op0=ALU.mult), r=[xt, rt], w=[xn])

        def xn_to_hT(xn, hT, j, s_ws, s_sh, pbs=None):
            if pbs is None:
                pbs = (PB[0], PB[1])
            for hh in range(2):
                pb = pbs[hh]
                for q in range(4):
                    c = hh * 4 + q
                    P(lambda e: e.transpose(out=pb[:, q * 128:(q + 1) * 128], in_=xn[:, c * 128:(c + 1) * 128], identity=ID32[:]), r=[xn, ID32], w=[pb])
                for q in range(4):
                    c = hh * 4 + q
                    A(lambda e: e.activation(out=hT[:, c, :], in_=pb[:, q * 128:(q + 1) * 128], func=AF.Identity, bias=FT[:, s_sh, c, j:j + 1], scale=FT[:, s_ws, c, j:j + 1]), r=[pb, FT], w=[hT])

        for part in range(2):
          with ExitStack() as es:
            es.enter_context(nc.named_scope('B%d_%d' % (l, part)))
            wc0, wc1 = (0, NST) if part == 0 else (NST, N_IN)
            WIN = k.sbuf("win%d" % part, [128, 8, wc1 - wc0], BF16, es)
            cc = wc0
            while cc < wc1:
                ce = min(cc + 1024, wc1)
                k.dma("pool", lambda e: e.dma_start(out=WIN[:, :, cc - wc0:ce - wc0], in_=w_in_d[l, :, cc:ce].rearrange("(kc p) n -> p kc n", p=128)), writes=[WIN], sembuf=WIN)
                cc = ce
            junk = k.sbuf("bjunk", [128, D], BF16, es)
            btiles = tiles_all if part == 0 else tiles_out
            BW = []
            for s_ in range(2):
                W = {}
                W["pre"] = []
                for pp in range(2):
                    W["pre"].append({"xt": k.sbuf("bxt%d%d" % (s_, pp), [128, D], F32, es), "xn": k.sbuf("bxn%d%d" % (s_, pp), [128, D], F32, es),
                                     "ssq": k.sbuf("bssq%d%d" % (s_, pp), [128, 1], F32, es), "rt": k.sbuf("brt%d%d" % (s_, pp), [128, 1], F32, es),
                                     "hT": k.sbuf("bht%d%d" % (s_, pp), [128, 8, 128], BF16, es)})
                if part == 0:
                    W["qkv"] = k.sbuf("bqkv%d" % s_, [128, 3072], BF16, es)
                    W["GT"] = k.sbuf("bgt%d" % s_, [128, 16], F32, es)
                    W["SP"] = k.sbuf("bsp%d" % s_, [128, 8], F32, es)
                    W["TM"] = k.sbuf("btm%d" % s_, [128, 16], F32, es)
                    W["gg"] = k.sbuf("bgg%d" % s_, [128, 32], F32, es)
                else:
                    W["zr"] = k.sbuf("bzr%d" % s_, [128, ZR_W], BF16, es)
                    W["SG"] = k.sbuf("bsg%d" % s_, [128, 512], F32, es)
                    W["SIG"] = k.sbuf("bsig%d" % s_, [128, 512], F32, es)
                W["PR"] = Ring([PB[2 + 2 * s_], PB[3 + 2 * s_]] + ([PB[6 + s_]] if part == 1 else []))
                W["pt"] = PB[s_]
                W["pg"] = PB[6 + s_]
                BW.append(W)

            def b_prep1(ti, W, pp):
                pr = W["pre"][pp]
                LD(pr["xt"][:], xs_cur[ti * 128:(ti + 1) * 128, :], pr["xt"])
                rms_to_xn(pr["xt"], pr["xn"], junk, pr["ssq"], pr["rt"])

            def b_prep2(ti, W, pp):
                pr = W["pre"][pp]
                xn_to_hT(pr["xn"], pr["hT"], 1 if ti < 2 else 0, 0, 1, pbs=(W["pt"], W["pt"]))

            def b_tile(ti, W, pp, nxt_ti):
                hT, PR, pg = W["pre"][pp]["hT"], W["PR"], W["pg"]
                gi = 0
                if part == 0:
                    qkv, GT, SP_, TM, gg = W["qkv"], W["GT"], W["SP"], W["TM"], W["gg"]
                else:
                    zr, SG_, SIG = W["zr"], W["SG"], W["SIG"]
                for g in (range(7) if part == 0 else range(7, 19)):
                    c0 = g * 512
                    c1 = c0 + 512
                    if g == 6:
                        c1 = NST
                    if g >= 7:
                        c0 = NST + (g - 7) * 512
                        c1 = c0 + 512
                    w = c1 - c0
                    ps = PR.next()
                    for kc in range(8):
                        P(lambda e: e.matmul(ps[:, 0:w], lhsT=hT[:, kc, :], rhs=WIN[:, kc, c0 - wc0:c1 - wc0], start=(kc == 0), stop=(kc == 7)), r=[hT, WIN], w=[ps])
                    if g < 6:
                        sc = 0.0625 if g in (2, 3) else 1.0
                        if g % 2 == 0:
                            A(lambda e: e.activation(out=qkv[:, c0:c1], in_=ps[:, 0:512], func=AF.Copy, scale=sc), r=[ps], w=[qkv])
                        else:
                            V(lambda e: e.tensor_scalar(out=qkv[:, c0:c1], in0=ps[:, 0:512], scalar1=sc, scalar2=None, op0=ALU.mult), r=[ps], w=[qkv])
                    elif g == 6:
                        V(lambda e: e.tensor_tensor(out=GT[:], in0=ps[:, 0:16], in1=GBR[:], op=ALU.add), r=[ps, GBR], w=[GT])
                    else:
                        r0 = (g - 7) * 512
                        if r0 < 1024:
                            A(lambda e: e.activation(out=zr[:, ZR_OG + r0:ZR_OG + r0 + 512], in_=ps[:, 0:512], func=AF.Sigmoid), r=[ps], w=[zr])
                        elif r0 == 1024:
                            V(lambda e: e.tensor_copy(out=SG_[:], in_=ps[:, 0:512]), r=[ps], w=[SG_])
                        elif r0 == 1536:
                            A(lambda e: e.activation(out=SIG[:], in_=ps[:, 0:512], func=AF.Sigmoid), r=[ps], w=[SIG])
                            V(lambda e: e.tensor_tensor(out=zr[:, ZR_U:ZR_U + 512], in0=SG_[:], in1=SIG[:], op=ALU.mult), r=[SG_, SIG], w=[zr])
                        elif r0 < 3072:
                            o0 = ZR_ZS + (r0 - 2048)
                            V(lambda e: e.tensor_tensor(out=SG_[:], in0=ps[:, 0:512], in1=ps[:, 0:512], op=ALU.mult), r=[ps], w=[SG_]) if False else None
                            A(lambda e: e.activation(out=SG_[:], in_=ps[:, 0:512], func=AF.Square), r=[ps], w=[SG_])
                            V(lambda e: e.tensor_scalar(out=SG_[:], in0=SG_[:], scalar1=0.044715, scalar2=1.0, op0=ALU.mult, op1=ALU.add), r=[SG_], w=[SG_])
                            V(lambda e: e.tensor_tensor(out=SG_[:], in0=SG_[:], in1=ps[:, 0:512], op=ALU.mult), r=[SG_, ps], w=[SG_])
                            A(lambda e: e.activation(out=SIG[:], in_=SG_[:], func=AF.Sigmoid, scale=1.5957691216057308), r=[SG_], w=[SIG])
                            V(lambda e: e.tensor_tensor(out=zr[:, o0:o0 + 512], in0=SIG[:], in1=ps[:, 0:512], op=ALU.mult), r=[SIG, ps], w=[zr])
                        else:
                            o0 = ZR_GM + (r0 - 3072)
                            A(lambda e: e.activation(out=zr[:, o0:o0 + 512], in_=ps[:, 0:512], func=AF.Sigmoid), r=[ps], w=[zr])
                    gi += 1
                    if nxt_ti is not None and gi == 1:
                        b_prep1(nxt_ti, W, 1 - pp)
                    if nxt_ti is not None and gi == (4 if part == 0 else 7):
                        b_prep2(nxt_ti, W, 1 - pp)
                    yield
                if part == 1:
                    ST(ZR[ti, :, :], zr[:], zr)
                    yield
                    return
                for dd in range(2):
                    A(lambda e: e.activation(out=SP_[:, dd * 4:(dd + 1) * 4], in_=GT[:, dd * 8 + 4:dd * 8 + 8], func=AF.Exp, scale=-1.0), r=[GT], w=[SP_])
                yield
                A(lambda e: e.activation(out=SP_[:], in_=SP_[:], func=AF.Ln, bias=1.0, scale=1.0), r=[SP_], w=[SP_])
                yield
                P(lambda e: e.matmul(pg[:, 0:4], lhsT=U32[:], rhs=SP_[:, 0:4], start=True, stop=True), r=[U32, SP_], w=[pg])
                P(lambda e: e.matmul(pg[:, 4:8], lhsT=L32[:], rhs=SP_[:, 4:8], start=True, stop=True), r=[L32, SP_], w=[pg])
                P(lambda e: e.matmul(pg[:, 8:16], lhsT=ONES32[:], rhs=SP_[:, 0:8], start=True, stop=True), r=[ONES32, SP_], w=[pg])
                yield
                A(lambda e: e.activation(out=gg[:, 0:8], in_=pg[:, 0:8], func=AF.Exp, scale=-1.0), r=[pg], w=[gg])
                for dd in range(2):
                    V(lambda e: e.tensor_tensor(out=TM[:, dd * 4:(dd + 1) * 4], in0=GT[:, dd * 8:dd * 8 + 4], in1=pg[:, dd * 4:(dd + 1) * 4], op=ALU.add), r=[GT, pg], w=[TM])
                yield
                A(lambda e: e.activation(out=gg[:, 8:16], in_=TM[:, 0:8], func=AF.Exp), r=[TM], w=[gg])
                V(lambda e: e.tensor_tensor(out=TM[:, 8:16], in0=TM[:, 0:8], in1=pg[:, 8:16], op=ALU.subtract), r=[TM, pg], w=[TM])
                yield
                A(lambda e: e.activation(out=gg[:, 16:24], in_=TM[:, 8:16], func=AF.Exp), r=[TM], w=[gg])
                A(lambda e: e.activation(out=gg[:, 24:32], in_=pg[:, 8:16], func=AF.Exp, scale=-1.0), r=[pg], w=[gg])
                yield
                ST(ZQ[ti, :, :], qkv[:], qkv)
                ST(GGd[ti, :, :], gg[:], gg)
                yield

            def b_stream(s_):
                mine = btiles[s_::2]
                b_prep1(mine[0], BW[s_], 0)
                b_prep2(mine[0], BW[s_], 0)
                yield
                for n_, ti in enumerate(mine):
                    yield from b_tile(ti, BW[s_], n_ % 2, mine[n_ + 1] if n_ + 1 < len(mine) else None)

            interleave([b_stream(0), b_stream(1)], skew=[0, 9 if part == 0 else 8])
          k.barrier()
        if upto == "B" and l == layers - 1:
            break

        with ExitStack() as es:
            es.enter_context(nc.named_scope('C%d' % l))
            Cst = []
            for dd in range(2):
                c32 = k.sbuf("c32_%d" % dd, [128, 2, 4, 257], F32, es)
                cbf = k.sbuf("cbf_%d" % dd, [128, 2, 4, 257], BF16, es)
                G(lambda e: e.memset(c32[:], 0.0), w=[c32])
                G(lambda e: e.memset(cbf[:], 0.0), w=[cbf])
                Cst.append((c32, cbf))
            order = [tiles_all, [1, 0] + list(range(NT - 1, 1, -1))]
            SW = []
            for dd in range(2):
                W = {"q": k.sbuf("sq%d" % dd, [128, D], BF16, es), "kk": k.sbuf("sk%d" % dd, [128, D], BF16, es),
                     "qs": k.sbuf("sqs%d" % dd, [128, D], BF16, es), "ks": k.sbuf("sks%d" % dd, [128, D], BF16, es),
                     "kst": k.sbuf("skst%d" % dd, [128, 8, 128], BF16, es), "dn": k.sbuf("sdn%d" % dd, [128, 8], F32, es),
                     "ho": [k.sbuf("sho%d_%d" % (dd, i), [128, D], F32, es) for i in range(2)], "par": []}
                for pp in range(2):
                    va = k.sbuf("sv%d_%d" % (dd, pp), [128, 4, 257], BF16, es)
                    G(lambda e: e.memset(va[:], 1.0), w=[va])
                    W["par"].append({"va": va, "gg": k.sbuf("sg%d_%d" % (dd, pp), [128, 32], F32, es),
                                     "kss": k.sbuf("skss%d_%d" % (dd, pp), [128, D], BF16, es),
                                     "qst": k.sbuf("sqst%d_%d" % (dd, pp), [128, 8, 128], BF16, es),
                                     "stm": k.sbuf("sst%d_%d" % (dd, pp), [128, 4, 128], BF16, es)})
                W["T"] = PB[dd]
                W["S"] = PB[dd]
                W["PN"] = Ring([PB[3 + dd], PB[2] if dd == 0 else PB[7]])
                W["PU"] = PB[5 + dd]
                SW.append(W)

            def scan_stage1(dd, ti, pp):
                W = SW[dd]
                P_ = W["par"][pp]
                q, kk, qs, ks, kst = W["q"], W["kk"], W["qs"], W["ks"], W["kst"]
                va, gg, kss, qst, stm = P_["va"], P_["gg"], P_["kss"], P_["qst"], P_["stm"]
                T, S = W["T"], W["S"]
                Tb = T[:].bitcast(BF16)
                LD(gg[:], GGd[ti, :, :], gg)
                LD(q[:], ZQ[ti, :, 0:1024], q)
                LD(kk[:], ZQ[ti, :, 1024:2048], kk)
                LD(va[:, :, 0:256], ZQ[ti, :, 2048:3072].rearrange("p (h d) -> p h d", h=4), va)
                yield
                for h in range(4):
                    hs = slice(h * 256, (h + 1) * 256)
                    A(lambda e: e.activation(out=qs[:, hs], in_=q[:, hs], func=AF.Identity, scale=gg[:, dd * 4 + h:dd * 4 + h + 1]), r=[q, gg], w=[qs])
                    V(lambda e: e.tensor_scalar(out=ks[:, hs], in0=kk[:, hs], scalar1=gg[:, 8 + dd * 4 + h:8 + dd * 4 + h + 1], scalar2=None, op0=ALU.mult), r=[kk, gg], w=[ks])
                yield
                for c in range(8):
                    P(lambda e: e.transpose(out=Tb[:, c * 128:(c + 1) * 128], in_=qs[:, c * 128:(c + 1) * 128], identity=IDB[:]), r=[qs, IDB], w=[T])
                A(lambda e: e.activation(out=qst[:].rearrange("p c t -> p (c t)"), in_=Tb[:, 0:1024], func=AF.Copy), r=[T], w=[qst])
                yield
                for h in range(4):
                    hs = slice(h * 256, (h + 1) * 256)
                    eng_ = "act" if h % 2 == 0 else "dve"
                    if eng_ == "act":
                        A(lambda e: e.activation(out=kss[:, hs], in_=kk[:, hs], func=AF.Identity, scale=gg[:, 16 + dd * 4 + h:16 + dd * 4 + h + 1]), r=[kk, gg], w=[kss])
                    else:
                        V(lambda e: e.tensor_scalar(out=kss[:, hs], in0=kk[:, hs], scalar1=gg[:, 16 + dd * 4 + h:16 + dd * 4 + h + 1], scalar2=None, op0=ALU.mult), r=[kk, gg], w=[kss])
                yield
                for c in range(8):
                    P(lambda e: e.transpose(out=Tb[:, c * 128:(c + 1) * 128], in_=ks[:, c * 128:(c + 1) * 128], identity=IDB[:]), r=[ks, IDB], w=[T])
                V(lambda e: e.tensor_copy(out=kst[:].rearrange("p c t -> p (c t)"), in_=Tb[:, 0:1024]), r=[T], w=[kst])
                yield
                for h in range(4):
                    for jj in range(2):
                        P(lambda e: e.matmul(S[:, h * 128:(h + 1) * 128], lhsT=kst[:, 2 * h + jj, :], rhs=qst[:, 2 * h + jj, :], start=(jj == 0), stop=(jj == 1)), r=[kst, qst], w=[S])
                V(lambda e: e.tensor_tensor(out=stm[:].rearrange("p h t -> p (h t)"), in0=S[:, 0:512], in1=MASK4[:, dd, :, :].rearrange("p h t -> p (h t)"), op=ALU.mult), r=[S, MASK4], w=[stm])
                yield

            def scan_stage2(dd, ti, pp, n_):
                W = SW[dd]
                P_ = W["par"][pp]
                va, gg, kss, qst, stm = P_["va"], P_["gg"], P_["kss"], P_["qst"], P_["stm"]
                c32, cbf = Cst[dd]
                dn, pu = W["dn"], W["PU"]
                ho = W["ho"][n_ % 2]
                for h in range(4):
                    for jj in range(2):
                        P(lambda e: e.matmul(pu[:, 0:257], lhsT=kss[:, h * 256 + jj * 128:h * 256 + (jj + 1) * 128], rhs=va[:, h, :], start=True, stop=True), r=[kss, va], w=[pu])
                        V(lambda e: e.scalar_tensor_tensor(out=c32[:, jj, h, :], in0=c32[:, jj, h, :], scalar=gg[:, 24 + dd * 4 + h:24 + dd * 4 + h + 1], in1=pu[:, 0:257], op0=ALU.mult, op1=ALU.add), r=[c32, gg, pu], w=[c32])
                    pn = W["PN"].next()
                    P(lambda e: e.matmul(pn[:, 0:257], lhsT=stm[:, h, :], rhs=va[:, h, :], start=True, stop=False), r=[stm, va], w=[pn])
                    for jj in range(2):
                        P(lambda e: e.matmul(pn[:, 0:257], lhsT=qst[:, 2 * h + jj, :], rhs=cbf[:, jj, h, :], start=False, stop=(jj == 1)), r=[qst, cbf], w=[pn])
                    V(lambda e: e.tensor_scalar(out=dn[:, 4 + h:5 + h], in0=pn[:, 256:257], scalar1=-1.0, scalar2=1.0, op0=ALU.mult, op1=ALU.max), r=[pn], w=[dn])
                    V(lambda e: e.tensor_tensor(out=dn[:, h:h + 1], in0=dn[:, 4 + h:5 + h], in1=pn[:, 256:257], op=ALU.max), r=[dn, pn], w=[dn])
                    V(lambda e: e.reciprocal(out=dn[:, h:h + 1], in_=dn[:, h:h + 1]), r=[dn], w=[dn])
                    A(lambda e: e.activation(out=ho[:, h * 256:(h + 1) * 256], in_=pn[:, 0:256], func=AF.Identity, scale=dn[:, h:h + 1]), r=[pn, dn], w=[ho])
                    yield
                A(lambda e: e.activation(out=cbf[:, 0, :, :], in_=c32[:, 0, :, :], func=AF.Copy), r=[c32], w=[cbf])
                V(lambda e: e.tensor_copy(out=cbf[:, 1, :, :], in_=c32[:, 1, :, :]), r=[c32], w=[cbf])
                if ti in tiles_out:
                    ST(HD[dd][ti, :, :], ho[:], ho)
                yield

            def scan_stream(dd):
                od = order[dd]
                yield from scan_stage1(dd, od[0], 0)
                for n_, ti in enumerate(od):
                    if n_ + 1 < len(od):
                        yield from scan_stage1(dd, od[n_ + 1], (n_ + 1) % 2)
                    yield from scan_stage2(dd, ti, n_ % 2, n_)

            interleave([scan_stream(0), scan_stream(1)], skew=[0, 3])
        k.barrier()
        if upto == "C" and l == layers - 1:
            break

        with ExitStack() as es:
            es.enter_context(nc.named_scope('E%d' % l))
            WMO = k.sbuf("wmo", [128, 8, D], BF16, es)
            WCO = k.sbuf("wco", [128, 4, D], BF16, es)
            WSO = k.sbuf("wso", [128, 4, D], BF16, es)
            WO = k.sbuf("wo", [128, 8, D], BF16, es)
            for (wb, wd_) in ((WMO, wmo_d), (WCO, wco_d), (WSO, wso_d), (WO, wo_d)):
                k.dma("pool", lambda e: e.dma_start(out=wb[:], in_=wd_[l, :, :].rearrange("(kc p) n -> p kc n", p=128)), writes=[wb], sembuf=wb)
            ROWS = k.sbuf("erows", [64, 128], F32, es)
            CW = k.sbuf("ecw", [128, 4, 32], F32, es)
            SM = k.sbuf("esm", [128, 16], F32, es)
            for c in range(4):
                LD(ROWS[0:31, :], cdw_d[l, :, c * 128:(c + 1) * 128], ROWS)
                P(lambda e: e.transpose(out=PB[0][:, 0:31], in_=ROWS[0:31, :], identity=ID32[0:31, 0:31]), r=[ROWS, ID32], w=[PB[0]])
                V(lambda e: e.tensor_copy(out=CW[:, c, 0:31], in_=PB[0][:, 0:31]), r=[PB[0]], w=[CW])
            LD(ROWS[0:4, :], cdb_d[l, :].rearrange("(r p) -> r p", p=128), ROWS)
            LD(ROWS[4:8, :], clw_d[l, :].rearrange("(r p) -> r p", p=128), ROWS)
            LD(ROWS[8:12, :], clb_d[l, :].rearrange("(r p) -> r p", p=128), ROWS)
            LD(ROWS[12:16, :], sgb_d[l, :, :], ROWS)
            P(lambda e: e.transpose(out=PB[0][:, 0:16], in_=ROWS[0:16, :], identity=ID32[0:16, 0:16]), r=[ROWS, ID32], w=[PB[0]])
            V(lambda e: e.tensor_copy(out=SM[:], in_=PB[0][:, 0:16]), r=[PB[0]], w=[SM])
            SGT = k.sbuf("esgt", [128, 4, 128], BF16, es)
            SGL = k.sbuf("esgl", [128, 128], F32, es)
            for g in range(4):
                LD(SGL[:], sgw_d[l, g, :, :], SGL)
                P(lambda e: e.transpose(out=PB[0][:, 0:128], in_=SGL[:], identity=ID32[:]), r=[SGL, ID32], w=[PB[0]])
                V(lambda e: e.tensor_copy(out=SGT[:, g, :], in_=PB[0][:, 0:128]), r=[PB[0]], w=[SGT])
            MNW = k.sbuf("emnw", [128, D], F32, es)
            SLW = k.sbuf("eslw", [128, 512], F32, es)
            SLB = k.sbuf("eslb", [128, 512], F32, es)
            LD(MNW[:], mnw_d[l, :].partition_broadcast(128), MNW)
            LD(SLW[:], slw_d[l, :].partition_broadcast(128), SLW)
            LD(SLB[:], slb_d[l, :].partition_broadcast(128), SLB)
            G1R = [k.sbuf("eg1r%d" % j, [128, D], F32, es) for j in range(2 if need_ctx else 1)]
            for j in range(len(G1R)):
                bcast_row(G1R[j], MODd[j, 2 * D:3 * D])

            DIAG = k.sbuf("ediag", [128, 4, 31, 128], BF16, es)
            for c in range(4):
                for jt in range(31):
                    eng_ = "dve" if (c * 31 + jt) % 2 == 0 else "pool"
                    k.op(eng_, lambda e: e.tensor_scalar(out=DIAG[:, c, jt, :], in0=ID32[:], scalar1=CW[:, c, jt:jt + 1], scalar2=None, op0=ALU.mult), [ID32, CW], [DIAG])
            UPC = k.sbuf("eupc", [128, 4, 286], BF16, es)
            G(lambda e: e.memset(UPC[:], 0.0), w=[UPC])
            UB = k.sbuf("eub", [128, 512], BF16, es)
            junk = k.sbuf("ejunk", [128, D], BF16, es)
            TMP = k.sbuf("etmp", [128, 512], F32, es)
            WS = []
            for s_ in range(2):
                W = {}
                W["zr"] = k.sbuf("ezr%d" % s_, [128, ZR_W], BF16, es)
                W["hf"] = k.sbuf("ehf%d" % s_, [128, D], F32, es)
                W["xt"] = k.sbuf("ext%d" % s_, [128, D], F32, es)
                W["T1"] = k.sbuf("et1%d" % s_, [128, D], BF16, es)
                W["SS4"] = k.sbuf("ess4%d" % s_, [128, 4], F32, es)
                W["YN"] = k.sbuf("eyn%d" % s_, [128, D], BF16, es)
                W["YNT"] = k.sbuf("eynt%d" % s_, [128, 8, 128], BF16, es)
                W["MG"] = k.sbuf("emg%d" % s_, [128, D], F32, es)
                W["hb"] = W["MG"]
                W["UPL"] = k.sbuf("eupl%d" % s_, [128, 4, 2, 94], BF16, es)
                G(lambda e: e.memset(W["UPL"][:], 0.0), w=[W["UPL"]])
                W["CV"] = k.sbuf("ecv%d" % s_, [128, 4, 128], F32, es)
                W["CSQ"] = k.sbuf("ecsq%d" % s_, [128, 4, 128], F32, es)
                W["M2"] = k.sbuf("em2%d" % s_, [128, 128], F32, es)
                W["RSTD"] = k.sbuf("erstd%d" % s_, [128, 128], F32, es)
                W["CA"] = k.sbuf("eca%d" % s_, [128, 4, 128], BF16, es)
                W["ST2"] = k.sbuf("est2%d" % s_, [128, 4], F32, es)
                W["VNf"] = k.sbuf("evnf%d" % s_, [128, 512], F32, es)
                W["VNb"] = k.sbuf("evnb%d" % s_, [128, 512], BF16, es)
                W["SGO"] = k.sbuf("esgo%d" % s_, [128, 512], BF16, es)
                W["SGOT"] = k.sbuf("esgot%d" % s_, [128, 4, 128], BF16, es)
                W["MGB"] = k.sbuf("emgb%d" % s_, [128, D], BF16, es)
                W["MGT"] = k.sbuf("emgt%d" % s_, [128, 8, 128], BF16, es)
                W["pb"] = [PB[s_ * 4 + i] for i in range(4)]
                W["PY"] = Ring([PB[s_ * 4 + 2], PB[s_ * 4 + 3]])
                WS.append(W)

            if need_ctx:
                for ti in range(2):
                    LD(UB[:], ZR[ti, :, ZR_U:ZR_U + 512], UB)
                    for c in range(4):
                        P(lambda e: e.transpose(out=pbf(0)[:, c * 128:(c + 1) * 128], in_=UB[:, c * 128:(c + 1) * 128], identity=IDB[:]), r=[UB, IDB], w=[PB[0]])
                    V(lambda e: e.tensor_copy(out=UPC[:, :, 15 + ti * 128:15 + (ti + 1) * 128], in_=pbf(0)[:, 0:512].rearrange("p (c t) -> p c t", c=4)), r=[PB[0]], w=[UPC])

            def e_tile(ti, W):
                zr, hf, hb, xt = W["zr"], W["hf"], W["hb"], W["xt"]
                T1, SS4, YN, YNT, MG, UPL, CV, CSQ = W["T1"], W["SS4"], W["YN"], W["YNT"], W["MG"], W["UPL"], W["CV"], W["CSQ"]
                M2, RSTD, CA, ST2, VNf, VNb, SGO, SGOT, MGB, MGT = W["M2"], W["RSTD"], W["CA"], W["ST2"], W["VNf"], W["VNb"], W["SGO"], W["SGOT"], W["MGB"], W["MGT"]
                pT, pS = W["pb"][0], W["pb"][1]
                pTb = pT[:].bitcast(BF16)
                PY = W["PY"]
                j = 1 if ti < 2 else 0
                LD(zr[:], ZR[ti, :, :], zr)
                LD(hf[:], HD[0][ti, :, :], hf)
                LD(hb[:], HD[1][ti, :, :], hb)
                LD(xt[:], xs_cur[ti * 128:(ti + 1) * 128, :], xt)
                yield

                def gate_acc(py, half, goff, first, final):
                    gsl = zr[:, ZR_GM + goff + half * 512:ZR_GM + goff + (half + 1) * 512]
                    hs = slice(half * 512, (half + 1) * 512)
                    if first:
                        V(lambda e: e.tensor_tensor(out=MG[:, hs], in0=py[:, 0:512], in1=gsl, op=ALU.mult), r=[py, zr], w=[MG])
                    else:
                        V(lambda e: e.tensor_tensor(out=TMP[:], in0=py[:, 0:512], in1=gsl, op=ALU.mult), r=[py, zr], w=[TMP])
                        if final:
                            V(lambda e: e.tensor_tensor(out=MGB[:, hs], in0=MG[:, hs], in1=TMP[:], op=ALU.add), r=[MG, TMP], w=[MGB])
                        else:
                            V(lambda e: e.tensor_tensor(out=MG[:, hs], in0=MG[:, hs], in1=TMP[:], op=ALU.add), r=[MG, TMP], w=[MG])

                if ti >= 2:
                    for c in range(4):
                        P(lambda e: e.transpose(out=pTb[:, c * 128:(c + 1) * 128], in_=zr[:, ZR_U + c * 128:ZR_U + (c + 1) * 128], identity=IDB[:]), r=[zr, IDB], w=[pT])
                    for c in range(4):
                        A(lambda e: e.activation(out=UPL[:, c, :, 15:79], in_=pTb[:, c * 128:(c + 1) * 128].rearrange("p (r w) -> p r w", r=2), func=AF.Copy), r=[pT], w=[UPL])

                    def win(c, jt):
                        return UPL[:, c, :, jt:jt + 64]
                    ubuf = UPL
                else:
                    def win(c, jt):
                        return UPC[:, c, ti * 128 + jt:ti * 128 + jt + 128]
                    ubuf = UPC
                yield
                V(lambda e: e.tensor_tensor(out=hf[:], in0=hf[:], in1=hb[:], op=ALU.add), r=[hf, hb], w=[hf])
                for h in range(4):
                    A(lambda e: e.activation(out=junk[:, h * 256:(h + 1) * 256], in_=hf[:, h * 256:(h + 1) * 256], func=AF.Square, accum_out=SS4[:, h:h + 1]), r=[hf], w=[junk, SS4])
                G(lambda e: e.tensor_tensor(out=T1[:], in0=zr[:, ZR_OG:ZR_OG + 1024], in1=MNW[:], op=ALU.mult), r=[zr, MNW], w=[T1])
                yield
                for c in range(4):
                    for jt in range(31):
                        P(lambda e: e.matmul(pS[:, c * 128:(c + 1) * 128], lhsT=DIAG[:, c, jt, :], rhs=win(c, jt), start=(jt == 0), stop=(jt == 30)), r=[DIAG, ubuf], w=[pS])
                    if c % 2 == 1:
                        yield
                A(lambda e: e.activation(out=SS4[:], in_=SS4[:], func=AF.Sqrt, bias=EPSC[:], scale=1.0 / 256.0), r=[SS4, EPSC], w=[SS4])
                V(lambda e: e.reciprocal(out=SS4[:], in_=SS4[:]), r=[SS4], w=[SS4])
                for h in range(4):
                    hs = slice(h * 256, (h + 1) * 256)
                    V(lambda e: e.scalar_tensor_tensor(out=YN[:, hs], in0=hf[:, hs], scalar=SS4[:, h:h + 1], in1=T1[:, hs], op0=ALU.mult, op1=ALU.mult), r=[hf, SS4, T1], w=[YN])
                yield
                for c in range(4):
                    A(lambda e: e.activation(out=CV[:, c, :], in_=pS[:, c * 128:(c + 1) * 128], func=AF.Identity, bias=SM[:, c:c + 1], scale=1.0), r=[pS, SM], w=[CV])
                A(lambda e: e.activation(out=CSQ[:], in_=CV[:], func=AF.Square), r=[CV], w=[CSQ])
                yield
                for c in range(8):
                    P(lambda e: e.transpose(out=pTb[:, c * 128:(c + 1) * 128], in_=YN[:, c * 128:(c + 1) * 128], identity=IDB[:]), r=[YN, IDB], w=[pT])
                A(lambda e: e.activation(out=YNT[:].rearrange("p c t -> p (c t)"), in_=pTb[:, 0:1024], func=AF.Copy), r=[pT], w=[YNT])
                yield
                for c in range(4):
                    P(lambda e: e.matmul(pS[:, 0:128], lhsT=MEAN32[:], rhs=CV[:, c, :], start=(c == 0), stop=(c == 3)), r=[MEAN32, CV], w=[pS])
                for c in range(4):
                    P(lambda e: e.matmul(pS[:, 128:256], lhsT=MEAN32[:], rhs=CSQ[:, c, :], start=(c == 0), stop=(c == 3)), r=[MEAN32, CSQ], w=[pS])
                yield
                for half in range(2):
                    py = PY.next()
                    for kc in range(8):
                        P(lambda e: e.matmul(py[:, 0:512], lhsT=YNT[:, kc, :], rhs=WMO[:, kc, half * 512:(half + 1) * 512], start=(kc == 0), stop=(kc == 7)), r=[YNT, WMO], w=[py])
                    gate_acc(py, half, 0, True, False)
                    yield
                A(lambda e: e.activation(out=M2[:], in_=pS[:, 0:128], func=AF.Square), r=[pS], w=[M2])
                V(lambda e: e.tensor_tensor(out=RSTD[:], in0=pS[:, 128:256], in1=M2[:], op=ALU.subtract), r=[pS, M2], w=[RSTD])
                A(lambda e: e.activation(out=RSTD[:], in_=RSTD[:], func=AF.Sqrt, bias=EPSC[:], scale=1.0), r=[RSTD, EPSC], w=[RSTD])
                V(lambda e: e.reciprocal(out=RSTD[:], in_=RSTD[:]), r=[RSTD], w=[RSTD])
                V(lambda e: e.tensor_copy(out=M2[:], in_=pS[:, 0:128]), r=[pS], w=[M2])
                yield
                for c in range(4):
                    eng_ = "dve"
                    k.op(eng_, lambda e: e.tensor_tensor(out=CSQ[:, c, :], in0=CV[:, c, :], in1=M2[:], op=ALU.subtract), [CV, M2], [CSQ])
                    k.op(eng_, lambda e: e.tensor_tensor(out=CSQ[:, c, :], in0=CSQ[:, c, :], in1=RSTD[:], op=ALU.mult), [CSQ, RSTD], [CSQ])
                yield
                for c in range(4):
                    A(lambda e: e.activation(out=CA[:, c, :], in_=CSQ[:, c, :], func=AF.Silu, bias=SM[:, 8 + c:9 + c], scale=SM[:, 4 + c:5 + c]), r=[CSQ, SM], w=[CA])
                yield
                vv = zr[:, ZR_ZS + 512:ZR_ZS + 1024]
                A(lambda e: e.activation(out=junk[:, 0:512], in_=vv, func=AF.Identity, accum_out=ST2[:, 0:1]), r=[zr], w=[junk, ST2])
                A(lambda e: e.activation(out=junk[:, 512:1024], in_=vv, func=AF.Square, accum_out=ST2[:, 1:2]), r=[zr], w=[junk, ST2])
                yield
                for half in range(2):
                    py = PY.next()
                    for c in range(4):
                        P(lambda e: e.matmul(py[:, 0:512], lhsT=CA[:, c, :], rhs=WCO[:, c, half * 512:(half + 1) * 512], start=(c == 0), stop=(c == 3)), r=[CA, WCO], w=[py])
                    gate_acc(py, half, 1024, False, False)
                yield
                V(lambda e: e.tensor_scalar(out=ST2[:, 0:2], in0=ST2[:, 0:2], scalar1=1.0 / 512.0, scalar2=None, op0=ALU.mult), r=[ST2], w=[ST2])
                V(lambda e: e.tensor_tensor(out=ST2[:, 2:3], in0=ST2[:, 0:1], in1=ST2[:, 0:1], op=ALU.mult), r=[ST2], w=[ST2])
                yield
                V(lambda e: e.tensor_tensor(out=ST2[:, 2:3], in0=ST2[:, 1:2], in1=ST2[:, 2:3], op=ALU.subtract), r=[ST2], w=[ST2])
                A(lambda e: e.activation(out=ST2[:, 2:3], in_=ST2[:, 2:3], func=AF.Sqrt, bias=EPSC[:], scale=1.0), r=[ST2, EPSC], w=[ST2])
                yield
                V(lambda e: e.reciprocal(out=ST2[:, 2:3], in_=ST2[:, 2:3]), r=[ST2], w=[ST2])
                yield
                V(lambda e: e.tensor_scalar(out=VNf[:], in0=vv, scalar1=ST2[:, 0:1], scalar2=ST2[:, 2:3], op0=ALU.subtract, op1=ALU.mult), r=[zr, ST2], w=[VNf])
                yield
                V(lambda e: e.tensor_tensor(out=VNf[:], in0=VNf[:], in1=SLW[:], op=ALU.mult), r=[VNf, SLW], w=[VNf])
                yield
                V(lambda e: e.tensor_tensor(out=VNb[:], in0=VNf[:], in1=SLB[:], op=ALU.add), r=[VNf, SLB], w=[VNb])
                yield
                for g in range(4):
                    P(lambda e: e.matmul(pS[:, g * 128:(g + 1) * 128], lhsT=SGT[:, g, :], rhs=VNb[:, g * 128:(g + 1) * 128], start=True, stop=True), r=[SGT, VNb], w=[pS])
                yield
                for g in range(4):
                    V(lambda e: e.scalar_tensor_tensor(out=SGO[:, g * 128:(g + 1) * 128], in0=pS[:, g * 128:(g + 1) * 128], scalar=SM[:, 12 + g:13 + g], in1=zr[:, ZR_ZS + g * 128:ZR_ZS + (g + 1) * 128], op0=ALU.add, op1=ALU.mult), r=[pS, SM, zr], w=[SGO])
                yield
                for c in range(4):
                    P(lambda e: e.transpose(out=pTb[:, c * 128:(c + 1) * 128], in_=SGO[:, c * 128:(c + 1) * 128], identity=IDB[:]), r=[SGO, IDB], w=[pT])
                A(lambda e: e.activation(out=SGOT[:].rearrange("p c t -> p (c t)"), in_=pTb[:, 0:512], func=AF.Copy), r=[pT], w=[SGOT])
                yield
                for half in range(2):
                    py = PY.next()
                    for c in range(4):
                        P(lambda e: e.matmul(py[:, 0:512], lhsT=SGOT[:, c, :], rhs=WSO[:, c, half * 512:(half + 1) * 512], start=(c == 0), stop=(c == 3)), r=[SGOT, WSO], w=[py])
                    gate_acc(py, half, 2048, False, True)
                yield
                for c in range(8):
                    P(lambda e: e.transpose(out=pTb[:, c * 128:(c + 1) * 128], in_=MGB[:, c * 128:(c + 1) * 128], identity=IDB[:]), r=[MGB, IDB], w=[pT])
                A(lambda e: e.activation(out=MGT[:].rearrange("p c t -> p (c t)"), in_=pTb[:, 0:1024], func=AF.Copy), r=[pT], w=[MGT])
                yield
                for half in range(2):
                    py = PY.next()
                    hs = slice(half * 512, (half + 1) * 512)
                    for kc in range(8):
                        P(lambda e: e.matmul(py[:, 0:512], lhsT=MGT[:, kc, :], rhs=WO[:, kc, hs], start=(kc == 0), stop=(kc == 7)), r=[MGT, WO], w=[py])
                    V(lambda e: e.tensor_tensor(out=MG[:, hs], in0=py[:, 0:512], in1=G1R[j][:, hs], op=ALU.mult), r=[py, G1R[j]], w=[MG])
                    yield
                    G(lambda e: e.tensor_tensor(out=xt[:, hs], in0=MG[:, hs], in1=xt[:, hs], op=ALU.add), r=[MG, xt], w=[xt])
                    yield
                ST(xs_oth[ti * 128:(ti + 1) * 128, :], xt[:], xt)
                yield

            def e_stream(s_):
                for ti in tiles_out[s_::2]:
                    yield from e_tile(ti, WS[s_])

            interleave([e_stream(0), e_stream(1)], skew=[0, 18])
        k.barrier()
        if upto == "E" and l == layers - 1:
            break

        sets = [("lat", list(range(2, NT)), 512)]
        if need_ctx:
            sets.append(("ctx", [0, 1], 32))
        with ExitStack() as es:
            ZERO = k.sbuf("zero", [128, 1024], F32, es)
            IOTAF = k.sbuf("iotaf", [128, 512], F32, es)
            IOTAI = k.sbuf("iotai", [128, 512], I32, es)
            G(lambda e: e.memset(ZERO[:], 0.0), w=[ZERO])
            G(lambda e: e.iota(IOTAI[:], pattern=[[1, 512]], base=0, channel_multiplier=0), w=[IOTAI])
            V(lambda e: e.tensor_copy(out=IOTAF[:], in_=IOTAI[:]), r=[IOTAI], w=[IOTAF])
            RW = k.sbuf("frw", [128, 8, NE], F32, es)
            LD(RW[:], rw_d[l, :, :].rearrange("(kc p) n -> p kc n", p=128), RW)
            RBR = k.sbuf("frbr", [128, NE], F32, es)
            LD(RBR[:], rb_d[l, :].partition_broadcast(128), RBR)
            AFF = k.sbuf("faff", [128, NT, NE], F32, es)
            W2R = [k.sbuf("fw2r%d" % j, [128, D], F32, es) for j in range(len(sets))]
            S2R = [k.sbuf("fs2r%d" % j, [128, D], F32, es) for j in range(len(sets))]
            for j in range(len(sets)):
                bcast_row(W2R[j], WSd[j, 1, :])
                bcast_row(S2R[j], MODd[j, 3 * D:4 * D])

            with ExitStack() as es1:
                es1.enter_context(nc.named_scope('F1_%d' % l))
                junk = k.sbuf("fjunk", [128, D], BF16, es1)
                FW = []
                for s_ in range(2):
                    FW.append({"xt": k.sbuf("fxt%d" % s_, [128, D], F32, es1), "xn": k.sbuf("fxn%d" % s_, [128, D], F32, es1),
                               "tmp": k.sbuf("ftmp%d" % s_, [128, D], F32, es1),
                               "ssq": k.sbuf("fssq%d" % s_, [128, 1], F32, es1), "rt": k.sbuf("frt%d" % s_, [128, 1], F32, es1),
                               "h2T": k.sbuf("fh2t%d" % s_, [128, 8, 128], F32, es1), "h2r": k.sbuf("fh2r%d" % s_, [128, D], BF16, es1),
                               "LG": k.sbuf("flg%d" % s_, [128, NE], F32, es1), "MX": k.sbuf("fmx%d" % s_, [128, 2], F32, es1),
                               "pt": PB[s_], "pr": PB[2 + s_]})

                def f1_tile(ti, W):
                    xt, xn, tmp, ssq, rt, h2T, h2r, LG, MX, pr = W["xt"], W["xn"], W["tmp"], W["ssq"], W["rt"], W["h2T"], W["h2r"], W["LG"], W["MX"], W["pr"]
                    j = 1 if ti < 2 else 0
                    LD(xt[:], xs_oth[ti * 128:(ti + 1) * 128, :], xt)
                    k.dma("pool", lambda e: e.dma_start(out=MACC[ti * 128:(ti + 1) * 128, :], in_=ZERO[:]), reads=[ZERO], sembuf=ZERO)
                    yield
                    rms_to_xn(xt, xn, junk, ssq, rt)
                    yield
                    xn_to_hT(xn, h2T, j, 2, 3, pbs=(W["pt"], W["pt"]))
                    yield
                    V(lambda e: e.tensor_tensor(out=tmp[:], in0=xn[:], in1=W2R[j][:], op=ALU.mult), r=[xn, W2R[j]], w=[tmp])
                    yield
                    V(lambda e: e.tensor_tensor(out=h2r[:], in0=tmp[:], in1=S2R[j][:], op=ALU.add), r=[tmp, S2R[j]], w=[h2r])
                    ST(H2d[ti * 128:(ti + 1) * 128, :], h2r[:], h2r)
                    yield
                    for kc in range(8):
                        P(lambda e: e.matmul(pr[:, 0:NE], lhsT=h2T[:, kc, :], rhs=RW[:, kc, :], start=(kc == 0), stop=(kc == 7)), r=[h2T, RW], w=[pr])
                    yield
                    V(lambda e: e.tensor_tensor(out=LG[:], in0=pr[:, 0:NE], in1=RBR[:], op=ALU.add), r=[pr, RBR], w=[LG])
                    yield
                    V(lambda e: e.reduce_max(out=MX[:, 0:1], in_=LG[:], axis=mybir.AxisListType.X), r=[LG], w=[MX])
                    yield
                    V(lambda e: e.tensor_scalar(out=MX[:, 0:1], in0=MX[:, 0:1], scalar1=-1.0, scalar2=None, op0=ALU.mult), r=[MX], w=[MX])
                    yield
                    A(lambda e: e.activation(out=LG[:], in_=LG[:], func=AF.Exp, bias=MX[:, 0:1], scale=1.0, accum_out=MX[:, 1:2]), r=[LG, MX], w=[LG, MX])
                    yield
                    V(lambda e: e.reciprocal(out=MX[:, 1:2], in_=MX[:, 1:2]), r=[MX], w=[MX])
                    yield
                    V(lambda e: e.tensor_scalar(out=AFF[:, ti, :], in0=LG[:], scalar1=MX[:, 1:2], scalar2=None, op0=ALU.mult), r=[LG, MX], w=[AFF])
                    yield

                def f1_stream(s_):
                    for ti in tiles_out[s_::2]:
                        yield from f1_tile(ti, FW[s_])

                interleave([f1_stream(0), f1_stream(1)], skew=[0, 6])
            k.barrier()

            TOKI = k.sbuf("ftoki", [128, NE, 5], I32, es)
            GS = k.sbuf("fgs", [128, NE, 5], F32, es)
            with ExitStack() as es2:
                es2.enter_context(nc.named_scope('F3_%d' % l))
                AFFT = k.sbuf("fafft", [16, SEQ], F32, es2)
                JK = k.sbuf("fjk", [16, SEQ], F32, es2)
                BS = k.sbuf("fbs", [16, 8], F32, es2)
                DG = k.sbuf("fdg", [16, 16], F32, es2)
                THRB = k.sbuf("fthrb", [128, NE], F32, es2)
                MK = k.sbuf("fmk", [128, 32, NE], F32, es2)
                MKB = k.sbuf("fmkb", [128, 32, NE], BF16, es2)
                OFFS = k.sbuf("foffs", [128, 32, NE], F32, es2)
                POS = k.sbuf("fpos", [128, 32, NE], F32, es2)
                RH = k.sbuf("frh", [128, 32, NE, 5], BF16, es2)
                R1 = k.sbuf("fr1", [128, 32, NE], F32, es2)
                R2 = k.sbuf("fr2", [128, 32, NE], F32, es2)
                OH = Ring([k.sbuf("foh%d" % i, [128, 512], BF16, es2) for i in range(6)])
                TF = k.sbuf("ftf", [128, 8], F32, es2)
                for si, (sname, stiles, cap) in enumerate(sets):
                    nt = len(stiles)
                    ntok = nt * 128
                    t0 = stiles[0]
                    nst = (cap + 127) // 128
                    for q in range(nt):
                        P(lambda e: e.transpose(out=PB[0][0:16, (q % 4) * 128:(q % 4 + 1) * 128], in_=AFF[:, t0 + q, :], identity=ID32[:]), r=[AFF, ID32], w=[PB[0]])
                        if q % 4 == 3 or q == nt - 1:
                            q0 = (q // 4) * 4
                            wq = (q - q0 + 1) * 128
                            V(lambda e: e.tensor_copy(out=AFFT[:, q0 * 128:q0 * 128 + wq], in_=PB[0][0:16, 0:wq]), r=[PB[0]], w=[AFFT])
                    V(lambda e: e.memset(BS[:, 0:1], 0.0), w=[BS])
                    V(lambda e: e.memset(BS[:, 1:2], 1.0), w=[BS])
                    for it in range(26):
                        V(lambda e: e.tensor_scalar(out=BS[:, 2:3], in0=BS[:, 0:1], scalar1=BS[:, 1:2], scalar2=0.5, op0=ALU.add, op1=ALU.mult), r=[BS], w=[BS])
                        V(lambda e: e.tensor_scalar(out=JK[:, 0:ntok], in0=AFFT[:, 0:ntok], scalar1=BS[:, 2:3], scalar2=0.0, op0=ALU.is_ge, op1=ALU.add, accum_out=BS[:, 3:4]), r=[AFFT, BS], w=[JK, BS])
                        V(lambda e: e.tensor_scalar(out=BS[:, 4:5], in0=BS[:, 3:4], scalar1=float(cap) - 0.5, scalar2=None, op0=ALU.is_ge), r=[BS], w=[BS])
                        V(lambda e: e.tensor_tensor(out=BS[:, 5:6], in0=BS[:, 2:3], in1=BS[:, 0:1], op=ALU.subtract), r=[BS], w=[BS])
                        V(lambda e: e.tensor_tensor(out=BS[:, 6:7], in0=BS[:, 1:2], in1=BS[:, 2:3], op=ALU.subtract), r=[BS], w=[BS])
                        V(lambda e: e.scalar_tensor_tensor(out=BS[:, 0:1], in0=BS[:, 5:6], scalar=BS[:, 4:5], in1=BS[:, 0:1], op0=ALU.mult, op1=ALU.add), r=[BS], w=[BS])
                        V(lambda e: e.scalar_tensor_tensor(out=BS[:, 1:2], in0=BS[:, 6:7], scalar=BS[:, 4:5], in1=BS[:, 2:3], op0=ALU.mult, op1=ALU.add), r=[BS], w=[BS])
                    V(lambda e: e.tensor_scalar(out=DG[:], in0=ID32[0:16, 0:16], scalar1=BS[:, 0:1], scalar2=None, op0=ALU.mult), r=[ID32, BS], w=[DG])
                    P(lambda e: e.matmul(PB[1][:, 0:NE], lhsT=ONES32[0:16, :], rhs=DG[:], start=True, stop=True), r=[ONES32, DG], w=[PB[1]])
                    V(lambda e: e.tensor_copy(out=THRB[:], in_=PB[1][:, 0:NE]), r=[PB[1]], w=[THRB])
                    for q in range(nt):
                        V(lambda e: e.tensor_tensor(out=MK[:, q, :], in0=AFF[:, t0 + q, :], in1=THRB[:], op=ALU.is_ge), r=[AFF, THRB], w=[MK])
                    ncol = nt * NE
                    mkf = MK[:, 0:nt, :].rearrange("p t e -> p (t e)")
                    V(lambda e: e.tensor_copy(out=MKB[:, 0:nt, :].rearrange("p t e -> p (t e)"), in_=mkf), r=[MK], w=[MKB])
                    P(lambda e: e.matmul(PB[2][:, 0:ncol], lhsT=SUB[:], rhs=MKB[:, 0:nt, :].rearrange("p t e -> p (t e)"), start=True, stop=True), r=[SUB, MKB], w=[PB[2]])
                    P(lambda e: e.matmul(PB[3][:, 0:ncol], lhsT=ONESB[:], rhs=MKB[:, 0:nt, :].rearrange("p t e -> p (t e)"), start=True, stop=True), r=[ONESB, MKB], w=[PB[3]])
                    V(lambda e: e.memset(OFFS[:, 0, :], 0.0), w=[OFFS])
                    for q in range(1, nt):
                        V(lambda e: e.tensor_tensor(out=OFFS[:, q, :], in0=OFFS[:, q - 1, :], in1=PB[3][:, (q - 1) * NE:q * NE], op=ALU.add), r=[OFFS, PB[3]], w=[OFFS])
                    posf = POS[:, 0:nt, :].rearrange("p t e -> p (t e)")
                    V(lambda e: e.tensor_tensor(out=posf, in0=PB[2][:, 0:ncol], in1=OFFS[:, 0:nt, :].rearrange("p t e -> p (t e)"), op=ALU.add), r=[PB[2], OFFS], w=[POS])
                    V(lambda e: e.tensor_scalar(out=R1[:, 0:nt, :].rearrange("p t e -> p (t e)"), in0=posf, scalar1=float(cap) - 0.5, scalar2=None, op0=ALU.is_lt), r=[POS], w=[R1])
                    V(lambda e: e.tensor_tensor(out=mkf, in0=mkf, in1=R1[:, 0:nt, :].rearrange("p t e -> p (t e)"), op=ALU.mult), r=[MK, R1], w=[MK])
                    for q in range(nt):
                        V(lambda e: e.memset(RH[:, q, :, 0], float((t0 + q) * 128)), w=[RH])
                        V(lambda e: e.tensor_scalar(out=RH[:, q, :, 1], in0=ONES32[:, 0:NE], scalar1=PIDX[:, 0:1], scalar2=None, op0=ALU.mult), r=[ONES32, PIDX], w=[RH])
                    afs = AFF[:, t0:t0 + nt, :]
                    V(lambda e: e.tensor_copy(out=RH[:, 0:nt, :, 2], in_=afs), r=[AFF], w=[RH])
                    V(lambda e: e.tensor_tensor(out=R1[:, 0:nt, :], in0=afs, in1=RH[:, 0:nt, :, 2], op=ALU.subtract), r=[AFF, RH], w=[R1])
                    V(lambda e: e.tensor_copy(out=RH[:, 0:nt, :, 3], in_=R1[:, 0:nt, :]), r=[R1], w=[RH])
                    V(lambda e: e.tensor_tensor(out=R2[:, 0:nt, :], in0=R1[:, 0:nt, :], in1=RH[:, 0:nt, :, 3], op=ALU.subtract), r=[R1, RH], w=[R2])
                    V(lambda e: e.tensor_copy(out=RH[:, 0:nt, :, 4], in_=R2[:, 0:nt, :]), r=[R2], w=[RH])
                    PQ = [PB[4], PB[5], PB[6], PB[7]]
                    capw = nst * 128 if cap >= 128 else cap
                    for ex in range(NE):
                        for q in range(nt):
                            oh = OH.next()
                            k.op("pool" if q % 4 == 3 else "dve", lambda e: e.tensor_scalar(out=oh[:, 0:capw], in0=IOTAF[:, 0:capw], scalar1=POS[:, q, ex:ex + 1], scalar2=MK[:, q, ex:ex + 1], op0=ALU.is_equal, op1=ALU.mult), [IOTAF, POS, MK], [oh])
                            for s_ in range(nst):
                                mw = min(128, cap - s_ * 128)
                                P(lambda e: e.matmul(PQ[s_][0:mw, 0:5], lhsT=oh[:, s_ * 128:s_ * 128 + mw], rhs=RH[:, q, ex, :], start=(q == 0), stop=(q == nt - 1)), r=[oh, RH], w=[PQ[s_]])
                        for s_ in range(nst):
                            mw = min(128, cap - s_ * 128)
                            sl = s_ if si == 0 else 4
                            V(lambda e: e.tensor_copy(out=TF[0:mw, 0:5], in_=PQ[s_][0:mw, 0:5]), r=[PQ[s_]], w=[TF])
                            V(lambda e: e.tensor_tensor(out=TF[0:mw, 5:6], in0=TF[0:mw, 0:1], in1=TF[0:mw, 1:2], op=ALU.add), r=[TF], w=[TF])
                            V(lambda e: e.tensor_copy(out=TOKI[0:mw, ex, sl:sl + 1], in_=TF[0:mw, 5:6]), r=[TF], w=[TOKI])
                            V(lambda e: e.tensor_tensor(out=TF[0:mw, 6:7], in0=TF[0:mw, 2:3], in1=TF[0:mw, 3:4], op=ALU.add), r=[TF], w=[TF])
                            V(lambda e: e.tensor_tensor(out=GS[0:mw, ex, sl:sl + 1], in0=TF[0:mw, 6:7], in1=TF[0:mw, 4:5], op=ALU.add), r=[TF], w=[GS])
            k.barrier()

            with ExitStack() as es3:
                es3.enter_context(nc.named_scope('F5_%d' % l))
                WPR = Ring([k.sbuf("fwp%d" % i, [128, 16, 512], BF16, es3) for i in range(3)])
                NS = 544 if need_ctx else 512
                nsl = 5 if need_ctx else 4
                XE2 = [[k.sbuf("fxe%d_%d" % (pp, i), [128, D], BF16, es3) for i in range(nsl)] for pp in range(2)]
                XET2 = [k.sbuf("fxet%d" % pp, [128, 8, NS], BF16, es3) for pp in range(2)]
                HMT = k.sbuf("fhmt", [128, 16, NS], BF16, es3)
                SA = Ring([k.sbuf("fsa%d" % i, [128, NS], F32, es3) for i in range(2)])
                YS = [k.sbuf("fys%d" % i, [128, D], F32, es3) for i in range(5 if need_ctx else 4)]
                slot_tiles = [(s_, 128, s_ * 128) for s_ in range(4)]
                if need_ctx:
                    slot_tiles.append((4, 32, 512))
                pieces = []
                for ex in range(NE):
                    for g in range(4):
                        pieces.append((ex, "g", g))
                    for hf_ in range(2):
                        pieces.append((ex, "d", hf_))
                loaded = {}

                def issue_piece(pi):
                    if pi >= len(pieces) or pi in loaded:
                        return
                    ex, kind, g = pieces[pi]
                    wp = WPR.next()
                    if kind == "g":
                        k.dma("pool", lambda e: e.dma_start(out=wp[:, 0:8, :], in_=wg_d[l, ex, :, g * 512:(g + 1) * 512].rearrange("(kc p) n -> p kc n", p=128)), writes=[wp], sembuf=wp)
                        k.dma("pool", lambda e: e.dma_start(out=wp[:, 8:16, :], in_=wu_d[l, ex, :, g * 512:(g + 1) * 512].rearrange("(kc p) n -> p kc n", p=128)), writes=[], reads=[], sembuf=wp)
                        wp.writes = {("d", wp.dsem): k.dcount[wp.dsem]}
                    else:
                        k.dma("pool", lambda e: e.dma_start(out=wp[:], in_=wd_d[l, ex, :, g * 512:(g + 1) * 512].rearrange("(fc p) n -> p fc n", p=128)), writes=[wp], sembuf=wp)
                    loaded[pi] = wp

                def f5_gather(ex):
                    for (s_, mw, off) in slot_tiles:
                        xe = XE2[ex % 2][s_]
                        k.dma("pool", lambda e: e.indirect_dma_start(out=xe[0:mw, :], out_offset=None, in_=H2d[:, :], in_offset=bass.IndirectOffsetOnAxis(ap=TOKI[0:mw, ex, s_:s_ + 1], axis=0)), reads=[TOKI], writes=[xe], sembuf=xe)

                def f5_xet(ex):
                    for (s_, mw, off) in slot_tiles:
                        xe = XE2[ex % 2][s_]
                        for c in range(8):
                            P(lambda e: e.transpose(out=pbf(6)[:, c * 128:c * 128 + mw], in_=xe[0:mw, c * 128:(c + 1) * 128], identity=IDB[0:mw, 0:mw]), r=[xe, IDB], w=[PB[6]])
                        V(lambda e: e.tensor_copy(out=XET2[ex % 2][:, :, off:off + mw], in_=pbf(6)[:, 0:1024].rearrange("p (c t) -> p c t", c=8)[:, :, 0:mw]), r=[PB[6]], w=[XET2[ex % 2]])

                PA = Ring([PB[0], PB[1]])
                PUu = Ring([PB[2], PB[3]])
                PYr = Ring([PB[4], PB[5]])
                issue_piece(0)
                issue_piece(1)
                pi = 0
                prev_tokens = {}
                for ex in range(NE):
                    XET = XET2[ex % 2]
                    f5_gather(ex)
                    f5_xet(ex)
                    for g in range(4):
                        wp = loaded.pop(pi)
                        issue_piece(pi + 2)
                        pi += 1
                        for f in range(4):
                            fc = g * 4 + f
                            pa = PA.next(); pu = PUu.next()
                            for (wsel, pp) in ((0, pa), (8, pu)):
                                for kc in range(8):
                                    P(lambda e: e.matmul(pp[:, 0:512], lhsT=wp[:, wsel + kc, f * 128:(f + 1) * 128], rhs=XET[:, kc, 0:512], start=(kc == 0), stop=(kc == 7)), r=[wp, XET], w=[pp])
                            sa = SA.next()
                            A(lambda e: e.activation(out=sa[:, 0:512], in_=pa[:, 0:512], func=AF.Silu), r=[pa], w=[sa])
                            V(lambda e: e.tensor_tensor(out=HMT[:, fc, 0:512], in0=sa[:, 0:512], in1=pu[:, 0:512], op=ALU.mult), r=[sa, pu], w=[HMT])
                            if need_ctx:
                                for (wsel, c0_) in ((0, 0), (8, 32)):
                                    for kc in range(8):
                                        P(lambda e: e.matmul(PB[7][:, c0_:c0_ + 32], lhsT=wp[:, wsel + kc, f * 128:(f + 1) * 128], rhs=XET[:, kc, 512:544], start=(kc == 0), stop=(kc == 7)), r=[wp, XET], w=[PB[7]])
                                A(lambda e: e.activation(out=sa[:, 512:544], in_=PB[7][:, 0:32], func=AF.Silu), r=[PB[7]], w=[sa])
                                V(lambda e: e.tensor_tensor(out=HMT[:, fc, 512:544], in0=sa[:, 512:544], in1=PB[7][:, 32:64], op=ALU.mult), r=[sa, PB[7]], w=[HMT])
                    wpd = []
                    wpd.append(loaded.pop(pi))
                    issue_piece(pi + 2)
                    pi += 1
                    wpd.append(loaded.pop(pi))
                    pi += 1
                    cur_tokens = {}
                    for (s_, mw, off) in slot_tiles:
                        for hf_ in range(2):
                            py = PYr.next()
                            for fc in range(16):
                                P(lambda e: e.matmul(py[0:mw, 0:512], lhsT=HMT[:, fc, off:off + mw], rhs=wpd[hf_][:, fc, :], start=(fc == 0), stop=(fc == 15)), r=[HMT, wpd[hf_]], w=[py])
                            A(lambda e: e.activation(out=YS[s_][0:mw, hf_ * 512:(hf_ + 1) * 512], in_=py[0:mw, 0:512], func=AF.Identity, scale=GS[0:mw, ex, s_:s_ + 1]), r=[py, GS], w=[YS[s_]])
                        MACC.writes = dict(prev_tokens)
                        MACC.readers = {}
                        k.dma("pool", lambda e: e.indirect_dma_start(out=MACC[:, :], out_offset=bass.IndirectOffsetOnAxis(ap=TOKI[0:mw, ex, s_:s_ + 1], axis=0), in_=YS[s_][0:mw, :], in_offset=None, compute_op=ALU.add), reads=[TOKI, YS[s_]], writes=[MACC], sembuf=YS[s_])
                        _merge(cur_tokens, MACC.writes)
                    prev_tokens = cur_tokens
                    MACC.writes = dict(prev_tokens)
                    issue_piece(pi + 1)
            k.barrier()

            with ExitStack() as es4:
                es4.enter_context(nc.named_scope('F6_%d' % l))
                G2R = [k.sbuf("fg2r%d" % j, [128, D], F32, es4) for j in range(len(sets))]
                for j in range(len(sets)):
                    bcast_row(G2R[j], MODd[j, 5 * D:6 * D])
                XL = Ring([k.sbuf("gxt%d" % i, [128, D], F32, es4) for i in range(3)])
                ML = Ring([k.sbuf("gml%d" % i, [128, D], F32, es4) for i in range(3)])
                XO = Ring([k.sbuf("gxo%d" % i, [128, D], F32, es4) for i in range(3)])
                junk = k.sbuf("gjunk", [128, D], BF16, es4)
                SSQR = Ring([k.sbuf("gssq%d" % i, [128, 1], F32, es4) for i in range(2)])
                RTR = Ring([k.sbuf("grt%d" % i, [128, 1], F32, es4) for i in range(2)])
                if last:
                    FNW = k.sbuf("gfnw", [128, D], F32, es4)
                    LD(FNW[:], fnw_d.t.partition_broadcast(128), FNW)
                for ti in tiles_out:
                    j = 1 if ti < 2 else 0
                    xt = XL.next(); ml = ML.next(); xo = XO.next()
                    LD(xt[:], xs_oth[ti * 128:(ti + 1) * 128, :], xt)
                    LD(ml[:], MACC[ti * 128:(ti + 1) * 128, :], ml)
                    V(lambda e: e.tensor_tensor(out=ml[:], in0=ml[:], in1=G2R[j][:], op=ALU.mult), r=[ml, G2R[j]], w=[ml])
                    if not last:
                        V(lambda e: e.tensor_tensor(out=xo[:], in0=ml[:], in1=xt[:], op=ALU.add), r=[ml, xt], w=[xo])
                        ST(xs_cur[ti * 128:(ti + 1) * 128, :], xo[:], xo)
                    else:
                        V(lambda e: e.tensor_tensor(out=xt[:], in0=ml[:], in1=xt[:], op=ALU.add), r=[ml, xt], w=[xt])
                        rms_to_xn(xt, ml, junk, SSQR.next(), RTR.next())
                        V(lambda e: e.tensor_tensor(out=xo[:], in0=ml[:], in1=FNW[:], op=ALU.mult), r=[ml, FNW], w=[xo])
                        ST(out_d[(ti - 2) * 128:(ti - 1) * 128, :], xo[:], xo)
        k.barrier()
    k.finish()
    return nc, k


_CACHE = {}


def kernel(**inputs):
    n = 8
    if "nc" not in _CACHE:
        _CACHE["nc"] = build_program()[0]
    nc = _CACHE["nc"]
    shared = {kk: np.ascontiguousarray(v, dtype=np.float32) for kk, v in inputs.items() if kk not in ("x", "c", "ctx")}
    in_maps = []
    for b in range(n):
        m = dict(shared)
        m["x"] = np.ascontiguousarray(inputs["x"][b], dtype=np.float32)
        m["c"] = np.ascontiguousarray(inputs["c"][b], dtype=np.float32)
        m["ctx"] = np.ascontiguousarray(inputs["ctx"][b], dtype=np.float32)
        in_maps.append(m)
    res = run_bass_kernel_spmd(nc, in_maps, core_ids=list(range(n)))
    return np.stack([np.asarray(r["out"], dtype=np.float32) for r in res.results], axis=0)
```

```python
import numpy as np
from contextlib import ExitStack
import concourse.bass as bass
import concourse.mybir as mybir
from concourse.bass_utils import run_bass_kernel_spmd

F32 = mybir.dt.float32
BF16 = mybir.dt.bfloat16
I32 = mybir.dt.int32
AF = mybir.ActivationFunctionType
ALU = mybir.AluOpType

D = 1024
SEQ = 4096
CTX = 256
DEPTH = 2
NT = 34
NROW = NT * 128
NE = 16
FF = 2048
N_IN = 9232
NST = 3088
EPS = 1e-6
ZR_OG, ZR_U, ZR_ZS, ZR_GM, ZR_W = 0, 1024, 1536, 2560, 5632


class Buf:
    __slots__ = ("t", "name", "writes", "readers", "dsem", "dcnt")

    def __init__(self, t, name):
        self.t = t
        self.name = name
        self.writes = {}
        self.readers = {}
        self.dsem = None
        self.dcnt = 0

    def __getitem__(self, key):
        return self.t[key]


def _merge(d, s):
    for k, v in s.items():
        if d.get(k, 0) < v:
            d[k] = v


class KB:
    def __init__(self, nc):
        self.nc = nc
        self.es = ExitStack()
        self.eng = {"pe": nc.tensor, "dve": nc.vector, "act": nc.scalar, "pool": nc.gpsimd, "sp": nc.sync}
        self.semh = {}
        self.cnt = {}
        self.seen = {k: {} for k in self.eng}
        for k in self.eng:
            self.semh[k] = self.es.enter_context(nc.semaphore("s_" + k))
            self.cnt[k] = 0
        self.dfree = []
        self.dcount = []
        self.nop = 0

    def sbuf(self, name, shape, dtype, es=None):
        self.uid = getattr(self, "uid", 0) + 1
        name = "%s_u%d" % (name, self.uid)
        t = (es or self.es).enter_context(self.nc.sbuf_tensor(name, list(shape), dtype))
        b = Buf(t, name)
        if es is not None:
            es.callback(self._release, b)
        return b

    def _release(self, b):
        if b.dsem is not None:
            self.dfree.append(b.dsem)
            b.dsem = None

    def _dsem_for(self, b):
        if b.dsem is None:
            if self.dfree:
                b.dsem = self.dfree.pop()
            else:
                idx = len(self.dcount)
                h = self.es.enter_context(self.nc.semaphore("dq%d" % idx))
                self.dcount.append(0)
                self.semh[("d", idx)] = h
                b.dsem = idx
        return b.dsem

    def psum(self, name, shape, dtype, es=None):
        t = (es or self.es).enter_context(self.nc.psum_tensor(name, list(shape), dtype))
        return Buf(t, name)

    def dram(self, name, shape, dtype, kind="Internal"):
        t = self.nc.dram_tensor(name, list(shape), dtype, kind=kind)
        return Buf(t.ap(), name)

    def _wait(self, e, need):
        eng = self.eng[e]
        seen = self.seen[e]
        for key, v in need.items():
            if seen.get(key, 0) < v:
                eng.wait_ge(self.semh[key], v)
                seen[key] = v

    def op(self, e, fn, reads=(), writes=()):
        raw = {}
        oth = {}
        for b in reads:
            _merge(raw, b.writes)
        for b in writes:
            _merge(oth, b.readers)
            _merge(oth, b.writes)
        need = dict(raw)
        for key, v in oth.items():
            if key == e:
                continue
            if need.get(key, 0) < v:
                need[key] = v
        if e == "pe":
            need.pop("pe", None)
        self._wait(e, need)
        ins = fn(self.eng[e])
        self.cnt[e] += 1
        ins.then_inc(self.semh[e], 1)
        tok = {e: self.cnt[e]}
        for b in reads:
            _merge(b.readers, tok)
        for b in writes:
            b.writes = dict(tok)
            b.readers = {}
        self.nop += 1
        return ins

    def dma(self, q, fn, reads=(), writes=(), sembuf=None):
        need = {}
        for b in reads:
            _merge(need, b.writes)
        for b in writes:
            _merge(need, b.readers)
            _merge(need, b.writes)
        self._wait(q, need)
        idx = self._dsem_for(sembuf)
        key = ("d", idx)
        ins = fn(self.eng[q])
        self.dcount[idx] += 16
        ins.then_inc(self.semh[key], 16)
        tok = {key: self.dcount[idx]}
        for b in reads:
            _merge(b.readers, tok)
        for b in writes:
            b.writes = dict(tok)
            b.readers = {}
        self.nop += 1
        return ins

    def barrier(self):
        need = {k: c for k, c in self.cnt.items() if c > 0}
        for idx, c in enumerate(self.dcount):
            if c > 0:
                need[("d", idx)] = c
        for e in self.eng:
            self._wait(e, dict(need))

    def finish(self):
        self.barrier()
        self.es.close()


class Ring:
    def __init__(self, bufs):
        self.bufs = bufs
        self.i = 0

    def next(self):
        b = self.bufs[self.i % len(self.bufs)]
        self.i += 1
        return b


def interleave(gens, skew=None):
    gens = list(gens)
    delay = {id(g): (skew[i] if skew else 0) for i, g in enumerate(gens)}
    while gens:
        for g in list(gens):
            if delay[id(g)] > 0:
                delay[id(g)] -= 1
                continue
            try:
                next(g)
            except StopIteration:
                gens.remove(g)


def build_program(layers=DEPTH, upto="all", dbg=False):
    nc = bass.Bass("TRN2", target_bir_lowering=False)
    k = KB(nc)
    ins_ = {}

    def din(name, shape):
        ins_[name] = k.dram(name, shape, F32, kind="ExternalInput")
        return ins_[name]

    x_d = din("x", [SEQ, D]); c_d = din("c", [D]); ctx_d = din("ctx", [CTX, D]); cctx_d = din("c_ctx", [D])
    ada_w_d = din("ada_w", [DEPTH, D, 6 * D]); ada_b_d = din("ada_b", [DEPTH, 6 * D])
    n1_d = din("norm1_w", [DEPTH, D]); n2_d = din("norm2_w", [DEPTH, D])
    w_in_d = din("w_in", [DEPTH, D, N_IN]); gb_d = din("mlstm_gate_b", [DEPTH, 16])
    mnw_d = din("mlstm_norm_w", [DEPTH, D]); wmo_d = din("w_mlstm_out", [DEPTH, D, D])
    cdw_d = din("conv_dw_w", [DEPTH, 31, 512]); cdb_d = din("conv_dw_b", [DEPTH, 512])
    clw_d = din("conv_ln_w", [DEPTH, 512]); clb_d = din("conv_ln_b", [DEPTH, 512])
    wco_d = din("w_conv_out", [DEPTH, 512, D])
    slw_d = din("sg_ln_w", [DEPTH, 512]); slb_d = din("sg_ln_b", [DEPTH, 512])
    sgw_d = din("sg_w", [DEPTH, 4, 128, 128]); sgb_d = din("sg_b", [DEPTH, 4, 128])
    wso_d = din("w_sg_out", [DEPTH, 512, D]); wo_d = din("w_o", [DEPTH, D, D])
    rw_d = din("router_w", [DEPTH, D, NE]); rb_d = din("router_b", [DEPTH, NE])
    wg_d = din("expert_w_gate", [DEPTH, NE, D, FF]); wu_d = din("expert_w_up", [DEPTH, NE, D, FF])
    wd_d = din("expert_w_down", [DEPTH, NE, FF, D]); fnw_d = din("final_norm_w", [D])
    out_d = k.dram("out", [SEQ, D], F32, kind="ExternalOutput")

    sk = "ExternalOutput" if dbg else "Internal"
    XA = k.dram("XA", [NROW, D], F32, kind=sk)
    XB = k.dram("XB", [NROW, D], F32, kind=sk)
    ZQ = k.dram("ZQ", [NT, 128, 3072], BF16, kind=sk)
    GGd = k.dram("GGd", [NT, 128, 32], F32, kind=sk)
    ZR = k.dram("ZR", [NT, 128, ZR_W], BF16, kind=sk)
    HD = [k.dram("HF", [NT, 128, D], F32, kind=sk), k.dram("HB", [NT, 128, D], F32, kind=sk)]
    H2d = k.dram("H2d", [NROW, D], BF16, kind=sk)
    MACC = k.dram("MACC", [NROW, D], F32, kind=sk)
    MODd = k.dram("MODd", [2, 6 * D], F32, kind=sk)
    WSd = k.dram("WSd", [2, 2, D], F32, kind=sk)
    dbg_d = {}

    def V(fn, r=(), w=()):
        return k.op("dve", fn, r, w)

    def A(fn, r=(), w=()):
        return k.op("act", fn, r, w)

    def P(fn, r=(), w=()):
        return k.op("pe", fn, r, w)

    def G(fn, r=(), w=()):
        return k.op("pool", fn, r, w)

    def LD(out_ap, in_ap, buf, q="sp"):
        return k.dma(q, lambda e: e.dma_start(out=out_ap, in_=in_ap), writes=[buf], sembuf=buf)

    def ST(out_ap, in_ap, buf, q="pool"):
        return k.dma(q, lambda e: e.dma_start(out=out_ap, in_=in_ap), reads=[buf], sembuf=buf)

    PB = [k.psum("pb%d" % i, [128, 512], F32) for i in range(8)]

    def pbf(i):
        return PB[i][:].bitcast(BF16)

    ONES32 = k.sbuf("ones32", [128, 128], F32)
    ID32 = k.sbuf("id32", [128, 128], F32)
    IDB = k.sbuf("idb", [128, 128], BF16)
    ONESB = k.sbuf("onesb", [128, 128], BF16)
    U32 = k.sbuf("u32", [128, 128], F32)
    L32 = k.sbuf("l32", [128, 128], F32)
    SUB = k.sbuf("sub", [128, 128], BF16)
    MASK4 = k.sbuf("mask4", [128, 2, 4, 128], BF16)
    MEAN32 = k.sbuf("mean32", [128, 128], F32)
    EPSC = k.sbuf("epsc", [128, 1], F32)
    MHALF = k.sbuf("mhalf", [128, 128], F32)
    XINIT = k.sbuf("xinit", [128, 4], F32)
    PIDX = k.sbuf("pidx", [128, 1], F32)
    PIDXI = k.sbuf("pidxi", [128, 1], I32)
    TMPC = k.sbuf("tmpc", [128, 128], F32)

    G(lambda e: e.memset(ONES32[:], 1.0), w=[ONES32])
    G(lambda e: e.memset(EPSC[:], EPS), w=[EPSC])
    G(lambda e: e.memset(MHALF[:], -0.5), w=[MHALF])
    G(lambda e: e.memset(MEAN32[:], 1.0 / 512.0), w=[MEAN32])
    G(lambda e: e.affine_select(out=ID32[:], in_=ONES32[:], pattern=[[-1, 128]], compare_op=ALU.is_equal, fill=0.0, base=0, channel_multiplier=1), r=[ONES32], w=[ID32])
    G(lambda e: e.affine_select(out=U32[:], in_=ONES32[:], pattern=[[1, 128]], compare_op=ALU.is_ge, fill=0.0, base=0, channel_multiplier=-1), r=[ONES32], w=[U32])
    G(lambda e: e.affine_select(out=L32[:], in_=ONES32[:], pattern=[[-1, 128]], compare_op=ALU.is_ge, fill=0.0, base=0, channel_multiplier=1), r=[ONES32], w=[L32])
    G(lambda e: e.affine_select(out=TMPC[:], in_=ONES32[:], pattern=[[1, 128]], compare_op=ALU.is_gt, fill=0.0, base=0, channel_multiplier=-1), r=[ONES32], w=[TMPC])
    V(lambda e: e.tensor_copy(out=SUB[:], in_=TMPC[:]), r=[TMPC], w=[SUB])
    V(lambda e: e.tensor_copy(out=IDB[:], in_=ID32[:]), r=[ID32], w=[IDB])
    V(lambda e: e.tensor_copy(out=ONESB[:], in_=ONES32[:]), r=[ONES32], w=[ONESB])
    for h in range(4):
        V(lambda e: e.tensor_copy(out=MASK4[:, 0, h, :], in_=U32[:]), r=[U32], w=[MASK4])
        V(lambda e: e.tensor_copy(out=MASK4[:, 1, h, :], in_=L32[:]), r=[L32], w=[MASK4])
    G(lambda e: e.iota(PIDXI[:], pattern=[[0, 1]], base=0, channel_multiplier=1), w=[PIDXI])
    V(lambda e: e.tensor_copy(out=PIDX[:], in_=PIDXI[:]), r=[PIDXI], w=[PIDX])

    k.dma("sp", lambda e: e.dma_start(out=XA[0:CTX, :], in_=ctx_d[:, :]), sembuf=XINIT)
    for i in range(4):
        k.dma("sp", lambda e: e.dma_start(out=XA[CTX + i * 1024:CTX + (i + 1) * 1024, :], in_=x_d[i * 1024:(i + 1) * 1024, :]), sembuf=XINIT)

    CROW = k.sbuf("crow", [16, 128], F32)
    CSB = k.sbuf("csb", [128, 8, 2], BF16)
    LD(CROW[0:8, :], c_d.t.rearrange("(r p) -> r p", p=128), CROW)
    LD(CROW[8:16, :], cctx_d.t.rearrange("(r p) -> r p", p=128), CROW)
    P(lambda e: e.transpose(out=PB[0][:, 0:16], in_=CROW[:, :], identity=ID32[0:16, 0:16]), r=[CROW, ID32], w=[PB[0]])
    for j in range(2):
        A(lambda e: e.activation(out=CSB[:, :, j], in_=PB[0][:, j * 8:(j + 1) * 8], func=AF.Silu), r=[PB[0]], w=[CSB])

    FT = k.sbuf("ft", [128, 4, 8, 2], F32)
    GBR = k.sbuf("gbr", [128, 16], F32)

    def bcast_row(dst, src_ap):
        LD(dst[:], src_ap.partition_broadcast(128), dst)

    xs_cur, xs_oth = XA, XB

    for l in range(layers):
        need_ctx = l < DEPTH - 1
        last = l == DEPTH - 1
        with ExitStack() as es:
            es.enter_context(nc.named_scope('A%d' % l))
            AWR = Ring([k.sbuf("aw%d" % i, [128, 8, 512], BF16, es) for i in range(2)])
            MODROW = k.sbuf("modrow", [2, 6 * D], F32, es)
            WSROW = k.sbuf("wsrow", [2, 2, D], F32, es)
            NROWS = k.sbuf("nrows", [2, 2, D], F32, es)
            for g in range(12):
                aw = AWR.next()
                k.dma("pool", lambda e: e.dma_start(out=aw[:], in_=ada_w_d[l, :, g * 512:(g + 1) * 512].rearrange("(kc p) n -> p kc n", p=128)), writes=[aw], sembuf=aw)
                for kc in range(8):
                    P(lambda e: e.matmul(PB[0][0:2, 0:512], lhsT=CSB[:, kc, :], rhs=aw[:, kc, :], start=(kc == 0), stop=(kc == 7)), r=[CSB, aw], w=[PB[0]])
                V(lambda e: e.tensor_copy(out=MODROW[0:2, g * 512:(g + 1) * 512], in_=PB[0][0:2, 0:512]), r=[PB[0]], w=[MODROW])
            BROW = k.sbuf("brow", [2, 6 * D], F32, es)
            LD(BROW[:], ada_b_d[l, :].partition_broadcast(2), BROW)
            V(lambda e: e.tensor_tensor(out=MODROW[:], in0=MODROW[:], in1=BROW[:], op=ALU.add), r=[MODROW, BROW], w=[MODROW])
            LD(NROWS[:, 0, :], n1_d[l, :].partition_broadcast(2), NROWS)
            LD(NROWS[:, 1, :], n2_d[l, :].partition_broadcast(2), NROWS)
            for w_, sc_set in ((0, 1), (1, 4)):
                V(lambda e: e.tensor_scalar(out=WSROW[:, w_, :], in0=MODROW[:, sc_set * D:(sc_set + 1) * D], scalar1=1.0, scalar2=None, op0=ALU.add), r=[MODROW], w=[WSROW])
                V(lambda e: e.tensor_tensor(out=WSROW[:, w_, :], in0=WSROW[:, w_, :], in1=NROWS[:, w_, :], op=ALU.mult), r=[WSROW, NROWS], w=[WSROW])
            srcs = [(WSROW, lambda c: WSROW[0:2, 0, c * 128:(c + 1) * 128]), (MODROW, lambda c: MODROW[0:2, 0 * D + c * 128:0 * D + (c + 1) * 128]),
                    (WSROW, lambda c: WSROW[0:2, 1, c * 128:(c + 1) * 128]), (MODROW, lambda c: MODROW[0:2, 3 * D + c * 128:3 * D + (c + 1) * 128])]
            for s, (sb, fn) in enumerate(srcs):
                for c in range(8):
                    P(lambda e: e.transpose(out=PB[1][:, (s * 8 + c) * 2:(s * 8 + c) * 2 + 2], in_=fn(c), identity=ID32[0:2, 0:2]), r=[sb, ID32], w=[PB[1]])
            V(lambda e: e.tensor_copy(out=FT[:].rearrange("p s c j -> p (s c j)"), in_=PB[1][:, 0:64]), r=[PB[1]], w=[FT])
            LD(GBR[:], gb_d[l, :].partition_broadcast(128), GBR)
            ST(MODd[:, :], MODROW[:], MODROW)
            ST(WSd[:, :, :], WSROW[:], WSROW)
        k.barrier()

        tiles_all = list(range(NT))
        tiles_out = list(range(NT)) if need_ctx else list(range(2, NT))

        def rsqrt_pool(out_ap, in_ap, scale, w, rbufs, wbuf):
            G(lambda e: e.tensor_scalar(out=out_ap, in0=in_ap, scalar1=scale, scalar2=EPS, op0=ALU.mult, op1=ALU.add), r=rbufs, w=[wbuf])
            G(lambda e: e.tensor_tensor(out=out_ap, in0=out_ap, in1=MHALF[0:out_ap.shape[0], 0:w], op=ALU.pow), r=[wbuf, MHALF], w=[wbuf])

        def rms_to_xn(xt, xn, junk, ssq, rt):
            A(lambda e: e.activation(out=junk[:], in_=xt[:], func=AF.Square, accum_out=ssq[:]), r=[xt], w=[junk, ssq])
            rsqrt_pool(rt[:], ssq[:], 1.0 / D, 1, [ssq], rt)
            V(lambda e: e.tensor_scalar(out=xn[:], in0=xt[:], scalar1=rt[:, 0:1], scalar2=None, op0=ALU.mult), r=[xt, rt], w=[xn])

        def xn_to_hT(xn, hT, j, s_ws, s_sh, pbs=None):
            if pbs is None:
                pbs = (PB[0], PB[1])
            for hh in range(2):
                pb = pbs[hh]
                for q in range(4):
                    c = hh * 4 + q
                    P(lambda e: e.transpose(out=pb[:, q * 128:(q + 1) * 128], in_=xn[:, c * 128:(c + 1) * 128], identity=ID32[:]), r=[xn, ID32], w=[pb])
                for q in range(4):
                    c = hh * 4 + q
                    A(lambda e: e.activation(out=hT[:, c, :], in_=pb[:, q * 128:(q + 1) * 128], func=AF.Identity, bias=FT[:, s_sh, c, j:j + 1], scale=FT[:, s_ws, c, j:j + 1]), r=[pb, FT], w=[hT])

        for part in range(2):
          with ExitStack() as es:
            es.enter_context(nc.named_scope('B%d_%d' % (l, part)))
            wc0, wc1 = (0, NST) if part == 0 else (NST, N_IN)
            WIN = k.sbuf("win%d" % part, [128, 8, wc1 - wc0], BF16, es)
            cc = wc0
            while cc < wc1:
                ce = min(cc + 1024, wc1)
                k.dma("pool", lambda e: e.dma_start(out=WIN[:, :, cc - wc0:ce - wc0], in_=w_in_d[l, :, cc:ce].rearrange("(kc p) n -> p kc n", p=128)), writes=[WIN], sembuf=WIN)
                cc = ce
            junk = k.sbuf("bjunk", [128, D], BF16, es)
            btiles = tiles_all if part == 0 else tiles_out
            BW = []
            for s_ in range(2):
                W = {}
                W["pre"] = []
                for pp in range(2):
                    W["pre"].append({"xt": k.sbuf("bxt%d%d" % (s_, pp), [128, D], F32, es), "xn": k.sbuf("bxn%d%d" % (s_, pp), [128, D], F32, es),
                                     "ssq": k.sbuf("bssq%d%d" % (s_, pp), [128, 1], F32, es), "rt": k.sbuf("brt%d%d" % (s_, pp), [128, 1], F32, es),
                                     "hT": k.sbuf("bht%d%d" % (s_, pp), [128, 8, 128], BF16, es)})
                if part == 0:
                    W["qkv"] = k.sbuf("bqkv%d" % s_, [128, 3072], BF16, es)
                    W["GT"] = k.sbuf("bgt%d" % s_, [128, 16], F32, es)
                    W["SP"] = k.sbuf("bsp%d" % s_, [128, 8], F32, es)
                    W["TM"] = k.sbuf("btm%d" % s_, [128, 16], F32, es)
                    W["gg"] = k.sbuf("bgg%d" % s_, [128, 32], F32, es)
                else:
                    W["zr"] = k.sbuf("bzr%d" % s_, [128, ZR_W], BF16, es)
                    W["SG"] = k.sbuf("bsg%d" % s_, [128, 512], F32, es)
                    W["SIG"] = k.sbuf("bsig%d" % s_, [128, 512], F32, es)
                W["PR"] = Ring([PB[2 + 2 * s_], PB[3 + 2 * s_]] + ([PB[6 + s_]] if part == 1 else []))
                W["pt"] = PB[s_]
                W["pg"] = PB[6 + s_]
                BW.append(W)

            def b_prep1(ti, W, pp):
                pr = W["pre"][pp]
                LD(pr["xt"][:], xs_cur[ti * 128:(ti + 1) * 128, :], pr["xt"])
                rms_to_xn(pr["xt"], pr["xn"], junk, pr["ssq"], pr["rt"])

            def b_prep2(ti, W, pp):
                pr = W["pre"][pp]
                xn_to_hT(pr["xn"], pr["hT"], 1 if ti < 2 else 0, 0, 1, pbs=(W["pt"], W["pt"]))

            def b_tile(ti, W, pp, nxt_ti):
                hT, PR, pg = W["pre"][pp]["hT"], W["PR"], W["pg"]
                gi = 0
                if part == 0:
                    qkv, GT, SP_, TM, gg = W["qkv"], W["GT"], W["SP"], W["TM"], W["gg"]
                else:
                    zr, SG_, SIG = W["zr"], W["SG"], W["SIG"]
                for g in (range(7) if part == 0 else range(7, 19)):
                    c0 = g * 512
                    c1 = c0 + 512
                    if g == 6:
                        c1 = NST
                    if g >= 7:
                        c0 = NST + (g - 7) * 512
                        c1 = c0 + 512
                    w = c1 - c0
                    ps = PR.next()
                    for kc in range(8):
                        P(lambda e: e.matmul(ps[:, 0:w], lhsT=hT[:, kc, :], rhs=WIN[:, kc, c0 - wc0:c1 - wc0], start=(kc == 0), stop=(kc == 7)), r=[hT, WIN], w=[ps])
                    if g < 6:
                        sc = 0.0625 if g in (2, 3) else 1.0
                        if g % 2 == 0:
                            A(lambda e: e.activation(out=qkv[:, c0:c1], in_=ps[:, 0:512], func=AF.Copy, scale=sc), r=[ps], w=[qkv])
                        else:
                            V(lambda e: e.tensor_scalar(out=qkv[:, c0:c1], in0=ps[:, 0:512], scalar1=sc, scalar2=None, op0=ALU.mult), r=[ps], w=[qkv])
                    elif g == 6:
                        V(lambda e: e.tensor_tensor(out=GT[:], in0=ps[:, 0:16], in1=GBR[:], op=ALU.add), r=[ps, GBR], w=[GT])
                    else:
                        r0 = (g - 7) * 512
                        if r0 < 1024:
                            A(lambda e: e.activation(out=zr[:, ZR_OG + r0:ZR_OG + r0 + 512], in_=ps[:, 0:512], func=AF.Sigmoid), r=[ps], w=[zr])
                        elif r0 == 1024:
                            V(lambda e: e.tensor_copy(out=SG_[:], in_=ps[:, 0:512]), r=[ps], w=[SG_])
                        elif r0 == 1536:
                            A(lambda e: e.activation(out=SIG[:], in_=ps[:, 0:512], func=AF.Sigmoid), r=[ps], w=[SIG])
                            V(lambda e: e.tensor_tensor(out=zr[:, ZR_U:ZR_U + 512], in0=SG_[:], in1=SIG[:], op=ALU.mult), r=[SG_, SIG], w=[zr])
                        elif r0 < 3072:
                            o0 = ZR_ZS + (r0 - 2048)
                            V(lambda e: e.tensor_tensor(out=SG_[:], in0=ps[:, 0:512], in1=ps[:, 0:512], op=ALU.mult), r=[ps], w=[SG_]) if False else None
                            A(lambda e: e.activation(out=SG_[:], in_=ps[:, 0:512], func=AF.Square), r=[ps], w=[SG_])
                            V(lambda e: e.tensor_scalar(out=SG_[:], in0=SG_[:], scalar1=0.044715, scalar2=1.0, op0=ALU.mult, op1=ALU.add), r=[SG_], w=[SG_])
                            V(lambda e: e.tensor_tensor(out=SG_[:], in0=SG_[:], in1=ps[:, 0:512], op=ALU.mult), r=[SG_, ps], w=[SG_])
                            A(lambda e: e.activation(out=SIG[:], in_=SG_[:], func=AF.Sigmoid, scale=1.5957691216057308), r=[SG_], w=[SIG])
                            V(lambda e: e.tensor_tensor(out=zr[:, o0:o0 + 512], in0=SIG[:], in1=ps[:, 0:512], op=ALU.mult), r=[SIG, ps], w=[zr])
                        else:
                            o0 = ZR_GM + (r0 - 3072)
                            A(lambda e: e.activation(out=zr[:, o0:o0 + 512], in_=ps[:, 0:512], func=AF.Sigmoid), r=[ps], w=[zr])
                    gi += 1
                    if nxt_ti is not None and gi == 1:
                        b_prep1(nxt_ti, W, 1 - pp)
                    if nxt_ti is not None and gi == (4 if part == 0 else 7):
                        b_prep2(nxt_ti, W, 1 - pp)
                    yield
                if part == 1:
                    ST(ZR[ti, :, :], zr[:], zr)
                    yield
                    return
                for dd in range(2):
                    A(lambda e: e.activation(out=SP_[:, dd * 4:(dd + 1) * 4], in_=GT[:, dd * 8 + 4:dd * 8 + 8], func=AF.Exp, scale=-1.0), r=[GT], w=[SP_])
                yield
                A(lambda e: e.activation(out=SP_[:], in_=SP_[:], func=AF.Ln, bias=1.0, scale=1.0), r=[SP_], w=[SP_])
                yield
                P(lambda e: e.matmul(pg[:, 0:4], lhsT=U32[:], rhs=SP_[:, 0:4], start=True, stop=True), r=[U32, SP_], w=[pg])
                P(lambda e: e.matmul(pg[:, 4:8], lhsT=L32[:], rhs=SP_[:, 4:8], start=True, stop=True), r=[L32, SP_], w=[pg])
                P(lambda e: e.matmul(pg[:, 8:16], lhsT=ONES32[:], rhs=SP_[:, 0:8], start=True, stop=True), r=[ONES32, SP_], w=[pg])
                yield
                A(lambda e: e.activation(out=gg[:, 0:8], in_=pg[:, 0:8], func=AF.Exp, scale=-1.0), r=[pg], w=[gg])
                for dd in range(2):
                    V(lambda e: e.tensor_tensor(out=TM[:, dd * 4:(dd + 1) * 4], in0=GT[:, dd * 8:dd * 8 + 4], in1=pg[:, dd * 4:(dd + 1) * 4], op=ALU.add), r=[GT, pg], w=[TM])
                yield
                A(lambda e: e.activation(out=gg[:, 8:16], in_=TM[:, 0:8], func=AF.Exp), r=[TM], w=[gg])
                V(lambda e: e.tensor_tensor(out=TM[:, 8:16], in0=TM[:, 0:8], in1=pg[:, 8:16], op=ALU.subtract), r=[TM, pg], w=[TM])
                yield
                A(lambda e: e.activation(out=gg[:, 16:24], in_=TM[:, 8:16], func=AF.Exp), r=[TM], w=[gg])
                A(lambda e: e.activation(out=gg[:, 24:32], in_=pg[:, 8:16], func=AF.Exp, scale=-1.0), r=[pg], w=[gg])
                yield
                ST(ZQ[ti, :, :], qkv[:], qkv)
                ST(GGd[ti, :, :], gg[:], gg)
                yield

            def b_stream(s_):
                mine = btiles[s_::2]
                b_prep1(mine[0], BW[s_], 0)
                b_prep2(mine[0], BW[s_], 0)
                yield
                for n_, ti in enumerate(mine):
                    yield from b_tile(ti, BW[s_], n_ % 2, mine[n_ + 1] if n_ + 1 < len(mine) else None)

            interleave([b_stream(0), b_stream(1)], skew=[0, 9 if part == 0 else 8])
          k.barrier()
        if upto == "B" and l == layers - 1:
            break

        with ExitStack() as es:
            es.enter_context(nc.named_scope('C%d' % l))
            Cst = []
            for dd in range(2):
                c32 = k.sbuf("c32_%d" % dd, [128, 2, 4, 257], F32, es)
                cbf = k.sbuf("cbf_%d" % dd, [128, 2, 4, 257], BF16, es)
                G(lambda e: e.memset(c32[:], 0.0), w=[c32])
                G(lambda e: e.memset(cbf[:], 0.0), w=[cbf])
                Cst.append((c32, cbf))
            order = [tiles_all, [1, 0] + list(range(NT - 1, 1, -1))]
            SW = []
            for dd in range(2):
                W = {"q": k.sbuf("sq%d" % dd, [128, D], BF16, es), "kk": k.sbuf("sk%d" % dd, [128, D], BF16, es),
                     "qs": k.sbuf("sqs%d" % dd, [128, D], BF16, es), "ks": k.sbuf("sks%d" % dd, [128, D], BF16, es),
                     "kst": k.sbuf("skst%d" % dd, [128, 8, 128], BF16, es), "dn": k.sbuf("sdn%d" % dd, [128, 8], F32, es),
                     "ho": [k.sbuf("sho%d_%d" % (dd, i), [128, D], F32, es) for i in range(2)], "par": []}
                for pp in range(2):
                    va = k.sbuf("sv%d_%d" % (dd, pp), [128, 4, 257], BF16, es)
                    G(lambda e: e.memset(va[:], 1.0), w=[va])
                    W["par"].append({"va": va, "gg": k.sbuf("sg%d_%d" % (dd, pp), [128, 32], F32, es),
                                     "kss": k.sbuf("skss%d_%d" % (dd, pp), [128, D], BF16, es),
                                     "qst": k.sbuf("sqst%d_%d" % (dd, pp), [128, 8, 128], BF16, es),
                                     "stm": k.sbuf("sst%d_%d" % (dd, pp), [128, 4, 128], BF16, es)})
                W["T"] = PB[dd]
                W["S"] = PB[dd]
                W["PN"] = Ring([PB[3 + dd], PB[2] if dd == 0 else PB[7]])
                W["PU"] = PB[5 + dd]
                SW.append(W)

            def scan_stage1(dd, ti, pp):
                W = SW[dd]
                P_ = W["par"][pp]
                q, kk, qs, ks, kst = W["q"], W["kk"], W["qs"], W["ks"], W["kst"]
                va, gg, kss, qst, stm = P_["va"], P_["gg"], P_["kss"], P_["qst"], P_["stm"]
                T, S = W["T"], W["S"]
                Tb = T[:].bitcast(BF16)
                LD(gg[:], GGd[ti, :, :], gg)
                LD(q[:], ZQ[ti, :, 0:1024], q)
                LD(kk[:], ZQ[ti, :, 1024:2048], kk)
                LD(va[:, :, 0:256], ZQ[ti, :, 2048:3072].rearrange("p (h d) -> p h d", h=4), va)
                yield
                for h in range(4):
                    hs = slice(h * 256, (h + 1) * 256)
                    A(lambda e: e.activation(out=qs[:, hs], in_=q[:, hs], func=AF.Identity, scale=gg[:, dd * 4 + h:dd * 4 + h + 1]), r=[q, gg], w=[qs])
                    V(lambda e: e.tensor_scalar(out=ks[:, hs], in0=kk[:, hs], scalar1=gg[:, 8 + dd * 4 + h:8 + dd * 4 + h + 1], scalar2=None, op0=ALU.mult), r=[kk, gg], w=[ks])
                yield
                for c in range(8):
                    P(lambda e: e.transpose(out=Tb[:, c * 128:(c + 1) * 128], in_=qs[:, c * 128:(c + 1) * 128], identity=IDB[:]), r=[qs, IDB], w=[T])
                A(lambda e: e.activation(out=qst[:].rearrange("p c t -> p (c t)"), in_=Tb[:, 0:1024], func=AF.Copy), r=[T], w=[qst])
                yield
                for h in range(4):
                    hs = slice(h * 256, (h + 1) * 256)
                    eng_ = "act" if h % 2 == 0 else "dve"
                    if eng_ == "act":
                        A(lambda e: e.activation(out=kss[:, hs], in_=kk[:, hs], func=AF.Identity, scale=gg[:, 16 + dd * 4 + h:16 + dd * 4 + h + 1]), r=[kk, gg], w=[kss])
                    else:
                        V(lambda e: e.tensor_scalar(out=kss[:, hs], in0=kk[:, hs], scalar1=gg[:, 16 + dd * 4 + h:16 + dd * 4 + h + 1], scalar2=None, op0=ALU.mult), r=[kk, gg], w=[kss])
                yield
                for c in range(8):
                    P(lambda e: e.transpose(out=Tb[:, c * 128:(c + 1) * 128], in_=ks[:, c * 128:(c + 1) * 128], identity=IDB[:]), r=[ks, IDB], w=[T])
                V(lambda e: e.tensor_copy(out=kst[:].rearrange("p c t -> p (c t)"), in_=Tb[:, 0:1024]), r=[T], w=[kst])
                yield
                for h in range(4):
                    for jj in range(2):
                        P(lambda e: e.matmul(S[:, h * 128:(h + 1) * 128], lhsT=kst[:, 2 * h + jj, :], rhs=qst[:, 2 * h + jj, :], start=(jj == 0), stop=(jj == 1)), r=[kst, qst], w=[S])
                V(lambda e: e.tensor_tensor(out=stm[:].rearrange("p h t -> p (h t)"), in0=S[:, 0:512], in1=MASK4[:, dd, :, :].rearrange("p h t -> p (h t)"), op=ALU.mult), r=[S, MASK4], w=[stm])
                yield

            def scan_stage2(dd, ti, pp, n_):
                W = SW[dd]
                P_ = W["par"][pp]
                va, gg, kss, qst, stm = P_["va"], P_["gg"], P_["kss"], P_["qst"], P_["stm"]
                c32, cbf = Cst[dd]
                dn, pu = W["dn"], W["PU"]
                ho = W["ho"][n_ % 2]
                for h in range(4):
                    for jj in range(2):
                        P(lambda e: e.matmul(pu[:, 0:257], lhsT=kss[:, h * 256 + jj * 128:h * 256 + (jj + 1) * 128], rhs=va[:, h, :], start=True, stop=True), r=[kss, va], w=[pu])
                        V(lambda e: e.scalar_tensor_tensor(out=c32[:, jj, h, :], in0=c32[:, jj, h, :], scalar=gg[:, 24 + dd * 4 + h:24 + dd * 4 + h + 1], in1=pu[:, 0:257], op0=ALU.mult, op1=ALU.add), r=[c32, gg, pu], w=[c32])
                    pn = W["PN"].next()
                    P(lambda e: e.matmul(pn[:, 0:257], lhsT=stm[:, h, :], rhs=va[:, h, :], start=True, stop=False), r=[stm, va], w=[pn])
                    for jj in range(2):
                        P(lambda e: e.matmul(pn[:, 0:257], lhsT=qst[:, 2 * h + jj, :], rhs=cbf[:, jj, h, :], start=False, stop=(jj == 1)), r=[qst, cbf], w=[pn])
                    V(lambda e: e.tensor_scalar(out=dn[:, 4 + h:5 + h], in0=pn[:, 256:257], scalar1=-1.0, scalar2=1.0, op0=ALU.mult, op1=ALU.max), r=[pn], w=[dn])
                    V(lambda e: e.tensor_tensor(out=dn[:, h:h + 1], in0=dn[:, 4 + h:5 + h], in1=pn[:, 256:257], op=ALU.max), r=[dn, pn], w=[dn])
                    V(lambda e: e.reciprocal(out=dn[:, h:h + 1], in_=dn[:, h:h + 1]), r=[dn], w=[dn])
                    A(lambda e: e.activation(out=ho[:, h * 256:(h + 1) * 256], in_=pn[:, 0:256], func=AF.Identity, scale=dn[:, h:h + 1]), r=[pn, dn], w=[ho])
                    yield
                A(lambda e: e.activation(out=cbf[:, 0, :, :], in_=c32[:, 0, :, :], func=AF.Copy), r=[c32], w=[cbf])
                V(lambda e: e.tensor_copy(out=cbf[:, 1, :, :], in_=c32[:, 1, :, :]), r=[c32], w=[cbf])
                if ti in tiles_out:
                    ST(HD[dd][ti, :, :], ho[:], ho)
                yield

            def scan_stream(dd):
                od = order[dd]
                yield from scan_stage1(dd, od[0], 0)
                for n_, ti in enumerate(od):
                    if n_ + 1 < len(od):
                        yield from scan_stage1(dd, od[n_ + 1], (n_ + 1) % 2)
                    yield from scan_stage2(dd, ti, n_ % 2, n_)

            interleave([scan_stream(0), scan_stream(1)], skew=[0, 3])
        k.barrier()
        if upto == "C" and l == layers - 1:
            break

        with ExitStack() as es:
            es.enter_context(nc.named_scope('E%d' % l))
            WMO = k.sbuf("wmo", [128, 8, D], BF16, es)
            WCO = k.sbuf("wco", [128, 4, D], BF16, es)
            WSO = k.sbuf("wso", [128, 4, D], BF16, es)
            WO = k.sbuf("wo", [128, 8, D], BF16, es)
            for (wb, wd_) in ((WMO, wmo_d), (WCO, wco_d), (WSO, wso_d), (WO, wo_d)):
                k.dma("pool", lambda e: e.dma_start(out=wb[:], in_=wd_[l, :, :].rearrange("(kc p) n -> p kc n", p=128)), writes=[wb], sembuf=wb)
            ROWS = k.sbuf("erows", [64, 128], F32, es)
            CW = k.sbuf("ecw", [128, 4, 32], F32, es)
            SM = k.sbuf("esm", [128, 16], F32, es)
            for c in range(4):
                LD(ROWS[0:31, :], cdw_d[l, :, c * 128:(c + 1) * 128], ROWS)
                P(lambda e: e.transpose(out=PB[0][:, 0:31], in_=ROWS[0:31, :], identity=ID32[0:31, 0:31]), r=[ROWS, ID32], w=[PB[0]])
                V(lambda e: e.tensor_copy(out=CW[:, c, 0:31], in_=PB[0][:, 0:31]), r=[PB[0]], w=[CW])
            LD(ROWS[0:4, :], cdb_d[l, :].rearrange("(r p) -> r p", p=128), ROWS)
            LD(ROWS[4:8, :], clw_d[l, :].rearrange("(r p) -> r p", p=128), ROWS)
            LD(ROWS[8:12, :], clb_d[l, :].rearrange("(r p) -> r p", p=128), ROWS)
            LD(ROWS[12:16, :], sgb_d[l, :, :], ROWS)
            P(lambda e: e.transpose(out=PB[0][:, 0:16], in_=ROWS[0:16, :], identity=ID32[0:16, 0:16]), r=[ROWS, ID32], w=[PB[0]])
            V(lambda e: e.tensor_copy(out=SM[:], in_=PB[0][:, 0:16]), r=[PB[0]], w=[SM])
            SGT = k.sbuf("esgt", [128, 4, 128], BF16, es)
            SGL = k.sbuf("esgl", [128, 128], F32, es)
            for g in range(4):
                LD(SGL[:], sgw_d[l, g, :, :], SGL)
                P(lambda e: e.transpose(out=PB[0][:, 0:128], in_=SGL[:], identity=ID32[:]), r=[SGL, ID32], w=[PB[0]])
                V(lambda e: e.tensor_copy(out=SGT[:, g, :], in_=PB[0][:, 0:128]), r=[PB[0]], w=[SGT])
            MNW = k.sbuf("emnw", [128, D], F32, es)
            SLW = k.sbuf("eslw", [128, 512], F32, es)
            SLB = k.sbuf("eslb", [128, 512], F32, es)
            LD(MNW[:], mnw_d[l, :].partition_broadcast(128), MNW)
            LD(SLW[:], slw_d[l, :].partition_broadcast(128), SLW)
            LD(SLB[:], slb_d[l, :].partition_broadcast(128), SLB)
            G1R = [k.sbuf("eg1r%d" % j, [128, D], F32, es) for j in range(2 if need_ctx else 1)]
            for j in range(len(G1R)):
                bcast_row(G1R[j], MODd[j, 2 * D:3 * D])

            DIAG = k.sbuf("ediag", [128, 4, 31, 128], BF16, es)
            for c in range(4):
                for jt in range(31):
                    if (c * 31 + jt) % 2 == 0:
                        V(lambda e: e.tensor_scalar(out=DIAG[:, c, jt, :], in0=ID32[:], scalar1=CW[:, c, jt:jt + 1], scalar2=None, op0=ALU.mult), r=[ID32, CW], w=[DIAG])
                    else:
                        A(lambda e: e.activation(out=DIAG[:, c, jt, :], in_=ID32[:], func=AF.Identity, scale=CW[:, c, jt:jt + 1]), r=[ID32, CW], w=[DIAG])
            UPC = k.sbuf("eupc", [128, 4, 286], BF16, es)
            G(lambda e: e.memset(UPC[:], 0.0), w=[UPC])
            UB = k.sbuf("eub", [128, 512], BF16, es)
            junk = k.sbuf("ejunk", [128, D], BF16, es)
            TMP = k.sbuf("etmp", [128, 512], F32, es)
            WS = []
            for s_ in range(2):
                W = {}
                W["zr"] = k.sbuf("ezr%d" % s_, [128, ZR_W], BF16, es)
                W["hf"] = k.sbuf("ehf%d" % s_, [128, D], F32, es)
                W["xt"] = k.sbuf("ext%d" % s_, [128, D], F32, es)
                W["T1"] = k.sbuf("et1%d" % s_, [128, D], BF16, es)
                W["SS4"] = k.sbuf("ess4%d" % s_, [128, 4], F32, es)
                W["YN"] = k.sbuf("eyn%d" % s_, [128, D], BF16, es)
                W["YNT"] = k.sbuf("eynt%d" % s_, [128, 8, 128], BF16, es)
                W["MG"] = k.sbuf("emg%d" % s_, [128, D], F32, es)
                W["hb"] = W["MG"]
                W["UPL"] = k.sbuf("eupl%d" % s_, [128, 4, 2, 94], BF16, es)
                G(lambda e: e.memset(W["UPL"][:], 0.0), w=[W["UPL"]])
                W["CV"] = k.sbuf("ecv%d" % s_, [128, 4, 128], F32, es)
                W["CSQ"] = k.sbuf("ecsq%d" % s_, [128, 4, 128], F32, es)
                W["M2"] = k.sbuf("em2%d" % s_, [128, 128], F32, es)
                W["RSTD"] = k.sbuf("erstd%d" % s_, [128, 128], F32, es)
                W["CA"] = k.sbuf("eca%d" % s_, [128, 4, 128], BF16, es)
                W["ST2"] = k.sbuf("est2%d" % s_, [128, 4], F32, es)
                W["VNf"] = k.sbuf("evnf%d" % s_, [128, 512], F32, es)
                W["VNb"] = k.sbuf("evnb%d" % s_, [128, 512], BF16, es)
                W["SGO"] = k.sbuf("esgo%d" % s_, [128, 512], BF16, es)
                W["SGOT"] = k.sbuf("esgot%d" % s_, [128, 4, 128], BF16, es)
                W["MGB"] = k.sbuf("emgb%d" % s_, [128, D], BF16, es)
                W["MGT"] = k.sbuf("emgt%d" % s_, [128, 8, 128], BF16, es)
                W["pb"] = [PB[s_ * 4 + i] for i in range(4)]
                W["PY"] = Ring([PB[s_ * 4 + 2], PB[s_ * 4 + 3]])
                WS.append(W)

            if need_ctx:
                for ti in range(2):
                    LD(UB[:], ZR[ti, :, ZR_U:ZR_U + 512], UB)
                    for c in range(4):
                        P(lambda e: e.transpose(out=pbf(0)[:, c * 128:(c + 1) * 128], in_=UB[:, c * 128:(c + 1) * 128], identity=IDB[:]), r=[UB, IDB], w=[PB[0]])
                    V(lambda e: e.tensor_copy(out=UPC[:, :, 15 + ti * 128:15 + (ti + 1) * 128], in_=pbf(0)[:, 0:512].rearrange("p (c t) -> p c t", c=4)), r=[PB[0]], w=[UPC])

            def e_tile(ti, W):
                zr, hf, hb, xt = W["zr"], W["hf"], W["hb"], W["xt"]
                T1, SS4, YN, YNT, MG, UPL, CV, CSQ = W["T1"], W["SS4"], W["YN"], W["YNT"], W["MG"], W["UPL"], W["CV"], W["CSQ"]
                M2, RSTD, CA, ST2, VNf, VNb, SGO, SGOT, MGB, MGT = W["M2"], W["RSTD"], W["CA"], W["ST2"], W["VNf"], W["VNb"], W["SGO"], W["SGOT"], W["MGB"], W["MGT"]
                pT, pS = W["pb"][0], W["pb"][1]
                pTb = pT[:].bitcast(BF16)
                PY = W["PY"]
                j = 1 if ti < 2 else 0
                LD(zr[:], ZR[ti, :, :], zr)
                LD(hf[:], HD[0][ti, :, :], hf)
                LD(hb[:], HD[1][ti, :, :], hb)
                LD(xt[:], xs_cur[ti * 128:(ti + 1) * 128, :], xt)
                yield

                def gate_acc(py, half, goff, first, final):
                    gsl = zr[:, ZR_GM + goff + half * 512:ZR_GM + goff + (half + 1) * 512]
                    hs = slice(half * 512, (half + 1) * 512)
                    if first:
                        V(lambda e: e.tensor_tensor(out=MG[:, hs], in0=py[:, 0:512], in1=gsl, op=ALU.mult), r=[py, zr], w=[MG])
                    else:
                        V(lambda e: e.tensor_tensor(out=TMP[:], in0=py[:, 0:512], in1=gsl, op=ALU.mult), r=[py, zr], w=[TMP])
                        if final:
                            V(lambda e: e.tensor_tensor(out=MGB[:, hs], in0=MG[:, hs], in1=TMP[:], op=ALU.add), r=[MG, TMP], w=[MGB])
                        else:
                            V(lambda e: e.tensor_tensor(out=MG[:, hs], in0=MG[:, hs], in1=TMP[:], op=ALU.add), r=[MG, TMP], w=[MG])

                if ti >= 2:
                    for c in range(4):
                        P(lambda e: e.transpose(out=pTb[:, c * 128:(c + 1) * 128], in_=zr[:, ZR_U + c * 128:ZR_U + (c + 1) * 128], identity=IDB[:]), r=[zr, IDB], w=[pT])
                    for c in range(4):
                        A(lambda e: e.activation(out=UPL[:, c, :, 15:79], in_=pTb[:, c * 128:(c + 1) * 128].rearrange("p (r w) -> p r w", r=2), func=AF.Copy), r=[pT], w=[UPL])

                    def win(c, jt):
                        return UPL[:, c, :, jt:jt + 64]
                    ubuf = UPL
                else:
                    def win(c, jt):
                        return UPC[:, c, ti * 128 + jt:ti * 128 + jt + 128]
                    ubuf = UPC
                yield
                V(lambda e: e.tensor_tensor(out=hf[:], in0=hf[:], in1=hb[:], op=ALU.add), r=[hf, hb], w=[hf])
                for h in range(4):
                    A(lambda e: e.activation(out=junk[:, h * 256:(h + 1) * 256], in_=hf[:, h * 256:(h + 1) * 256], func=AF.Square, accum_out=SS4[:, h:h + 1]), r=[hf], w=[junk, SS4])
                G(lambda e: e.tensor_tensor(out=T1[:], in0=zr[:, ZR_OG:ZR_OG + 1024], in1=MNW[:], op=ALU.mult), r=[zr, MNW], w=[T1])
                yield
                for c in range(4):
                    for jt in range(31):
                        P(lambda e: e.matmul(pS[:, c * 128:(c + 1) * 128], lhsT=DIAG[:, c, jt, :], rhs=win(c, jt), start=(jt == 0), stop=(jt == 30)), r=[DIAG, ubuf], w=[pS])
                    if c % 2 == 1:
                        yield
                A(lambda e: e.activation(out=SS4[:], in_=SS4[:], func=AF.Sqrt, bias=EPSC[:], scale=1.0 / 256.0), r=[SS4, EPSC], w=[SS4])
                V(lambda e: e.reciprocal(out=SS4[:], in_=SS4[:]), r=[SS4], w=[SS4])
                for h in range(4):
                    hs = slice(h * 256, (h + 1) * 256)
                    V(lambda e: e.scalar_tensor_tensor(out=YN[:, hs], in0=hf[:, hs], scalar=SS4[:, h:h + 1], in1=T1[:, hs], op0=ALU.mult, op1=ALU.mult), r=[hf, SS4, T1], w=[YN])
                yield
                for c in range(4):
                    A(lambda e: e.activation(out=CV[:, c, :], in_=pS[:, c * 128:(c + 1) * 128], func=AF.Identity, bias=SM[:, c:c + 1], scale=1.0), r=[pS, SM], w=[CV])
                A(lambda e: e.activation(out=CSQ[:], in_=CV[:], func=AF.Square), r=[CV], w=[CSQ])
                yield
                for c in range(8):
                    P(lambda e: e.transpose(out=pTb[:, c * 128:(c + 1) * 128], in_=YN[:, c * 128:(c + 1) * 128], identity=IDB[:]), r=[YN, IDB], w=[pT])
                A(lambda e: e.activation(out=YNT[:].rearrange("p c t -> p (c t)"), in_=pTb[:, 0:1024], func=AF.Copy), r=[pT], w=[YNT])
                yield
                for c in range(4):
                    P(lambda e: e.matmul(pS[:, 0:128], lhsT=MEAN32[:], rhs=CV[:, c, :], start=(c == 0), stop=(c == 3)), r=[MEAN32, CV], w=[pS])
                for c in range(4):
                    P(lambda e: e.matmul(pS[:, 128:256], lhsT=MEAN32[:], rhs=CSQ[:, c, :], start=(c == 0), stop=(c == 3)), r=[MEAN32, CSQ], w=[pS])
                yield
                for half in range(2):
                    py = PY.next()
                    for kc in range(8):
                        P(lambda e: e.matmul(py[:, 0:512], lhsT=YNT[:, kc, :], rhs=WMO[:, kc, half * 512:(half + 1) * 512], start=(kc == 0), stop=(kc == 7)), r=[YNT, WMO], w=[py])
                    gate_acc(py, half, 0, True, False)
                    yield
                A(lambda e: e.activation(out=M2[:], in_=pS[:, 0:128], func=AF.Square), r=[pS], w=[M2])
                V(lambda e: e.tensor_tensor(out=RSTD[:], in0=pS[:, 128:256], in1=M2[:], op=ALU.subtract), r=[pS, M2], w=[RSTD])
                A(lambda e: e.activation(out=RSTD[:], in_=RSTD[:], func=AF.Sqrt, bias=EPSC[:], scale=1.0), r=[RSTD, EPSC], w=[RSTD])
                V(lambda e: e.reciprocal(out=RSTD[:], in_=RSTD[:]), r=[RSTD], w=[RSTD])
                V(lambda e: e.tensor_copy(out=M2[:], in_=pS[:, 0:128]), r=[pS], w=[M2])
                yield
                for c in range(4):
                    eng_ = "dve"
                    k.op(eng_, lambda e: e.tensor_tensor(out=CSQ[:, c, :], in0=CV[:, c, :], in1=M2[:], op=ALU.subtract), [CV, M2], [CSQ])
                    k.op(eng_, lambda e: e.tensor_tensor(out=CSQ[:, c, :], in0=CSQ[:, c, :], in1=RSTD[:], op=ALU.mult), [CSQ, RSTD], [CSQ])
                yield
                for c in range(4):
                    A(lambda e: e.activation(out=CA[:, c, :], in_=CSQ[:, c, :], func=AF.Silu, bias=SM[:, 8 + c:9 + c], scale=SM[:, 4 + c:5 + c]), r=[CSQ, SM], w=[CA])
                yield
                vv = zr[:, ZR_ZS + 512:ZR_ZS + 1024]
                A(lambda e: e.activation(out=junk[:, 0:512], in_=vv, func=AF.Identity, accum_out=ST2[:, 0:1]), r=[zr], w=[junk, ST2])
                A(lambda e: e.activation(out=junk[:, 512:1024], in_=vv, func=AF.Square, accum_out=ST2[:, 1:2]), r=[zr], w=[junk, ST2])
                yield
                for half in range(2):
                    py = PY.next()
                    for c in range(4):
                        P(lambda e: e.matmul(py[:, 0:512], lhsT=CA[:, c, :], rhs=WCO[:, c, half * 512:(half + 1) * 512], start=(c == 0), stop=(c == 3)), r=[CA, WCO], w=[py])
                    gate_acc(py, half, 1024, False, False)
                yield
                V(lambda e: e.tensor_scalar(out=ST2[:, 0:2], in0=ST2[:, 0:2], scalar1=1.0 / 512.0, scalar2=None, op0=ALU.mult), r=[ST2], w=[ST2])
                V(lambda e: e.tensor_tensor(out=ST2[:, 2:3], in0=ST2[:, 0:1], in1=ST2[:, 0:1], op=ALU.mult), r=[ST2], w=[ST2])
                yield
                V(lambda e: e.tensor_tensor(out=ST2[:, 2:3], in0=ST2[:, 1:2], in1=ST2[:, 2:3], op=ALU.subtract), r=[ST2], w=[ST2])
                A(lambda e: e.activation(out=ST2[:, 2:3], in_=ST2[:, 2:3], func=AF.Sqrt, bias=EPSC[:], scale=1.0), r=[ST2, EPSC], w=[ST2])
                yield
                V(lambda e: e.reciprocal(out=ST2[:, 2:3], in_=ST2[:, 2:3]), r=[ST2], w=[ST2])
                yield
                V(lambda e: e.tensor_scalar(out=VNf[:], in0=vv, scalar1=ST2[:, 0:1], scalar2=ST2[:, 2:3], op0=ALU.subtract, op1=ALU.mult), r=[zr, ST2], w=[VNf])
                yield
                V(lambda e: e.tensor_tensor(out=VNf[:], in0=VNf[:], in1=SLW[:], op=ALU.mult), r=[VNf, SLW], w=[VNf])
                yield
                V(lambda e: e.tensor_tensor(out=VNb[:], in0=VNf[:], in1=SLB[:], op=ALU.add), r=[VNf, SLB], w=[VNb])
                yield
                for g in range(4):
                    P(lambda e: e.matmul(pS[:, g * 128:(g + 1) * 128], lhsT=SGT[:, g, :], rhs=VNb[:, g * 128:(g + 1) * 128], start=True, stop=True), r=[SGT, VNb], w=[pS])
                yield
                for g in range(4):
                    V(lambda e: e.scalar_tensor_tensor(out=SGO[:, g * 128:(g + 1) * 128], in0=pS[:, g * 128:(g + 1) * 128], scalar=SM[:, 12 + g:13 + g], in1=zr[:, ZR_ZS + g * 128:ZR_ZS + (g + 1) * 128], op0=ALU.add, op1=ALU.mult), r=[pS, SM, zr], w=[SGO])
                yield
                for c in range(4):
                    P(lambda e: e.transpose(out=pTb[:, c * 128:(c + 1) * 128], in_=SGO[:, c * 128:(c + 1) * 128], identity=IDB[:]), r=[SGO, IDB], w=[pT])
                A(lambda e: e.activation(out=SGOT[:].rearrange("p c t -> p (c t)"), in_=pTb[:, 0:512], func=AF.Copy), r=[pT], w=[SGOT])
                yield
                for half in range(2):
                    py = PY.next()
                    for c in range(4):
                        P(lambda e: e.matmul(py[:, 0:512], lhsT=SGOT[:, c, :], rhs=WSO[:, c, half * 512:(half + 1) * 512], start=(c == 0), stop=(c == 3)), r=[SGOT, WSO], w=[py])
                    gate_acc(py, half, 2048, False, True)
                yield
                for c in range(8):
                    P(lambda e: e.transpose(out=pTb[:, c * 128:(c + 1) * 128], in_=MGB[:, c * 128:(c + 1) * 128], identity=IDB[:]), r=[MGB, IDB], w=[pT])
                A(lambda e: e.activation(out=MGT[:].rearrange("p c t -> p (c t)"), in_=pTb[:, 0:1024], func=AF.Copy), r=[pT], w=[MGT])
                yield
                for half in range(2):
                    py = PY.next()
                    hs = slice(half * 512, (half + 1) * 512)
                    for kc in range(8):
                        P(lambda e: e.matmul(py[:, 0:512], lhsT=MGT[:, kc, :], rhs=WO[:, kc, hs], start=(kc == 0), stop=(kc == 7)), r=[MGT, WO], w=[py])
                    V(lambda e: e.tensor_tensor(out=MG[:, hs], in0=py[:, 0:512], in1=G1R[j][:, hs], op=ALU.mult), r=[py, G1R[j]], w=[MG])
                    yield
                    G(lambda e: e.tensor_tensor(out=xt[:, hs], in0=MG[:, hs], in1=xt[:, hs], op=ALU.add), r=[MG, xt], w=[xt])
                    yield
                ST(xs_oth[ti * 128:(ti + 1) * 128, :], xt[:], xt)
                yield

            def e_stream(s_):
                for ti in tiles_out[s_::2]:
                    yield from e_tile(ti, WS[s_])

            interleave([e_stream(0), e_stream(1)], skew=[0, 18])
        k.barrier()
        if upto == "E" and l == layers - 1:
            break

        sets = [("lat", list(range(2, NT)), 512)]
        if need_ctx:
            sets.append(("ctx", [0, 1], 32))
        with ExitStack() as es:
            ZERO = k.sbuf("zero", [128, 1024], F32, es)
            IOTAF = k.sbuf("iotaf", [128, 512], F32, es)
            IOTAI = k.sbuf("iotai", [128, 512], I32, es)
            G(lambda e: e.memset(ZERO[:], 0.0), w=[ZERO])
            G(lambda e: e.iota(IOTAI[:], pattern=[[1, 512]], base=0, channel_multiplier=0), w=[IOTAI])
            V(lambda e: e.tensor_copy(out=IOTAF[:], in_=IOTAI[:]), r=[IOTAI], w=[IOTAF])
            RW = k.sbuf("frw", [128, 8, NE], F32, es)
            LD(RW[:], rw_d[l, :, :].rearrange("(kc p) n -> p kc n", p=128), RW)
            RBR = k.sbuf("frbr", [128, NE], F32, es)
            LD(RBR[:], rb_d[l, :].partition_broadcast(128), RBR)
            AFF = k.sbuf("faff", [128, NT, NE], F32, es)
            W2R = [k.sbuf("fw2r%d" % j, [128, D], F32, es) for j in range(len(sets))]
            S2R = [k.sbuf("fs2r%d" % j, [128, D], F32, es) for j in range(len(sets))]
            for j in range(len(sets)):
                bcast_row(W2R[j], WSd[j, 1, :])
                bcast_row(S2R[j], MODd[j, 3 * D:4 * D])

            with ExitStack() as es1:
                es1.enter_context(nc.named_scope('F1_%d' % l))
                junk = k.sbuf("fjunk", [128, D], BF16, es1)
                FW = []
                for s_ in range(2):
                    FW.append({"xt": k.sbuf("fxt%d" % s_, [128, D], F32, es1), "xn": k.sbuf("fxn%d" % s_, [128, D], F32, es1),
                               "tmp": k.sbuf("ftmp%d" % s_, [128, D], F32, es1),
                               "ssq": k.sbuf("fssq%d" % s_, [128, 1], F32, es1), "rt": k.sbuf("frt%d" % s_, [128, 1], F32, es1),
                               "h2T": k.sbuf("fh2t%d" % s_, [128, 8, 128], F32, es1), "h2r": k.sbuf("fh2r%d" % s_, [128, D], BF16, es1),
                               "LG": k.sbuf("flg%d" % s_, [128, NE], F32, es1), "MX": k.sbuf("fmx%d" % s_, [128, 2], F32, es1),
                               "pt": PB[s_], "pr": PB[2 + s_]})

                def f1_tile(ti, W):
                    xt, xn, tmp, ssq, rt, h2T, h2r, LG, MX, pr = W["xt"], W["xn"], W["tmp"], W["ssq"], W["rt"], W["h2T"], W["h2r"], W["LG"], W["MX"], W["pr"]
                    j = 1 if ti < 2 else 0
                    LD(xt[:], xs_oth[ti * 128:(ti + 1) * 128, :], xt)
                    k.dma("pool", lambda e: e.dma_start(out=MACC[ti * 128:(ti + 1) * 128, :], in_=ZERO[:]), reads=[ZERO], sembuf=ZERO)
                    yield
                    rms_to_xn(xt, xn, junk, ssq, rt)
                    yield
                    xn_to_hT(xn, h2T, j, 2, 3, pbs=(W["pt"], W["pt"]))
                    yield
                    V(lambda e: e.tensor_tensor(out=tmp[:], in0=xn[:], in1=W2R[j][:], op=ALU.mult), r=[xn, W2R[j]], w=[tmp])
                    yield
                    V(lambda e: e.tensor_tensor(out=h2r[:], in0=tmp[:], in1=S2R[j][:], op=ALU.add), r=[tmp, S2R[j]], w=[h2r])
                    ST(H2d[ti * 128:(ti + 1) * 128, :], h2r[:], h2r)
                    yield
                    for kc in range(8):
                        P(lambda e: e.matmul(pr[:, 0:NE], lhsT=h2T[:, kc, :], rhs=RW[:, kc, :], start=(kc == 0), stop=(kc == 7)), r=[h2T, RW], w=[pr])
                    yield
                    V(lambda e: e.tensor_tensor(out=LG[:], in0=pr[:, 0:NE], in1=RBR[:], op=ALU.add), r=[pr, RBR], w=[LG])
                    yield
                    V(lambda e: e.reduce_max(out=MX[:, 0:1], in_=LG[:], axis=mybir.AxisListType.X), r=[LG], w=[MX])
                    yield
                    V(lambda e: e.tensor_scalar(out=MX[:, 0:1], in0=MX[:, 0:1], scalar1=-1.0, scalar2=None, op0=ALU.mult), r=[MX], w=[MX])
                    yield
                    A(lambda e: e.activation(out=LG[:], in_=LG[:], func=AF.Exp, bias=MX[:, 0:1], scale=1.0, accum_out=MX[:, 1:2]), r=[LG, MX], w=[LG, MX])
                    yield
                    V(lambda e: e.reciprocal(out=MX[:, 1:2], in_=MX[:, 1:2]), r=[MX], w=[MX])
                    yield
                    V(lambda e: e.tensor_scalar(out=AFF[:, ti, :], in0=LG[:], scalar1=MX[:, 1:2], scalar2=None, op0=ALU.mult), r=[LG, MX], w=[AFF])
                    yield

                def f1_stream(s_):
                    for ti in tiles_out[s_::2]:
                        yield from f1_tile(ti, FW[s_])

                interleave([f1_stream(0), f1_stream(1)], skew=[0, 6])
            k.barrier()

            TOKI = k.sbuf("ftoki", [128, NE, 5], I32, es)
            GS = k.sbuf("fgs", [128, NE, 5], F32, es)
            with ExitStack() as es2:
                es2.enter_context(nc.named_scope('F3_%d' % l))
                AFFT = k.sbuf("fafft", [16, SEQ], F32, es2)
                JK = k.sbuf("fjk", [16, SEQ], F32, es2)
                BS = k.sbuf("fbs", [16, 8], F32, es2)
                DG = k.sbuf("fdg", [16, 16], F32, es2)
                THRB = k.sbuf("fthrb", [128, NE], F32, es2)
                MK = k.sbuf("fmk", [128, 32, NE], F32, es2)
                MKB = k.sbuf("fmkb", [128, 32, NE], BF16, es2)
                OFFS = k.sbuf("foffs", [128, 32, NE], F32, es2)
                POS = k.sbuf("fpos", [128, 32, NE], F32, es2)
                RH = k.sbuf("frh", [128, 32, NE, 5], BF16, es2)
                R1 = k.sbuf("fr1", [128, 32, NE], F32, es2)
                R2 = k.sbuf("fr2", [128, 32, NE], F32, es2)
                OH = Ring([k.sbuf("foh%d" % i, [128, 512], BF16, es2) for i in range(6)])
                TF = k.sbuf("ftf", [128, 8], F32, es2)
                for si, (sname, stiles, cap) in enumerate(sets):
                    nt = len(stiles)
                    ntok = nt * 128
                    t0 = stiles[0]
                    nst = (cap + 127) // 128
                    for q in range(nt):
                        P(lambda e: e.transpose(out=PB[0][0:16, (q % 4) * 128:(q % 4 + 1) * 128], in_=AFF[:, t0 + q, :], identity=ID32[:]), r=[AFF, ID32], w=[PB[0]])
                        if q % 4 == 3 or q == nt - 1:
                            q0 = (q // 4) * 4
                            wq = (q - q0 + 1) * 128
                            V(lambda e: e.tensor_copy(out=AFFT[:, q0 * 128:q0 * 128 + wq], in_=PB[0][0:16, 0:wq]), r=[PB[0]], w=[AFFT])
                    V(lambda e: e.memset(BS[:, 0:1], 0.0), w=[BS])
                    V(lambda e: e.memset(BS[:, 1:2], 1.0), w=[BS])
                    for it in range(26):
                        V(lambda e: e.tensor_scalar(out=BS[:, 2:3], in0=BS[:, 0:1], scalar1=BS[:, 1:2], scalar2=0.5, op0=ALU.add, op1=ALU.mult), r=[BS], w=[BS])
                        V(lambda e: e.tensor_scalar(out=JK[:, 0:ntok], in0=AFFT[:, 0:ntok], scalar1=BS[:, 2:3], scalar2=0.0, op0=ALU.is_ge, op1=ALU.add, accum_out=BS[:, 3:4]), r=[AFFT, BS], w=[JK, BS])
                        V(lambda e: e.tensor_scalar(out=BS[:, 4:5], in0=BS[:, 3:4], scalar1=float(cap) - 0.5, scalar2=None, op0=ALU.is_ge), r=[BS], w=[BS])
                        V(lambda e: e.tensor_tensor(out=BS[:, 5:6], in0=BS[:, 2:3], in1=BS[:, 0:1], op=ALU.subtract), r=[BS], w=[BS])
                        V(lambda e: e.tensor_tensor(out=BS[:, 6:7], in0=BS[:, 1:2], in1=BS[:, 2:3], op=ALU.subtract), r=[BS], w=[BS])
                        V(lambda e: e.scalar_tensor_tensor(out=BS[:, 0:1], in0=BS[:, 5:6], scalar=BS[:, 4:5], in1=BS[:, 0:1], op0=ALU.mult, op1=ALU.add), r=[BS], w=[BS])
                        V(lambda e: e.scalar_tensor_tensor(out=BS[:, 1:2], in0=BS[:, 6:7], scalar=BS[:, 4:5], in1=BS[:, 2:3], op0=ALU.mult, op1=ALU.add), r=[BS], w=[BS])
                    V(lambda e: e.tensor_scalar(out=DG[:], in0=ID32[0:16, 0:16], scalar1=BS[:, 0:1], scalar2=None, op0=ALU.mult), r=[ID32, BS], w=[DG])
                    P(lambda e: e.matmul(PB[1][:, 0:NE], lhsT=ONES32[0:16, :], rhs=DG[:], start=True, stop=True), r=[ONES32, DG], w=[PB[1]])
                    V(lambda e: e.tensor_copy(out=THRB[:], in_=PB[1][:, 0:NE]), r=[PB[1]], w=[THRB])
                    for q in range(nt):
                        V(lambda e: e.tensor_tensor(out=MK[:, q, :], in0=AFF[:, t0 + q, :], in1=THRB[:], op=ALU.is_ge), r=[AFF, THRB], w=[MK])
                    ncol = nt * NE
                    mkf = MK[:, 0:nt, :].rearrange("p t e -> p (t e)")
                    V(lambda e: e.tensor_copy(out=MKB[:, 0:nt, :].rearrange("p t e -> p (t e)"), in_=mkf), r=[MK], w=[MKB])
                    P(lambda e: e.matmul(PB[2][:, 0:ncol], lhsT=SUB[:], rhs=MKB[:, 0:nt, :].rearrange("p t e -> p (t e)"), start=True, stop=True), r=[SUB, MKB], w=[PB[2]])
                    P(lambda e: e.matmul(PB[3][:, 0:ncol], lhsT=ONESB[:], rhs=MKB[:, 0:nt, :].rearrange("p t e -> p (t e)"), start=True, stop=True), r=[ONESB, MKB], w=[PB[3]])
                    V(lambda e: e.memset(OFFS[:, 0, :], 0.0), w=[OFFS])
                    for q in range(1, nt):
                        V(lambda e: e.tensor_tensor(out=OFFS[:, q, :], in0=OFFS[:, q - 1, :], in1=PB[3][:, (q - 1) * NE:q * NE], op=ALU.add), r=[OFFS, PB[3]], w=[OFFS])
                    posf = POS[:, 0:nt, :].rearrange("p t e -> p (t e)")
                    V(lambda e: e.tensor_tensor(out=posf, in0=PB[2][:, 0:ncol], in1=OFFS[:, 0:nt, :].rearrange("p t e -> p (t e)"), op=ALU.add), r=[PB[2], OFFS], w=[POS])
                    V(lambda e: e.tensor_scalar(out=R1[:, 0:nt, :].rearrange("p t e -> p (t e)"), in0=posf, scalar1=float(cap) - 0.5, scalar2=None, op0=ALU.is_lt), r=[POS], w=[R1])
                    V(lambda e: e.tensor_tensor(out=mkf, in0=mkf, in1=R1[:, 0:nt, :].rearrange("p t e -> p (t e)"), op=ALU.mult), r=[MK, R1], w=[MK])
                    for q in range(nt):
                        V(lambda e: e.memset(RH[:, q, :, 0], float((t0 + q) * 128)), w=[RH])
                        V(lambda e: e.tensor_scalar(out=RH[:, q, :, 1], in0=ONES32[:, 0:NE], scalar1=PIDX[:, 0:1], scalar2=None, op0=ALU.mult), r=[ONES32, PIDX], w=[RH])
                    afs = AFF[:, t0:t0 + nt, :]
                    V(lambda e: e.tensor_copy(out=RH[:, 0:nt, :, 2], in_=afs), r=[AFF], w=[RH])
                    V(lambda e: e.tensor_tensor(out=R1[:, 0:nt, :], in0=afs, in1=RH[:, 0:nt, :, 2], op=ALU.subtract), r=[AFF, RH], w=[R1])
                    V(lambda e: e.tensor_copy(out=RH[:, 0:nt, :, 3], in_=R1[:, 0:nt, :]), r=[R1], w=[RH])
                    V(lambda e: e.tensor_tensor(out=R2[:, 0:nt, :], in0=R1[:, 0:nt, :], in1=RH[:, 0:nt, :, 3], op=ALU.subtract), r=[R1, RH], w=[R2])
                    V(lambda e: e.tensor_copy(out=RH[:, 0:nt, :, 4], in_=R2[:, 0:nt, :]), r=[R2], w=[RH])
                    PQ = [PB[4], PB[5], PB[6], PB[7]]
                    capw = nst * 128 if cap >= 128 else cap
                    for ex in range(NE):
                        for q in range(nt):
                            oh = OH.next()
                            k.op("dve", lambda e: e.tensor_scalar(out=oh[:, 0:capw], in0=IOTAF[:, 0:capw], scalar1=POS[:, q, ex:ex + 1], scalar2=MK[:, q, ex:ex + 1], op0=ALU.is_equal, op1=ALU.mult), [IOTAF, POS, MK], [oh])
                            for s_ in range(nst):
                                mw = min(128, cap - s_ * 128)
                                P(lambda e: e.matmul(PQ[s_][0:mw, 0:5], lhsT=oh[:, s_ * 128:s_ * 128 + mw], rhs=RH[:, q, ex, :], start=(q == 0), stop=(q == nt - 1)), r=[oh, RH], w=[PQ[s_]])
                        for s_ in range(nst):
                            mw = min(128, cap - s_ * 128)
                            sl = s_ if si == 0 else 4
                            V(lambda e: e.tensor_copy(out=TF[0:mw, 0:5], in_=PQ[s_][0:mw, 0:5]), r=[PQ[s_]], w=[TF])
                            V(lambda e: e.tensor_tensor(out=TF[0:mw, 5:6], in0=TF[0:mw, 0:1], in1=TF[0:mw, 1:2], op=ALU.add), r=[TF], w=[TF])
                            V(lambda e: e.tensor_copy(out=TOKI[0:mw, ex, sl:sl + 1], in_=TF[0:mw, 5:6]), r=[TF], w=[TOKI])
                            V(lambda e: e.tensor_tensor(out=TF[0:mw, 6:7], in0=TF[0:mw, 2:3], in1=TF[0:mw, 3:4], op=ALU.add), r=[TF], w=[TF])
                            V(lambda e: e.tensor_tensor(out=GS[0:mw, ex, sl:sl + 1], in0=TF[0:mw, 6:7], in1=TF[0:mw, 4:5], op=ALU.add), r=[TF], w=[GS])
            k.barrier()

            with ExitStack() as es3:
                es3.enter_context(nc.named_scope('F5_%d' % l))
                WPR = Ring([k.sbuf("fwp%d" % i, [128, 16, 512], BF16, es3) for i in range(3)])
                NS = 544 if need_ctx else 512
                nsl = 5 if need_ctx else 4
                XE2 = [[k.sbuf("fxe%d_%d" % (pp, i), [128, D], BF16, es3) for i in range(nsl)] for pp in range(2)]
                XET2 = [k.sbuf("fxet%d" % pp, [128, 8, NS], BF16, es3) for pp in range(2)]
                HMT = k.sbuf("fhmt", [128, 16, NS], BF16, es3)
                SA = Ring([k.sbuf("fsa%d" % i, [128, NS], F32, es3) for i in range(2)])
                YS = [k.sbuf("fys%d" % i, [128, D], F32, es3) for i in range(5 if need_ctx else 4)]
                slot_tiles = [(s_, 128, s_ * 128) for s_ in range(4)]
                if need_ctx:
                    slot_tiles.append((4, 32, 512))
                pieces = []
                for ex in range(NE):
                    for g in range(4):
                        pieces.append((ex, "g", g))
                    for hf_ in range(2):
                        pieces.append((ex, "d", hf_))
                loaded = {}

                def issue_piece(pi):
                    if pi >= len(pieces) or pi in loaded:
                        return
                    ex, kind, g = pieces[pi]
                    wp = WPR.next()
                    if kind == "g":
                        k.dma("pool", lambda e: e.dma_start(out=wp[:, 0:8, :], in_=wg_d[l, ex, :, g * 512:(g + 1) * 512].rearrange("(kc p) n -> p kc n", p=128)), writes=[wp], sembuf=wp)
                        k.dma("pool", lambda e: e.dma_start(out=wp[:, 8:16, :], in_=wu_d[l, ex, :, g * 512:(g + 1) * 512].rearrange("(kc p) n -> p kc n", p=128)), writes=[], reads=[], sembuf=wp)
                        wp.writes = {("d", wp.dsem): k.dcount[wp.dsem]}
                    else:
                        k.dma("pool", lambda e: e.dma_start(out=wp[:], in_=wd_d[l, ex, :, g * 512:(g + 1) * 512].rearrange("(fc p) n -> p fc n", p=128)), writes=[wp], sembuf=wp)
                    loaded[pi] = wp

                def f5_gather(ex):
                    for (s_, mw, off) in slot_tiles:
                        xe = XE2[ex % 2][s_]
                        k.dma("pool", lambda e: e.indirect_dma_start(out=xe[0:mw, :], out_offset=None, in_=H2d[:, :], in_offset=bass.IndirectOffsetOnAxis(ap=TOKI[0:mw, ex, s_:s_ + 1], axis=0)), reads=[TOKI], writes=[xe], sembuf=xe)

                def f5_xet(ex):
                    for (s_, mw, off) in slot_tiles:
                        xe = XE2[ex % 2][s_]
                        for c in range(8):
                            P(lambda e: e.transpose(out=pbf(6)[:, c * 128:c * 128 + mw], in_=xe[0:mw, c * 128:(c + 1) * 128], identity=IDB[0:mw, 0:mw]), r=[xe, IDB], w=[PB[6]])
                        V(lambda e: e.tensor_copy(out=XET2[ex % 2][:, :, off:off + mw], in_=pbf(6)[:, 0:1024].rearrange("p (c t) -> p c t", c=8)[:, :, 0:mw]), r=[PB[6]], w=[XET2[ex % 2]])

                PA = Ring([PB[0], PB[1]])
                PUu = Ring([PB[2], PB[3]])
                PYr = Ring([PB[4], PB[5]])
                issue_piece(0)
                issue_piece(1)
                pi = 0
                prev_tokens = {}
                for ex in range(NE):
                    XET = XET2[ex % 2]
                    f5_gather(ex)
                    f5_xet(ex)
                    for g in range(4):
                        wp = loaded.pop(pi)
                        issue_piece(pi + 2)
                        pi += 1
                        for f in range(4):
                            fc = g * 4 + f
                            pa = PA.next(); pu = PUu.next()
                            for (wsel, pp) in ((0, pa), (8, pu)):
                                for kc in range(8):
                                    P(lambda e: e.matmul(pp[:, 0:512], lhsT=wp[:, wsel + kc, f * 128:(f + 1) * 128], rhs=XET[:, kc, 0:512], start=(kc == 0), stop=(kc == 7)), r=[wp, XET], w=[pp])
                            sa = SA.next()
                            A(lambda e: e.activation(out=sa[:, 0:512], in_=pa[:, 0:512], func=AF.Silu), r=[pa], w=[sa])
                            V(lambda e: e.tensor_tensor(out=HMT[:, fc, 0:512], in0=sa[:, 0:512], in1=pu[:, 0:512], op=ALU.mult), r=[sa, pu], w=[HMT])
                            if need_ctx:
                                for (wsel, c0_) in ((0, 0), (8, 32)):
                                    for kc in range(8):
                                        P(lambda e: e.matmul(PB[7][:, c0_:c0_ + 32], lhsT=wp[:, wsel + kc, f * 128:(f + 1) * 128], rhs=XET[:, kc, 512:544], start=(kc == 0), stop=(kc == 7)), r=[wp, XET], w=[PB[7]])
                                A(lambda e: e.activation(out=sa[:, 512:544], in_=PB[7][:, 0:32], func=AF.Silu), r=[PB[7]], w=[sa])
                                V(lambda e: e.tensor_tensor(out=HMT[:, fc, 512:544], in0=sa[:, 512:544], in1=PB[7][:, 32:64], op=ALU.mult), r=[sa, PB[7]], w=[HMT])
                    wpd = []
                    wpd.append(loaded.pop(pi))
                    issue_piece(pi + 2)
                    pi += 1
                    wpd.append(loaded.pop(pi))
                    pi += 1
                    cur_tokens = {}
                    for (s_, mw, off) in slot_tiles:
                        for hf_ in range(2):
                            py = PYr.next()
                            for fc in range(16):
                                P(lambda e: e.matmul(py[0:mw, 0:512], lhsT=HMT[:, fc, off:off + mw], rhs=wpd[hf_][:, fc, :], start=(fc == 0), stop=(fc == 15)), r=[HMT, wpd[hf_]], w=[py])
                            A(lambda e: e.activation(out=YS[s_][0:mw, hf_ * 512:(hf_ + 1) * 512], in_=py[0:mw, 0:512], func=AF.Identity, scale=GS[0:mw, ex, s_:s_ + 1]), r=[py, GS], w=[YS[s_]])
                        MACC.writes = dict(prev_tokens)
                        MACC.readers = {}
                        k.dma("pool", lambda e: e.indirect_dma_start(out=MACC[:, :], out_offset=bass.IndirectOffsetOnAxis(ap=TOKI[0:mw, ex, s_:s_ + 1], axis=0), in_=YS[s_][0:mw, :], in_offset=None, compute_op=ALU.add), reads=[TOKI, YS[s_]], writes=[MACC], sembuf=YS[s_])
                        _merge(cur_tokens, MACC.writes)
                    prev_tokens = cur_tokens
                    MACC.writes = dict(prev_tokens)
                    issue_piece(pi + 1)
            k.barrier()

            with ExitStack() as es4:
                es4.enter_context(nc.named_scope('F6_%d' % l))
                G2R = [k.sbuf("fg2r%d" % j, [128, D], F32, es4) for j in range(len(sets))]
                for j in range(len(sets)):
                    bcast_row(G2R[j], MODd[j, 5 * D:6 * D])
                XL = Ring([k.sbuf("gxt%d" % i, [128, D], F32, es4) for i in range(3)])
                ML = Ring([k.sbuf("gml%d" % i, [128, D], F32, es4) for i in range(3)])
                XO = Ring([k.sbuf("gxo%d" % i, [128, D], F32, es4) for i in range(3)])
                junk = k.sbuf("gjunk", [128, D], BF16, es4)
                SSQR = Ring([k.sbuf("gssq%d" % i, [128, 1], F32, es4) for i in range(2)])
                RTR = Ring([k.sbuf("grt%d" % i, [128, 1], F32, es4) for i in range(2)])
                if last:
                    FNW = k.sbuf("gfnw", [128, D], F32, es4)
                    LD(FNW[:], fnw_d.t.partition_broadcast(128), FNW)
                for ti in tiles_out:
                    j = 1 if ti < 2 else 0
                    xt = XL.next(); ml = ML.next(); xo = XO.next()
                    LD(xt[:], xs_oth[ti * 128:(ti + 1) * 128, :], xt)
                    LD(ml[:], MACC[ti * 128:(ti + 1) * 128, :], ml)
                    V(lambda e: e.tensor_tensor(out=ml[:], in0=ml[:], in1=G2R[j][:], op=ALU.mult), r=[ml, G2R[j]], w=[ml])
                    if not last:
                        V(lambda e: e.tensor_tensor(out=xo[:], in0=ml[:], in1=xt[:], op=ALU.add), r=[ml, xt], w=[xo])
                        ST(xs_cur[ti * 128:(ti + 1) * 128, :], xo[:], xo)
                    else:
                        V(lambda e: e.tensor_tensor(out=xt[:], in0=ml[:], in1=xt[:], op=ALU.add), r=[ml, xt], w=[xt])
                        rms_to_xn(xt, ml, junk, SSQR.next(), RTR.next())
                        V(lambda e: e.tensor_tensor(out=xo[:], in0=ml[:], in1=FNW[:], op=ALU.mult), r=[ml, FNW], w=[xo])
                        ST(out_d[(ti - 2) * 128:(ti - 1) * 128, :], xo[:], xo)
        k.barrier()
    k.finish()
    return nc, k


_CACHE = {}


def kernel(**inputs):
    n = 8
    if "nc" not in _CACHE:
        _CACHE["nc"] = build_program()[0]
    nc = _CACHE["nc"]
    shared = {kk: np.ascontiguousarray(v, dtype=np.float32) for kk, v in inputs.items() if kk not in ("x", "c", "ctx")}
    in_maps = []
    for b in range(n):
        m = dict(shared)
        m["x"] = np.ascontiguousarray(inputs["x"][b], dtype=np.float32)
        m["c"] = np.ascontiguousarray(inputs["c"][b], dtype=np.float32)
        m["ctx"] = np.ascontiguousarray(inputs["ctx"][b], dtype=np.float32)
        in_maps.append(m)
    res = run_bass_kernel_spmd(nc, in_maps, core_ids=list(range(n)))
    return np.stack([np.asarray(r["out"], dtype=np.float32) for r in res.results], axis=0)
```

```python
import numpy as np
from contextlib import ExitStack
import concourse.bass as bass
import concourse.mybir as mybir
from concourse.bass_utils import run_bass_kernel_spmd

F32 = mybir.dt.float32
BF16 = mybir.dt.bfloat16
I32 = mybir.dt.int32
AF = mybir.ActivationFunctionType
ALU = mybir.AluOpType

D = 1024
SEQ = 4096
CTX = 256
DEPTH = 2
NT = 34
NROW = NT * 128
NE = 16
FF = 2048
N_IN = 9232
NST = 3088
EPS = 1e-6
ZR_OG, ZR_U, ZR_ZS, ZR_GM, ZR_W = 0, 1024, 1536, 2560, 5632


class Buf:
    __slots__ = ("t", "name", "writes", "readers", "dsem", "dcnt")

    def __init__(self, t, name):
        self.t = t
        self.name = name
        self.writes = {}
        self.readers = {}
        self.dsem = None
        self.dcnt = 0

    def __getitem__(self, key):
        return self.t[key]


def _merge(d, s):
    for k, v in s.items():
        if d.get(k, 0) < v:
            d[k] = v


class KB:
    def __init__(self, nc):
        self.nc = nc
        self.es = ExitStack()
        self.eng = {"pe": nc.tensor, "dve": nc.vector, "act": nc.scalar, "pool": nc.gpsimd, "sp": nc.sync}
        self.semh = {}
        self.cnt = {}
        self.seen = {k: {} for k in self.eng}
        for k in self.eng:
            self.semh[k] = self.es.enter_context(nc.semaphore("s_" + k))
            self.cnt[k] = 0
        self.dfree = []
        self.dcount = []
        self.nop = 0

    def sbuf(self, name, shape, dtype, es=None):
        self.uid = getattr(self, "uid", 0) + 1
        name = "%s_u%d" % (name, self.uid)
        t = (es or self.es).enter_context(self.nc.sbuf_tensor(name, list(shape), dtype))
        b = Buf(t, name)
        if es is not None:
            es.callback(self._release, b)
        return b

    def _release(self, b):
        if b.dsem is not None:
            self.dfree.append(b.dsem)
            b.dsem = None

    def _dsem_for(self, b):
        if b.dsem is None:
            if self.dfree:
                b.dsem = self.dfree.pop()
            else:
                idx = len(self.dcount)
                h = self.es.enter_context(self.nc.semaphore("dq%d" % idx))
                self.dcount.append(0)
                self.semh[("d", idx)] = h
                b.dsem = idx
        return b.dsem

    def psum(self, name, shape, dtype, es=None):
        t = (es or self.es).enter_context(self.nc.psum_tensor(name, list(shape), dtype))
        return Buf(t, name)

    def dram(self, name, shape, dtype, kind="Internal"):
        t = self.nc.dram_tensor(name, list(shape), dtype, kind=kind)
        return Buf(t.ap(), name)

    def _wait(self, e, need):
        eng = self.eng[e]
        seen = self.seen[e]
        for key, v in need.items():
            if seen.get(key, 0) < v:
                eng.wait_ge(self.semh[key], v)
                seen[key] = v

    def op(self, e, fn, reads=(), writes=()):
        raw = {}
        oth = {}
        for b in reads:
            _merge(raw, b.writes)
        for b in writes:
            _merge(oth, b.readers)
            _merge(oth, b.writes)
        need = dict(raw)
        for key, v in oth.items():
            if key == e:
                continue
            if need.get(key, 0) < v:
                need[key] = v
        if e == "pe":
            need.pop("pe", None)
        self._wait(e, need)
        ins = fn(self.eng[e])
        self.cnt[e] += 1
        ins.then_inc(self.semh[e], 1)
        tok = {e: self.cnt[e]}
        for b in reads:
            _merge(b.readers, tok)
        for b in writes:
            b.writes = dict(tok)
            b.readers = {}
        self.nop += 1
        return ins

    def dma(self, q, fn, reads=(), writes=(), sembuf=None):
        need = {}
        for b in reads:
            _merge(need, b.writes)
        for b in writes:
            _merge(need, b.readers)
            _merge(need, b.writes)
        self._wait(q, need)
        idx = self._dsem_for(sembuf)
        key = ("d", idx)
        ins = fn(self.eng[q])
        self.dcount[idx] += 16
        ins.then_inc(self.semh[key], 16)
        tok = {key: self.dcount[idx]}
        for b in reads:
            _merge(b.readers, tok)
        for b in writes:
            b.writes = dict(tok)
            b.readers = {}
        self.nop += 1
        return ins

    def barrier(self):
        need = {k: c for k, c in self.cnt.items() if c > 0}
        for idx, c in enumerate(self.dcount):
            if c > 0:
                need[("d", idx)] = c
        for e in self.eng:
            self._wait(e, dict(need))

    def finish(self):
        self.barrier()
        self.es.close()


class Ring:
    def __init__(self, bufs):
        self.bufs = bufs
        self.i = 0

    def next(self):
        b = self.bufs[self.i % len(self.bufs)]
        self.i += 1
        return b


def interleave(gens, skew=None):
    gens = list(gens)
    delay = {id(g): (skew[i] if skew else 0) for i, g in enumerate(gens)}
    while gens:
        for g in list(gens):
            if delay[id(g)] > 0:
                delay[id(g)] -= 1
                continue
            try:
                next(g)
            except StopIteration:
                gens.remove(g)


def build_program(layers=DEPTH, upto="all", dbg=False):
    nc = bass.Bass("TRN2", target_bir_lowering=False)
    k = KB(nc)
    ins_ = {}

    def din(name, shape):
        ins_[name] = k.dram(name, shape, F32, kind="ExternalInput")
        return ins_[name]

    x_d = din("x", [SEQ, D]); c_d = din("c", [D]); ctx_d = din("ctx", [CTX, D]); cctx_d = din("c_ctx", [D])
    ada_w_d = din("ada_w", [DEPTH, D, 6 * D]); ada_b_d = din("ada_b", [DEPTH, 6 * D])
    n1_d = din("norm1_w", [DEPTH, D]); n2_d = din("norm2_w", [DEPTH, D])
    w_in_d = din("w_in", [DEPTH, D, N_IN]); gb_d = din("mlstm_gate_b", [DEPTH, 16])
    mnw_d = din("mlstm_norm_w", [DEPTH, D]); wmo_d = din("w_mlstm_out", [DEPTH, D, D])
    cdw_d = din("conv_dw_w", [DEPTH, 31, 512]); cdb_d = din("conv_dw_b", [DEPTH, 512])
    clw_d = din("conv_ln_w", [DEPTH, 512]); clb_d = din("conv_ln_b", [DEPTH, 512])
    wco_d = din("w_conv_out", [DEPTH, 512, D])
    slw_d = din("sg_ln_w", [DEPTH, 512]); slb_d = din("sg_ln_b", [DEPTH, 512])
    sgw_d = din("sg_w", [DEPTH, 4, 128, 128]); sgb_d = din("sg_b", [DEPTH, 4, 128])
    wso_d = din("w_sg_out", [DEPTH, 512, D]); wo_d = din("w_o", [DEPTH, D, D])
    rw_d = din("router_w", [DEPTH, D, NE]); rb_d = din("router_b", [DEPTH, NE])
    wg_d = din("expert_w_gate", [DEPTH, NE, D, FF]); wu_d = din("expert_w_up", [DEPTH, NE, D, FF])
    wd_d = din("expert_w_down", [DEPTH, NE, FF, D]); fnw_d = din("final_norm_w", [D])
    out_d = k.dram("out", [SEQ, D], F32, kind="ExternalOutput")

    sk = "ExternalOutput" if dbg else "Internal"
    XA = k.dram("XA", [NROW, D], F32, kind=sk)
    XB = k.dram("XB", [NROW, D], F32, kind=sk)
    ZQ = k.dram("ZQ", [NT, 128, 3072], BF16, kind=sk)
    GGd = k.dram("GGd", [NT, 128, 32], F32, kind=sk)
    ZR = k.dram("ZR", [NT, 128, ZR_W], BF16, kind=sk)
    HD = [k.dram("HF", [NT, 128, D], F32, kind=sk), k.dram("HB", [NT, 128, D], F32, kind=sk)]
    H2d = k.dram("H2d", [NROW, D], BF16, kind=sk)
    MACC = k.dram("MACC", [NROW, D], F32, kind=sk)
    MODd = k.dram("MODd", [2, 6 * D], F32, kind=sk)
    WSd = k.dram("WSd", [2, 2, D], F32, kind=sk)
    dbg_d = {}

    def V(fn, r=(), w=()):
        return k.op("dve", fn, r, w)

    def A(fn, r=(), w=()):
        return k.op("act", fn, r, w)

    def P(fn, r=(), w=()):
        return k.op("pe", fn, r, w)

    def G(fn, r=(), w=()):
        return k.op("pool", fn, r, w)

    def LD(out_ap, in_ap, buf, q="sp"):
        return k.dma(q, lambda e: e.dma_start(out=out_ap, in_=in_ap), writes=[buf], sembuf=buf)

    def ST(out_ap, in_ap, buf, q="pool"):
        return k.dma(q, lambda e: e.dma_start(out=out_ap, in_=in_ap), reads=[buf], sembuf=buf)

    PB = [k.psum("pb%d" % i, [128, 512], F32) for i in range(8)]

    def pbf(i):
        return PB[i][:].bitcast(BF16)

    ONES32 = k.sbuf("ones32", [128, 128], F32)
    ID32 = k.sbuf("id32", [128, 128], F32)
    IDB = k.sbuf("idb", [128, 128], BF16)
    ONESB = k.sbuf("onesb", [128, 128], BF16)
    U32 = k.sbuf("u32", [128, 128], F32)
    L32 = k.sbuf("l32", [128, 128], F32)
    SUB = k.sbuf("sub", [128, 128], BF16)
    MASK4 = k.sbuf("mask4", [128, 2, 4, 128], BF16)
    MEAN32 = k.sbuf("mean32", [128, 128], F32)
    EPSC = k.sbuf("epsc", [128, 1], F32)
    MHALF = k.sbuf("mhalf", [128, 128], F32)
    XINIT = k.sbuf("xinit", [128, 4], F32)
    PIDX = k.sbuf("pidx", [128, 1], F32)
    PIDXI = k.sbuf("pidxi", [128, 1], I32)
    TMPC = k.sbuf("tmpc", [128, 128], F32)

    G(lambda e: e.memset(ONES32[:], 1.0), w=[ONES32])
    G(lambda e: e.memset(EPSC[:], EPS), w=[EPSC])
    G(lambda e: e.memset(MHALF[:], -0.5), w=[MHALF])
    G(lambda e: e.memset(MEAN32[:], 1.0 / 512.0), w=[MEAN32])
    G(lambda e: e.affine_select(out=ID32[:], in_=ONES32[:], pattern=[[-1, 128]], compare_op=ALU.is_equal, fill=0.0, base=0, channel_multiplier=1), r=[ONES32], w=[ID32])
    G(lambda e: e.affine_select(out=U32[:], in_=ONES32[:], pattern=[[1, 128]], compare_op=ALU.is_ge, fill=0.0, base=0, channel_multiplier=-1), r=[ONES32], w=[U32])
    G(lambda e: e.affine_select(out=L32[:], in_=ONES32[:], pattern=[[-1, 128]], compare_op=ALU.is_ge, fill=0.0, base=0, channel_multiplier=1), r=[ONES32], w=[L32])
    G(lambda e: e.affine_select(out=TMPC[:], in_=ONES32[:], pattern=[[1, 128]], compare_op=ALU.is_gt, fill=0.0, base=0, channel_multiplier=-1), r=[ONES32], w=[TMPC])
    V(lambda e: e.tensor_copy(out=SUB[:], in_=TMPC[:]), r=[TMPC], w=[SUB])
    V(lambda e: e.tensor_copy(out=IDB[:], in_=ID32[:]), r=[ID32], w=[IDB])
    V(lambda e: e.tensor_copy(out=ONESB[:], in_=ONES32[:]), r=[ONES32], w=[ONESB])
    for h in range(4):
        V(lambda e: e.tensor_copy(out=MASK4[:, 0, h, :], in_=U32[:]), r=[U32], w=[MASK4])
        V(lambda e: e.tensor_copy(out=MASK4[:, 1, h, :], in_=L32[:]), r=[L32], w=[MASK4])
    G(lambda e: e.iota(PIDXI[:], pattern=[[0, 1]], base=0, channel_multiplier=1), w=[PIDXI])
    V(lambda e: e.tensor_copy(out=PIDX[:], in_=PIDXI[:]), r=[PIDXI], w=[PIDX])

    k.dma("sp", lambda e: e.dma_start(out=XA[0:CTX, :], in_=ctx_d[:, :]), sembuf=XINIT)
    for i in range(4):
        k.dma("sp", lambda e: e.dma_start(out=XA[CTX + i * 1024:CTX + (i + 1) * 1024, :], in_=x_d[i * 1024:(i + 1) * 1024, :]), sembuf=XINIT)

    CROW = k.sbuf("crow", [16, 128], F32)
    CSB = k.sbuf("csb", [128, 8, 2], BF16)
    LD(CROW[0:8, :], c_d.t.rearrange("(r p) -> r p", p=128), CROW)
    LD(CROW[8:16, :], cctx_d.t.rearrange("(r p) -> r p", p=128), CROW)
    P(lambda e: e.transpose(out=PB[0][:, 0:16], in_=CROW[:, :], identity=ID32[0:16, 0:16]), r=[CROW, ID32], w=[PB[0]])
    for j in range(2):
        A(lambda e: e.activation(out=CSB[:, :, j], in_=PB[0][:, j * 8:(j + 1) * 8], func=AF.Silu), r=[PB[0]], w=[CSB])

    FT = k.sbuf("ft", [128, 4, 8, 2], F32)
    GBR = k.sbuf("gbr", [128, 16], F32)

    def bcast_row(dst, src_ap):
        LD(dst[:], src_ap.partition_broadcast(128), dst)

    xs_cur, xs_oth = XA, XB

    for l in range(layers):
        need_ctx = l < DEPTH - 1
        last = l == DEPTH - 1
        with ExitStack() as es:
            es.enter_context(nc.named_scope('A%d' % l))
            AWR = Ring([k.sbuf("aw%d" % i, [128, 8, 512], BF16, es) for i in range(2)])
            MODROW = k.sbuf("modrow", [2, 6 * D], F32, es)
            WSROW = k.sbuf("wsrow", [2, 2, D], F32, es)
            NROWS = k.sbuf("nrows", [2, 2, D], F32, es)
            for g in range(12):
                aw = AWR.next()
                k.dma("pool", lambda e: e.dma_start(out=aw[:], in_=ada_w_d[l, :, g * 512:(g + 1) * 512].rearrange("(kc p) n -> p kc n", p=128)), writes=[aw], sembuf=aw)
                for kc in range(8):
                    P(lambda e: e.matmul(PB[0][0:2, 0:512], lhsT=CSB[:, kc, :], rhs=aw[:, kc, :], start=(kc == 0), stop=(kc == 7)), r=[CSB, aw], w=[PB[0]])
                V(lambda e: e.tensor_copy(out=MODROW[0:2, g * 512:(g + 1) * 512], in_=PB[0][0:2, 0:512]), r=[PB[0]], w=[MODROW])
            BROW = k.sbuf("brow", [2, 6 * D], F32, es)
            LD(BROW[:], ada_b_d[l, :].partition_broadcast(2), BROW)
            V(lambda e: e.tensor_tensor(out=MODROW[:], in0=MODROW[:], in1=BROW[:], op=ALU.add), r=[MODROW, BROW], w=[MODROW])
            LD(NROWS[:, 0, :], n1_d[l, :].partition_broadcast(2), NROWS)
            LD(NROWS[:, 1, :], n2_d[l, :].partition_broadcast(2), NROWS)
            for w_, sc_set in ((0, 1), (1, 4)):
                V(lambda e: e.tensor_scalar(out=WSROW[:, w_, :], in0=MODROW[:, sc_set * D:(sc_set + 1) * D], scalar1=1.0, scalar2=None, op0=ALU.add), r=[MODROW], w=[WSROW])
                V(lambda e: e.tensor_tensor(out=WSROW[:, w_, :], in0=WSROW[:, w_, :], in1=NROWS[:, w_, :], op=ALU.mult), r=[WSROW, NROWS], w=[WSROW])
            srcs = [(WSROW, lambda c: WSROW[0:2, 0, c * 128:(c + 1) * 128]), (MODROW, lambda c: MODROW[0:2, 0 * D + c * 128:0 * D + (c + 1) * 128]),
                    (WSROW, lambda c: WSROW[0:2, 1, c * 128:(c + 1) * 128]), (MODROW, lambda c: MODROW[0:2, 3 * D + c * 128:3 * D + (c + 1) * 128])]
            for s, (sb, fn) in enumerate(srcs):
                for c in range(8):
                    P(lambda e: e.transpose(out=PB[1][:, (s * 8 + c) * 2:(s * 8 + c) * 2 + 2], in_=fn(c), identity=ID32[0:2, 0:2]), r=[sb, ID32], w=[PB[1]])
            V(lambda e: e.tensor_copy(out=FT[:].rearrange("p s c j -> p (s c j)"), in_=PB[1][:, 0:64]), r=[PB[1]], w=[FT])
            LD(GBR[:], gb_d[l, :].partition_broadcast(128), GBR)
            ST(MODd[:, :], MODROW[:], MODROW)
            ST(WSd[:, :, :], WSROW[:], WSROW)
        k.barrier()

        tiles_all = list(range(NT))
        tiles_out = list(range(NT)) if need_ctx else list(range(2, NT))

        def rsqrt_pool(out_ap, in_ap, scale, w, rbufs, wbuf):
            G(lambda e: e.tensor_scalar(out=out_ap, in0=in_ap, scalar1=scale, scalar2=EPS, op0=ALU.mult, op1=ALU.add), r=rbufs, w=[wbuf])
            G(lambda e: e.tensor_tensor(out=out_ap, in0=out_ap, in1=MHALF[0:out_ap.shape[0], 0:w], op=ALU.pow), r=[wbuf, MHALF], w=[wbuf])

        def rms_to_xn(xt, xn, junk, ssq, rt):
            A(lambda e: e.activation(out=junk[:], in_=xt[:], func=AF.Square, accum_out=ssq[:]), r=[xt], w=[junk, ssq])
            rsqrt_pool(rt[:], ssq[:], 1.0 / D, 1, [ssq], rt)
            V(lambda e: e.tensor_scalar(out=xn[:], in0=xt[:], scalar1=rt[:, 0:1], scalar2=None, op0=ALU.mult), r=[xt, rt], w=[xn])

        def xn_to_hT(xn, hT, j, s_ws, s_sh, pbs=None):
            if pbs is None:
                pbs = (PB[0], PB[1])
            for hh in range(2):
                pb = pbs[hh]
                for q in range(4):
                    c = hh * 4 + q
                    P(lambda e: e.transpose(out=pb[:, q * 128:(q + 1) * 128], in_=xn[:, c * 128:(c + 1) * 128], identity=ID32[:]), r=[xn, ID32], w=[pb])
                for q in range(4):
                    c = hh * 4 + q
                    A(lambda e: e.activation(out=hT[:, c, :], in_=pb[:, q * 128:(q + 1) * 128], func=AF.Identity, bias=FT[:, s_sh, c, j:j + 1], scale=FT[:, s_ws, c, j:j + 1]), r=[pb, FT], w=[hT])

        for part in range(2):
          with ExitStack() as es:
            es.enter_context(nc.named_scope('B%d_%d' % (l, part)))
            wc0, wc1 = (0, NST) if part == 0 else (NST, N_IN)
            WIN = k.sbuf("win%d" % part, [128, 8, wc1 - wc0], BF16, es)
            cc = wc0
            while cc < wc1:
                ce = min(cc + 1024, wc1)
                k.dma("pool", lambda e: e.dma_start(out=WIN[:, :, cc - wc0:ce - wc0], in_=w_in_d[l, :, cc:ce].rearrange("(kc p) n -> p kc n", p=128)), writes=[WIN], sembuf=WIN)
                cc = ce
            junk = k.sbuf("bjunk", [128, D], BF16, es)
            btiles = tiles_all if part == 0 else tiles_out
            BW = []
            for s_ in range(2):
                W = {}
                W["pre"] = []
                for pp in range(2):
                    W["pre"].append({"xt": k.sbuf("bxt%d%d" % (s_, pp), [128, D], F32, es), "xn": k.sbuf("bxn%d%d" % (s_, pp), [128, D], F32, es),
                                     "ssq": k.sbuf("bssq%d%d" % (s_, pp), [128, 1], F32, es), "rt": k.sbuf("brt%d%d" % (s_, pp), [128, 1], F32, es),
                                     "hT": k.sbuf("bht%d%d" % (s_, pp), [128, 8, 128], BF16, es)})
                if part == 0:
                    W["qkv"] = k.sbuf("bqkv%d" % s_, [128, 3072], BF16, es)
                    W["GT"] = k.sbuf("bgt%d" % s_, [128, 16], F32, es)
                    W["SP"] = k.sbuf("bsp%d" % s_, [128, 8], F32, es)
                    W["TM"] = k.sbuf("btm%d" % s_, [128, 16], F32, es)
                    W["gg"] = k.sbuf("bgg%d" % s_, [128, 32], F32, es)
                else:
                    W["zr"] = k.sbuf("bzr%d" % s_, [128, ZR_W], BF16, es)
                    W["SG"] = k.sbuf("bsg%d" % s_, [128, 512], F32, es)
                    W["SIG"] = k.sbuf("bsig%d" % s_, [128, 512], F32, es)
                W["PR"] = Ring([PB[2 + 2 * s_], PB[3 + 2 * s_]] + ([PB[6 + s_]] if part == 1 else []))
                W["pt"] = PB[s_]
                W["pg"] = PB[6 + s_]
                BW.append(W)

            def b_prep1(ti, W, pp):
                pr = W["pre"][pp]
                LD(pr["xt"][:], xs_cur[ti * 128:(ti + 1) * 128, :], pr["xt"])
                rms_to_xn(pr["xt"], pr["xn"], junk, pr["ssq"], pr["rt"])

            def b_prep2(ti, W, pp):
                pr = W["pre"][pp]
                xn_to_hT(pr["xn"], pr["hT"], 1 if ti < 2 else 0, 0, 1, pbs=(W["pt"], W["pt"]))

            def b_tile(ti, W, pp, nxt_ti):
                hT, PR, pg = W["pre"][pp]["hT"], W["PR"], W["pg"]
                gi = 0
                if part == 0:
                    qkv, GT, SP_, TM, gg = W["qkv"], W["GT"], W["SP"], W["TM"], W["gg"]
                else:
                    zr, SG_, SIG = W["zr"], W["SG"], W["SIG"]
                for g in (range(7) if part == 0 else range(7, 19)):
                    c0 = g * 512
                    c1 = c0 + 512
                    if g == 6:
                        c1 = NST
                    if g >= 7:
                        c0 = NST + (g - 7) * 512
                        c1 = c0 + 512
                    w = c1 - c0
                    ps = PR.next()
                    for kc in range(8):
                        P(lambda e: e.matmul(ps[:, 0:w], lhsT=hT[:, kc, :], rhs=WIN[:, kc, c0 - wc0:c1 - wc0], start=(kc == 0), stop=(kc == 7)), r=[hT, WIN], w=[ps])
                    if g < 6:
                        sc = 0.0625 if g in (2, 3) else 1.0
                        if g % 2 == 0:
                            A(lambda e: e.activation(out=qkv[:, c0:c1], in_=ps[:, 0:512], func=AF.Copy, scale=sc), r=[ps], w=[qkv])
                        else:
                            V(lambda e: e.tensor_scalar(out=qkv[:, c0:c1], in0=ps[:, 0:512], scalar1=sc, scalar2=None, op0=ALU.mult), r=[ps], w=[qkv])
                    elif g == 6:
                        V(lambda e: e.tensor_tensor(out=GT[:], in0=ps[:, 0:16], in1=GBR[:], op=ALU.add), r=[ps, GBR], w=[GT])
                    else:
                        r0 = (g - 7) * 512
                        if r0 < 1024:
                            A(lambda e: e.activation(out=zr[:, ZR_OG + r0:ZR_OG + r0 + 512], in_=ps[:, 0:512], func=AF.Sigmoid), r=[ps], w=[zr])
                        elif r0 == 1024:
                            V(lambda e: e.tensor_copy(out=SG_[:], in_=ps[:, 0:512]), r=[ps], w=[SG_])
                        elif r0 == 1536:
                            A(lambda e: e.activation(out=SIG[:], in_=ps[:, 0:512], func=AF.Sigmoid), r=[ps], w=[SIG])
                            V(lambda e: e.tensor_tensor(out=zr[:, ZR_U:ZR_U + 512], in0=SG_[:], in1=SIG[:], op=ALU.mult), r=[SG_, SIG], w=[zr])
                        elif r0 < 3072:
                            o0 = ZR_ZS + (r0 - 2048)
                            V(lambda e: e.tensor_tensor(out=SG_[:], in0=ps[:, 0:512], in1=ps[:, 0:512], op=ALU.mult), r=[ps], w=[SG_]) if False else None
                            A(lambda e: e.activation(out=SG_[:], in_=ps[:, 0:512], func=AF.Square), r=[ps], w=[SG_])
                            V(lambda e: e.tensor_scalar(out=SG_[:], in0=SG_[:], scalar1=0.044715, scalar2=1.0, op0=ALU.mult, op1=ALU.add), r=[SG_], w=[SG_])
                            V(lambda e: e.tensor_tensor(out=SG_[:], in0=SG_[:], in1=ps[:, 0:512], op=ALU.mult), r=[SG_, ps], w=[SG_])
                            A(lambda e: e.activation(out=SIG[:], in_=SG_[:], func=AF.Sigmoid, scale=1.5957691216057308), r=[SG_], w=[SIG])
                            V(lambda e: e.tensor_tensor(out=zr[:, o0:o0 + 512], in0=SIG[:], in1=ps[:, 0:512], op=ALU.mult), r=[SIG, ps], w=[zr])
                        else:
                            o0 = ZR_GM + (r0 - 3072)
                            A(lambda e: e.activation(out=zr[:, o0:o0 + 512], in_=ps[:, 0:512], func=AF.Sigmoid), r=[ps], w=[zr])
                    gi += 1
                    if nxt_ti is not None and gi == 1:
                        b_prep1(nxt_ti, W, 1 - pp)
                    if nxt_ti is not None and gi == (4 if part == 0 else 7):
                        b_prep2(nxt_ti, W, 1 - pp)
                    yield
                if part == 1:
                    ST(ZR[ti, :, :], zr[:], zr)
                    yield
                    return
                for dd in range(2):
                    A(lambda e: e.activation(out=SP_[:, dd * 4:(dd + 1) * 4], in_=GT[:, dd * 8 + 4:dd * 8 + 8], func=AF.Exp, scale=-1.0), r=[GT], w=[SP_])
                yield
                A(lambda e: e.activation(out=SP_[:], in_=SP_[:], func=AF.Ln, bias=1.0, scale=1.0), r=[SP_], w=[SP_])
                yield
                P(lambda e: e.matmul(pg[:, 0:4], lhsT=U32[:], rhs=SP_[:, 0:4], start=True, stop=True), r=[U32, SP_], w=[pg])
                P(lambda e: e.matmul(pg[:, 4:8], lhsT=L32[:], rhs=SP_[:, 4:8], start=True, stop=True), r=[L32, SP_], w=[pg])
                P(lambda e: e.matmul(pg[:, 8:16], lhsT=ONES32[:], rhs=SP_[:, 0:8], start=True, stop=True), r=[ONES32, SP_], w=[pg])
                yield
                A(lambda e: e.activation(out=gg[:, 0:8], in_=pg[:, 0:8], func=AF.Exp, scale=-1.0), r=[pg], w=[gg])
                for dd in range(2):
                    V(lambda e: e.tensor_tensor(out=TM[:, dd * 4:(dd + 1) * 4], in0=GT[:, dd * 8:dd * 8 + 4], in1=pg[:, dd * 4:(dd + 1) * 4], op=ALU.add), r=[GT, pg], w=[TM])
                yield
                A(lambda e: e.activation(out=gg[:, 8:16], in_=TM[:, 0:8], func=AF.Exp), r=[TM], w=[gg])
                V(lambda e: e.tensor_tensor(out=TM[:, 8:16], in0=TM[:, 0:8], in1=pg[:, 8:16], op=ALU.subtract), r=[TM, pg], w=[TM])
                yield
                A(lambda e: e.activation(out=gg[:, 16:24], in_=TM[:, 8:16], func=AF.Exp), r=[TM], w=[gg])
                A(lambda e: e.activation(out=gg[:, 24:32], in_=pg[:, 8:16], func=AF.Exp, scale=-1.0), r=[pg], w=[gg])
                yield
                ST(ZQ[ti, :, :], qkv[:], qkv)
                ST(GGd[ti, :, :], gg[:], gg)
                yield

            def b_stream(s_):
                mine = btiles[s_::2]
                b_prep1(mine[0], BW[s_], 0)
                b_prep2(mine[0], BW[s_], 0)
                yield
                for n_, ti in enumerate(mine):
                    yield from b_tile(ti, BW[s_], n_ % 2, mine[n_ + 1] if n_ + 1 < len(mine) else None)

            interleave([b_stream(0), b_stream(1)], skew=[0, 9 if part == 0 else 8])
          k.barrier()
        if upto == "B" and l == layers - 1:
            break

        with ExitStack() as es:
            es.enter_context(nc.named_scope('C%d' % l))
            Cst = []
            for dd in range(2):
                c32 = k.sbuf("c32_%d" % dd, [128, 2, 4, 257], F32, es)
                cbf = k.sbuf("cbf_%d" % dd, [128, 2, 4, 257], BF16, es)
                G(lambda e: e.memset(c32[:], 0.0), w=[c32])
                G(lambda e: e.memset(cbf[:], 0.0), w=[cbf])
                Cst.append((c32, cbf))
            order = [tiles_all, [1, 0] + list(range(NT - 1, 1, -1))]
            SW = []
            for dd in range(2):
                W = {"q": k.sbuf("sq%d" % dd, [128, D], BF16, es), "kk": k.sbuf("sk%d" % dd, [128, D], BF16, es),
                     "qs": k.sbuf("sqs%d" % dd, [128, D], BF16, es), "ks": k.sbuf("sks%d" % dd, [128, D], BF16, es),
                     "kst": k.sbuf("skst%d" % dd, [128, 8, 128], BF16, es), "dn": k.sbuf("sdn%d" % dd, [128, 8], F32, es),
                     "ho": [k.sbuf("sho%d_%d" % (dd, i), [128, D], F32, es) for i in range(2)], "par": []}
                for pp in range(2):
                    va = k.sbuf("sv%d_%d" % (dd, pp), [128, 4, 257], BF16, es)
                    G(lambda e: e.memset(va[:], 1.0), w=[va])
                    W["par"].append({"va": va, "gg": k.sbuf("sg%d_%d" % (dd, pp), [128, 32], F32, es),
                                     "kss": k.sbuf("skss%d_%d" % (dd, pp), [128, D], BF16, es),
                                     "qst": k.sbuf("sqst%d_%d" % (dd, pp), [128, 8, 128], BF16, es),
                                     "stm": k.sbuf("sst%d_%d" % (dd, pp), [128, 4, 128], BF16, es)})
                W["T"] = PB[dd]
                W["S"] = PB[dd]
                W["PN"] = Ring([PB[3 + dd]])
                W["PU"] = Ring([PB[5 + dd], PB[2] if dd == 0 else PB[7]])
                SW.append(W)

            def scan_stage1(dd, ti, pp):
                W = SW[dd]
                P_ = W["par"][pp]
                q, kk, qs, ks, kst = W["q"], W["kk"], W["qs"], W["ks"], W["kst"]
                va, gg, kss, qst, stm = P_["va"], P_["gg"], P_["kss"], P_["qst"], P_["stm"]
                T, S = W["T"], W["S"]
                Tb = T[:].bitcast(BF16)
                LD(gg[:], GGd[ti, :, :], gg)
                LD(q[:], ZQ[ti, :, 0:1024], q)
                LD(kk[:], ZQ[ti, :, 1024:2048], kk)
                LD(va[:, :, 0:256], ZQ[ti, :, 2048:3072].rearrange("p (h d) -> p h d", h=4), va)
                yield
                for h in range(4):
                    hs = slice(h * 256, (h + 1) * 256)
                    A(lambda e: e.activation(out=qs[:, hs], in_=q[:, hs], func=AF.Identity, scale=gg[:, dd * 4 + h:dd * 4 + h + 1]), r=[q, gg], w=[qs])
                    V(lambda e: e.tensor_scalar(out=ks[:, hs], in0=kk[:, hs], scalar1=gg[:, 8 + dd * 4 + h:8 + dd * 4 + h + 1], scalar2=None, op0=ALU.mult), r=[kk, gg], w=[ks])
                yield
                for c in range(8):
                    P(lambda e: e.transpose(out=Tb[:, c * 128:(c + 1) * 128], in_=qs[:, c * 128:(c + 1) * 128], identity=IDB[:]), r=[qs, IDB], w=[T])
                A(lambda e: e.activation(out=qst[:].rearrange("p c t -> p (c t)"), in_=Tb[:, 0:1024], func=AF.Copy), r=[T], w=[qst])
                yield
                for h in range(4):
                    hs = slice(h * 256, (h + 1) * 256)
                    eng_ = "act" if h % 2 == 0 else "dve"
                    if eng_ == "act":
                        A(lambda e: e.activation(out=kss[:, hs], in_=kk[:, hs], func=AF.Identity, scale=gg[:, 16 + dd * 4 + h:16 + dd * 4 + h + 1]), r=[kk, gg], w=[kss])
                    else:
                        V(lambda e: e.tensor_scalar(out=kss[:, hs], in0=kk[:, hs], scalar1=gg[:, 16 + dd * 4 + h:16 + dd * 4 + h + 1], scalar2=None, op0=ALU.mult), r=[kk, gg], w=[kss])
                yield
                for c in range(8):
                    P(lambda e: e.transpose(out=Tb[:, c * 128:(c + 1) * 128], in_=ks[:, c * 128:(c + 1) * 128], identity=IDB[:]), r=[ks, IDB], w=[T])
                V(lambda e: e.tensor_copy(out=kst[:].rearrange("p c t -> p (c t)"), in_=Tb[:, 0:1024]), r=[T], w=[kst])
                yield
                for h in range(4):
                    for jj in range(2):
                        P(lambda e: e.matmul(S[:, h * 128:(h + 1) * 128], lhsT=kst[:, 2 * h + jj, :], rhs=qst[:, 2 * h + jj, :], start=(jj == 0), stop=(jj == 1)), r=[kst, qst], w=[S])
                V(lambda e: e.tensor_tensor(out=stm[:].rearrange("p h t -> p (h t)"), in0=S[:, 0:512], in1=MASK4[:, dd, :, :].rearrange("p h t -> p (h t)"), op=ALU.mult), r=[S, MASK4], w=[stm])
                yield

            def scan_stage2(dd, ti, pp, n_):
                W = SW[dd]
                P_ = W["par"][pp]
                va, gg, kss, qst, stm = P_["va"], P_["gg"], P_["kss"], P_["qst"], P_["stm"]
                c32, cbf = Cst[dd]
                dn = W["dn"]
                ho = W["ho"][n_ % 2]
                for h in range(4):
                    for jj in range(2):
                        pu = W["PU"].next()
                        P(lambda e: e.matmul(pu[:, 0:257], lhsT=kss[:, h * 256 + jj * 128:h * 256 + (jj + 1) * 128], rhs=va[:, h, :], start=True, stop=True), r=[kss, va], w=[pu])
                        V(lambda e: e.scalar_tensor_tensor(out=c32[:, jj, h, :], in0=c32[:, jj, h, :], scalar=gg[:, 24 + dd * 4 + h:24 + dd * 4 + h + 1], in1=pu[:, 0:257], op0=ALU.mult, op1=ALU.add), r=[c32, gg, pu], w=[c32])
                    pn = W["PN"].next()
                    P(lambda e: e.matmul(pn[:, 0:257], lhsT=stm[:, h, :], rhs=va[:, h, :], start=True, stop=False), r=[stm, va], w=[pn])
                    for jj in range(2):
                        P(lambda e: e.matmul(pn[:, 0:257], lhsT=qst[:, 2 * h + jj, :], rhs=cbf[:, jj, h, :], start=False, stop=(jj == 1)), r=[qst, cbf], w=[pn])
                    V(lambda e: e.tensor_scalar(out=dn[:, 4 + h:5 + h], in0=pn[:, 256:257], scalar1=-1.0, scalar2=1.0, op0=ALU.mult, op1=ALU.max), r=[pn], w=[dn])
                    V(lambda e: e.tensor_tensor(out=dn[:, h:h + 1], in0=dn[:, 4 + h:5 + h], in1=pn[:, 256:257], op=ALU.max), r=[dn, pn], w=[dn])
                    V(lambda e: e.reciprocal(out=dn[:, h:h + 1], in_=dn[:, h:h + 1]), r=[dn], w=[dn])
                    A(lambda e: e.activation(out=ho[:, h * 256:(h + 1) * 256], in_=pn[:, 0:256], func=AF.Identity, scale=dn[:, h:h + 1]), r=[pn, dn], w=[ho])
                    yield
                A(lambda e: e.activation(out=cbf[:, 0, :, :], in_=c32[:, 0, :, :], func=AF.Copy), r=[c32], w=[cbf])
                V(lambda e: e.tensor_copy(out=cbf[:, 1, :, :], in_=c32[:, 1, :, :]), r=[c32], w=[cbf])
                if ti in tiles_out:
                    ST(HD[dd][ti, :, :], ho[:], ho)
                yield

            def scan_stream(dd):
                od = order[dd]
                yield from scan_stage1(dd, od[0], 0)
                for n_, ti in enumerate(od):
                    if n_ + 1 < len(od):
                        yield from scan_stage1(dd, od[n_ + 1], (n_ + 1) % 2)
                    yield from scan_stage2(dd, ti, n_ % 2, n_)

            interleave([scan_stream(0), scan_stream(1)], skew=[0, 3])
        k.barrier()
        if upto == "C" and l == layers - 1:
            break

        with ExitStack() as es:
            es.enter_context(nc.named_scope('E%d' % l))
            WMO = k.sbuf("wmo", [128, 8, D], BF16, es)
            WCO = k.sbuf("wco", [128, 4, D], BF16, es)
            WSO = k.sbuf("wso", [128, 4, D], BF16, es)
            WO = k.sbuf("wo", [128, 8, D], BF16, es)
            for (wb, wd_) in ((WMO, wmo_d), (WCO, wco_d), (WSO, wso_d), (WO, wo_d)):
                k.dma("pool", lambda e: e.dma_start(out=wb[:], in_=wd_[l, :, :].rearrange("(kc p) n -> p kc n", p=128)), writes=[wb], sembuf=wb)
            ROWS = k.sbuf("erows", [64, 128], F32, es)
            CW = k.sbuf("ecw", [128, 4, 32], F32, es)
            SM = k.sbuf("esm", [128, 16], F32, es)
            for c in range(4):
                LD(ROWS[0:31, :], cdw_d[l, :, c * 128:(c + 1) * 128], ROWS)
                P(lambda e: e.transpose(out=PB[0][:, 0:31], in_=ROWS[0:31, :], identity=ID32[0:31, 0:31]), r=[ROWS, ID32], w=[PB[0]])
                V(lambda e: e.tensor_copy(out=CW[:, c, 0:31], in_=PB[0][:, 0:31]), r=[PB[0]], w=[CW])
            LD(ROWS[0:4, :], cdb_d[l, :].rearrange("(r p) -> r p", p=128), ROWS)
            LD(ROWS[4:8, :], clw_d[l, :].rearrange("(r p) -> r p", p=128), ROWS)
            LD(ROWS[8:12, :], clb_d[l, :].rearrange("(r p) -> r p", p=128), ROWS)
            LD(ROWS[12:16, :], sgb_d[l, :, :], ROWS)
            P(lambda e: e.transpose(out=PB[0][:, 0:16], in_=ROWS[0:16, :], identity=ID32[0:16, 0:16]), r=[ROWS, ID32], w=[PB[0]])
            V(lambda e: e.tensor_copy(out=SM[:], in_=PB[0][:, 0:16]), r=[PB[0]], w=[SM])
            SGT = k.sbuf("esgt", [128, 4, 128], BF16, es)
            SGL = k.sbuf("esgl", [128, 128], F32, es)
            for g in range(4):
                LD(SGL[:], sgw_d[l, g, :, :], SGL)
                P(lambda e: e.transpose(out=PB[0][:, 0:128], in_=SGL[:], identity=ID32[:]), r=[SGL, ID32], w=[PB[0]])
                V(lambda e: e.tensor_copy(out=SGT[:, g, :], in_=PB[0][:, 0:128]), r=[PB[0]], w=[SGT])
            MNW = k.sbuf("emnw", [128, D], F32, es)
            SLW = k.sbuf("eslw", [128, 512], F32, es)
            SLB = k.sbuf("eslb", [128, 512], F32, es)
            LD(MNW[:], mnw_d[l, :].partition_broadcast(128), MNW)
            LD(SLW[:], slw_d[l, :].partition_broadcast(128), SLW)
            LD(SLB[:], slb_d[l, :].partition_broadcast(128), SLB)
            G1R = [k.sbuf("eg1r%d" % j, [128, D], F32, es) for j in range(2 if need_ctx else 1)]
            for j in range(len(G1R)):
                bcast_row(G1R[j], MODd[j, 2 * D:3 * D])

            DIAG = k.sbuf("ediag", [128, 4, 31, 128], BF16, es)
            for c in range(4):
                for jt in range(31):
                    if (c * 31 + jt) % 2 == 0:
                        V(lambda e: e.tensor_scalar(out=DIAG[:, c, jt, :], in0=ID32[:], scalar1=CW[:, c, jt:jt + 1], scalar2=None, op0=ALU.mult), r=[ID32, CW], w=[DIAG])
                    else:
                        A(lambda e: e.activation(out=DIAG[:, c, jt, :], in_=ID32[:], func=AF.Identity, scale=CW[:, c, jt:jt + 1]), r=[ID32, CW], w=[DIAG])
            UPC = k.sbuf("eupc", [128, 4, 286], BF16, es)
            G(lambda e: e.memset(UPC[:], 0.0), w=[UPC])
            UB = k.sbuf("eub", [128, 512], BF16, es)
            junk = k.sbuf("ejunk", [128, D], BF16, es)
            TMP = k.sbuf("etmp", [128, 512], F32, es)
            WS = []
            for s_ in range(2):
                W = {}
                W["zr"] = k.sbuf("ezr%d" % s_, [128, ZR_W], BF16, es)
                W["hf"] = k.sbuf("ehf%d" % s_, [128, D], F32, es)
                W["xt"] = k.sbuf("ext%d" % s_, [128, D], F32, es)
                W["T1"] = k.sbuf("et1%d" % s_, [128, D], BF16, es)
                W["SS4"] = k.sbuf("ess4%d" % s_, [128, 4], F32, es)
                W["YN"] = k.sbuf("eyn%d" % s_, [128, D], BF16, es)
                W["YNT"] = k.sbuf("eynt%d" % s_, [128, 8, 128], BF16, es)
                W["MG"] = k.sbuf("emg%d" % s_, [128, D], F32, es)
                W["hb"] = W["MG"]
                W["UPL"] = k.sbuf("eupl%d" % s_, [128, 4, 2, 94], BF16, es)
                G(lambda e: e.memset(W["UPL"][:], 0.0), w=[W["UPL"]])
                W["CV"] = k.sbuf("ecv%d" % s_, [128, 4, 128], F32, es)
                W["CSQ"] = k.sbuf("ecsq%d" % s_, [128, 4, 128], F32, es)
                W["M2"] = k.sbuf("em2%d" % s_, [128, 128], F32, es)
                W["RSTD"] = k.sbuf("erstd%d" % s_, [128, 128], F32, es)
                W["CA"] = k.sbuf("eca%d" % s_, [128, 4, 128], BF16, es)
                W["ST2"] = k.sbuf("est2%d" % s_, [128, 4], F32, es)
                W["VNf"] = k.sbuf("evnf%d" % s_, [128, 512], F32, es)
                W["VNb"] = k.sbuf("evnb%d" % s_, [128, 512], BF16, es)
                W["SGO"] = k.sbuf("esgo%d" % s_, [128, 512], BF16, es)
                W["SGOT"] = k.sbuf("esgot%d" % s_, [128, 4, 128], BF16, es)
                W["MGB"] = k.sbuf("emgb%d" % s_, [128, D], BF16, es)
                W["MGT"] = k.sbuf("emgt%d" % s_, [128, 8, 128], BF16, es)
                W["pb"] = [PB[s_ * 4 + i] for i in range(4)]
                W["PY"] = Ring([PB[s_ * 4 + 2], PB[s_ * 4 + 3]])
                WS.append(W)

            if need_ctx:
                for ti in range(2):
                    LD(UB[:], ZR[ti, :, ZR_U:ZR_U + 512], UB)
                    for c in range(4):
                        P(lambda e: e.transpose(out=pbf(0)[:, c * 128:(c + 1) * 128], in_=UB[:, c * 128:(c + 1) * 128], identity=IDB[:]), r=[UB, IDB], w=[PB[0]])
                    V(lambda e: e.tensor_copy(out=UPC[:, :, 15 + ti * 128:15 + (ti + 1) * 128], in_=pbf(0)[:, 0:512].rearrange("p (c t) -> p c t", c=4)), r=[PB[0]], w=[UPC])

            def e_tile(ti, W):
                zr, hf, hb, xt = W["zr"], W["hf"], W["hb"], W["xt"]
                T1, SS4, YN, YNT, MG, UPL, CV, CSQ = W["T1"], W["SS4"], W["YN"], W["YNT"], W["MG"], W["UPL"], W["CV"], W["CSQ"]
                M2, RSTD, CA, ST2, VNf, VNb, SGO, SGOT, MGB, MGT = W["M2"], W["RSTD"], W["CA"], W["ST2"], W["VNf"], W["VNb"], W["SGO"], W["SGOT"], W["MGB"], W["MGT"]
                pT, pS = W["pb"][0], W["pb"][1]
                pTb = pT[:].bitcast(BF16)
                PY = W["PY"]
                j = 1 if ti < 2 else 0
                LD(zr[:], ZR[ti, :, :], zr)
                LD(hf[:], HD[0][ti, :, :], hf)
                LD(hb[:], HD[1][ti, :, :], hb)
                LD(xt[:], xs_cur[ti * 128:(ti + 1) * 128, :], xt)
                yield

                def gate_acc(py, half, goff, first, final):
                    gsl = zr[:, ZR_GM + goff + half * 512:ZR_GM + goff + (half + 1) * 512]
                    hs = slice(half * 512, (half + 1) * 512)
                    if first:
                        V(lambda e: e.tensor_tensor(out=MG[:, hs], in0=py[:, 0:512], in1=gsl, op=ALU.mult), r=[py, zr], w=[MG])
                    else:
                        V(lambda e: e.tensor_tensor(out=TMP[:], in0=py[:, 0:512], in1=gsl, op=ALU.mult), r=[py, zr], w=[TMP])
                        if final:
                            V(lambda e: e.tensor_tensor(out=MGB[:, hs], in0=MG[:, hs], in1=TMP[:], op=ALU.add), r=[MG, TMP], w=[MGB])
                        else:
                            V(lambda e: e.tensor_tensor(out=MG[:, hs], in0=MG[:, hs], in1=TMP[:], op=ALU.add), r=[MG, TMP], w=[MG])

                if ti >= 2:
                    for c in range(4):
                        P(lambda e: e.transpose(out=pTb[:, c * 128:(c + 1) * 128], in_=zr[:, ZR_U + c * 128:ZR_U + (c + 1) * 128], identity=IDB[:]), r=[zr, IDB], w=[pT])
                    for c in range(4):
                        A(lambda e: e.activation(out=UPL[:, c, :, 15:79], in_=pTb[:, c * 128:(c + 1) * 128].rearrange("p (r w) -> p r w", r=2), func=AF.Copy), r=[pT], w=[UPL])

                    def win(c, jt):
                        return UPL[:, c, :, jt:jt + 64]
                    ubuf = UPL
                else:
                    def win(c, jt):
                        return UPC[:, c, ti * 128 + jt:ti * 128 + jt + 128]
                    ubuf = UPC
                yield
                V(lambda e: e.tensor_tensor(out=hf[:], in0=hf[:], in1=hb[:], op=ALU.add), r=[hf, hb], w=[hf])
                for h in range(4):
                    A(lambda e: e.activation(out=junk[:, h * 256:(h + 1) * 256], in_=hf[:, h * 256:(h + 1) * 256], func=AF.Square, accum_out=SS4[:, h:h + 1]), r=[hf], w=[junk, SS4])
                G(lambda e: e.tensor_tensor(out=T1[:], in0=zr[:, ZR_OG:ZR_OG + 1024], in1=MNW[:], op=ALU.mult), r=[zr, MNW], w=[T1])
                yield
                for c in range(4):
                    for jt in range(31):
                        P(lambda e: e.matmul(pS[:, c * 128:(c + 1) * 128], lhsT=DIAG[:, c, jt, :], rhs=win(c, jt), start=(jt == 0), stop=(jt == 30)), r=[DIAG, ubuf], w=[pS])
                    if c % 2 == 1:
                        yield
                A(lambda e: e.activation(out=SS4[:], in_=SS4[:], func=AF.Sqrt, bias=EPSC[:], scale=1.0 / 256.0), r=[SS4, EPSC], w=[SS4])
                V(lambda e: e.reciprocal(out=SS4[:], in_=SS4[:]), r=[SS4], w=[SS4])
                for h in range(4):
                    hs = slice(h * 256, (h + 1) * 256)
                    V(lambda e: e.scalar_tensor_tensor(out=YN[:, hs], in0=hf[:, hs], scalar=SS4[:, h:h + 1], in1=T1[:, hs], op0=ALU.mult, op1=ALU.mult), r=[hf, SS4, T1], w=[YN])
                yield
                for c in range(4):
                    A(lambda e: e.activation(out=CV[:, c, :], in_=pS[:, c * 128:(c + 1) * 128], func=AF.Identity, bias=SM[:, c:c + 1], scale=1.0), r=[pS, SM], w=[CV])
                A(lambda e: e.activation(out=CSQ[:], in_=CV[:], func=AF.Square), r=[CV], w=[CSQ])
                yield
                for c in range(8):
                    P(lambda e: e.transpose(out=pTb[:, c * 128:(c + 1) * 128], in_=YN[:, c * 128:(c + 1) * 128], identity=IDB[:]), r=[YN, IDB], w=[pT])
                A(lambda e: e.activation(out=YNT[:].rearrange("p c t -> p (c t)"), in_=pTb[:, 0:1024], func=AF.Copy), r=[pT], w=[YNT])
                yield
                for c in range(4):
                    P(lambda e: e.matmul(pS[:, 0:128], lhsT=MEAN32[:], rhs=CV[:, c, :], start=(c == 0), stop=(c == 3)), r=[MEAN32, CV], w=[pS])
                for c in range(4):
                    P(lambda e: e.matmul(pS[:, 128:256], lhsT=MEAN32[:], rhs=CSQ[:, c, :], start=(c == 0), stop=(c == 3)), r=[MEAN32, CSQ], w=[pS])
                yield
                for half in range(2):
                    py = PY.next()
                    for kc in range(8):
                        P(lambda e: e.matmul(py[:, 0:512], lhsT=YNT[:, kc, :], rhs=WMO[:, kc, half * 512:(half + 1) * 512], start=(kc == 0), stop=(kc == 7)), r=[YNT, WMO], w=[py])
                    gate_acc(py, half, 0, True, False)
                    yield
                A(lambda e: e.activation(out=M2[:], in_=pS[:, 0:128], func=AF.Square), r=[pS], w=[M2])
                V(lambda e: e.tensor_tensor(out=RSTD[:], in0=pS[:, 128:256], in1=M2[:], op=ALU.subtract), r=[pS, M2], w=[RSTD])
                A(lambda e: e.activation(out=RSTD[:], in_=RSTD[:], func=AF.Sqrt, bias=EPSC[:], scale=1.0), r=[RSTD, EPSC], w=[RSTD])
                V(lambda e: e.reciprocal(out=RSTD[:], in_=RSTD[:]), r=[RSTD], w=[RSTD])
                V(lambda e: e.tensor_copy(out=M2[:], in_=pS[:, 0:128]), r=[pS], w=[M2])
                yield
                for c in range(4):
                    eng_ = "dve"
                    k.op(eng_, lambda e: e.tensor_tensor(out=CSQ[:, c, :], in0=CV[:, c, :], in1=M2[:], op=ALU.subtract), [CV, M2], [CSQ])
                    k.op(eng_, lambda e: e.tensor_tensor(out=CSQ[:, c, :], in0=CSQ[:, c, :], in1=RSTD[:], op=ALU.mult), [CSQ, RSTD], [CSQ])
                yield
                for c in range(4):
                    A(lambda e: e.activation(out=CA[:, c, :], in_=CSQ[:, c, :], func=AF.Silu, bias=SM[:, 8 + c:9 + c], scale=SM[:, 4 + c:5 + c]), r=[CSQ, SM], w=[CA])
                yield
                vv = zr[:, ZR_ZS + 512:ZR_ZS + 1024]
                A(lambda e: e.activation(out=junk[:, 0:512], in_=vv, func=AF.Identity, accum_out=ST2[:, 0:1]), r=[zr], w=[junk, ST2])
                A(lambda e: e.activation(out=junk[:, 512:1024], in_=vv, func=AF.Square, accum_out=ST2[:, 1:2]), r=[zr], w=[junk, ST2])
                yield
                for half in range(2):
                    py = PY.next()
                    for c in range(4):
                        P(lambda e: e.matmul(py[:, 0:512], lhsT=CA[:, c, :], rhs=WCO[:, c, half * 512:(half + 1) * 512], start=(c == 0), stop=(c == 3)), r=[CA, WCO], w=[py])
                    gate_acc(py, half, 1024, False, False)
                yield
                V(lambda e: e.tensor_scalar(out=ST2[:, 0:2], in0=ST2[:, 0:2], scalar1=1.0 / 512.0, scalar2=None, op0=ALU.mult), r=[ST2], w=[ST2])
                V(lambda e: e.tensor_tensor(out=ST2[:, 2:3], in0=ST2[:, 0:1], in1=ST2[:, 0:1], op=ALU.mult), r=[ST2], w=[ST2])
                yield
                V(lambda e: e.tensor_tensor(out=ST2[:, 2:3], in0=ST2[:, 1:2], in1=ST2[:, 2:3], op=ALU.subtract), r=[ST2], w=[ST2])
                A(lambda e: e.activation(out=ST2[:, 2:3], in_=ST2[:, 2:3], func=AF.Sqrt, bias=EPSC[:], scale=1.0), r=[ST2, EPSC], w=[ST2])
                yield
                V(lambda e: e.reciprocal(out=ST2[:, 2:3], in_=ST2[:, 2:3]), r=[ST2], w=[ST2])
                yield
                V(lambda e: e.tensor_scalar(out=VNf[:], in0=vv, scalar1=ST2[:, 0:1], scalar2=ST2[:, 2:3], op0=ALU.subtract, op1=ALU.mult), r=[zr, ST2], w=[VNf])
                yield
                V(lambda e: e.tensor_tensor(out=VNf[:], in0=VNf[:], in1=SLW[:], op=ALU.mult), r=[VNf, SLW], w=[VNf])
                yield
                V(lambda e: e.tensor_tensor(out=VNb[:], in0=VNf[:], in1=SLB[:], op=ALU.add), r=[VNf, SLB], w=[VNb])
                yield
                for g in range(4):
                    P(lambda e: e.matmul(pS[:, g * 128:(g + 1) * 128], lhsT=SGT[:, g, :], rhs=VNb[:, g * 128:(g + 1) * 128], start=True, stop=True), r=[SGT, VNb], w=[pS])
                yield
                for g in range(4):
                    V(lambda e: e.scalar_tensor_tensor(out=SGO[:, g * 128:(g + 1) * 128], in0=pS[:, g * 128:(g + 1) * 128], scalar=SM[:, 12 + g:13 + g], in1=zr[:, ZR_ZS + g * 128:ZR_ZS + (g + 1) * 128], op0=ALU.add, op1=ALU.mult), r=[pS, SM, zr], w=[SGO])
                yield
                for c in range(4):
                    P(lambda e: e.transpose(out=pTb[:, c * 128:(c + 1) * 128], in_=SGO[:, c * 128:(c + 1) * 128], identity=IDB[:]), r=[SGO, IDB], w=[pT])
                A(lambda e: e.activation(out=SGOT[:].rearrange("p c t -> p (c t)"), in_=pTb[:, 0:512], func=AF.Copy), r=[pT], w=[SGOT])
                yield
                for half in range(2):
                    py = PY.next()
                    for c in range(4):
                        P(lambda e: e.matmul(py[:, 0:512], lhsT=SGOT[:, c, :], rhs=WSO[:, c, half * 512:(half + 1) * 512], start=(c == 0), stop=(c == 3)), r=[SGOT, WSO], w=[py])
                    gate_acc(py, half, 2048, False, True)
                yield
                for c in range(8):
                    P(lambda e: e.transpose(out=pTb[:, c * 128:(c + 1) * 128], in_=MGB[:, c * 128:(c + 1) * 128], identity=IDB[:]), r=[MGB, IDB], w=[pT])
                A(lambda e: e.activation(out=MGT[:].rearrange("p c t -> p (c t)"), in_=pTb[:, 0:1024], func=AF.Copy), r=[pT], w=[MGT])
                yield
                for half in range(2):
                    py = PY.next()
                    hs = slice(half * 512, (half + 1) * 512)
                    for kc in range(8):
                        P(lambda e: e.matmul(py[:, 0:512], lhsT=MGT[:, kc, :], rhs=WO[:, kc, hs], start=(kc == 0), stop=(kc == 7)), r=[MGT, WO], w=[py])
                    V(lambda e: e.tensor_tensor(out=MG[:, hs], in0=py[:, 0:512], in1=G1R[j][:, hs], op=ALU.mult), r=[py, G1R[j]], w=[MG])
                    yield
                    G(lambda e: e.tensor_tensor(out=xt[:, hs], in0=MG[:, hs], in1=xt[:, hs], op=ALU.add), r=[MG, xt], w=[xt])
                    yield
                ST(xs_oth[ti * 128:(ti + 1) * 128, :], xt[:], xt)
                yield

            def e_stream(s_):
                for ti in tiles_out[s_::2]:
                    yield from e_tile(ti, WS[s_])

            interleave([e_stream(0), e_stream(1)], skew=[0, 18])
        k.barrier()
        if upto == "E" and l == layers - 1:
            break

        sets = [("lat", list(range(2, NT)), 512)]
        if need_ctx:
            sets.append(("ctx", [0, 1], 32))
        with ExitStack() as es:
            ZERO = k.sbuf("zero", [128, 1024], F32, es)
            IOTAF = k.sbuf("iotaf", [128, 512], F32, es)
            IOTAI = k.sbuf("iotai", [128, 512], I32, es)
            G(lambda e: e.memset(ZERO[:], 0.0), w=[ZERO])
            G(lambda e: e.iota(IOTAI[:], pattern=[[1, 512]], base=0, channel_multiplier=0), w=[IOTAI])
            V(lambda e: e.tensor_copy(out=IOTAF[:], in_=IOTAI[:]), r=[IOTAI], w=[IOTAF])
            RW = k.sbuf("frw", [128, 8, NE], F32, es)
            LD(RW[:], rw_d[l, :, :].rearrange("(kc p) n -> p kc n", p=128), RW)
            RBR = k.sbuf("frbr", [128, NE], F32, es)
            LD(RBR[:], rb_d[l, :].partition_broadcast(128), RBR)
            AFF = k.sbuf("faff", [128, NT, NE], F32, es)
            W2R = [k.sbuf("fw2r%d" % j, [128, D], F32, es) for j in range(len(sets))]
            S2R = [k.sbuf("fs2r%d" % j, [128, D], F32, es) for j in range(len(sets))]
            for j in range(len(sets)):
                bcast_row(W2R[j], WSd[j, 1, :])
                bcast_row(S2R[j], MODd[j, 3 * D:4 * D])

            with ExitStack() as es1:
                es1.enter_context(nc.named_scope('F1_%d' % l))
                junk = k.sbuf("fjunk", [128, D], BF16, es1)
                FW = []
                for s_ in range(2):
                    FW.append({"xt": k.sbuf("fxt%d" % s_, [128, D], F32, es1), "xn": k.sbuf("fxn%d" % s_, [128, D], F32, es1),
                               "tmp": k.sbuf("ftmp%d" % s_, [128, D], F32, es1),
                               "ssq": k.sbuf("fssq%d" % s_, [128, 1], F32, es1), "rt": k.sbuf("frt%d" % s_, [128, 1], F32, es1),
                               "h2T": k.sbuf("fh2t%d" % s_, [128, 8, 128], F32, es1), "h2r": k.sbuf("fh2r%d" % s_, [128, D], BF16, es1),
                               "LG": k.sbuf("flg%d" % s_, [128, NE], F32, es1), "MX": k.sbuf("fmx%d" % s_, [128, 2], F32, es1),
                               "pt": PB[s_], "pr": PB[2 + s_]})

                def f1_tile(ti, W):
                    xt, xn, tmp, ssq, rt, h2T, h2r, LG, MX, pr = W["xt"], W["xn"], W["tmp"], W["ssq"], W["rt"], W["h2T"], W["h2r"], W["LG"], W["MX"], W["pr"]
                    j = 1 if ti < 2 else 0
                    LD(xt[:], xs_oth[ti * 128:(ti + 1) * 128, :], xt)
                    k.dma("pool", lambda e: e.dma_start(out=MACC[ti * 128:(ti + 1) * 128, :], in_=ZERO[:]), reads=[ZERO], sembuf=ZERO)
                    yield
                    rms_to_xn(xt, xn, junk, ssq, rt)
                    yield
                    xn_to_hT(xn, h2T, j, 2, 3, pbs=(W["pt"], W["pt"]))
                    yield
                    V(lambda e: e.tensor_tensor(out=tmp[:], in0=xn[:], in1=W2R[j][:], op=ALU.mult), r=[xn, W2R[j]], w=[tmp])
                    yield
                    V(lambda e: e.tensor_tensor(out=h2r[:], in0=tmp[:], in1=S2R[j][:], op=ALU.add), r=[tmp, S2R[j]], w=[h2r])
                    ST(H2d[ti * 128:(ti + 1) * 128, :], h2r[:], h2r)
                    yield
                    for kc in range(8):
                        P(lambda e: e.matmul(pr[:, 0:NE], lhsT=h2T[:, kc, :], rhs=RW[:, kc, :], start=(kc == 0), stop=(kc == 7)), r=[h2T, RW], w=[pr])
                    yield
                    V(lambda e: e.tensor_tensor(out=LG[:], in0=pr[:, 0:NE], in1=RBR[:], op=ALU.add), r=[pr, RBR], w=[LG])
                    yield
                    V(lambda e: e.reduce_max(out=MX[:, 0:1], in_=LG[:], axis=mybir.AxisListType.X), r=[LG], w=[MX])
                    yield
                    V(lambda e: e.tensor_scalar(out=MX[:, 0:1], in0=MX[:, 0:1], scalar1=-1.0, scalar2=None, op0=ALU.mult), r=[MX], w=[MX])
                    yield
                    A(lambda e: e.activation(out=LG[:], in_=LG[:], func=AF.Exp, bias=MX[:, 0:1], scale=1.0, accum_out=MX[:, 1:2]), r=[LG, MX], w=[LG, MX])
                    yield
                    V(lambda e: e.reciprocal(out=MX[:, 1:2], in_=MX[:, 1:2]), r=[MX], w=[MX])
                    yield
                    V(lambda e: e.tensor_scalar(out=AFF[:, ti, :], in0=LG[:], scalar1=MX[:, 1:2], scalar2=None, op0=ALU.mult), r=[LG, MX], w=[AFF])
                    yield

                def f1_stream(s_):
                    for ti in tiles_out[s_::2]:
                        yield from f1_tile(ti, FW[s_])

                interleave([f1_stream(0), f1_stream(1)], skew=[0, 6])
            k.barrier()

            TOKI = k.sbuf("ftoki", [128, NE, 5], I32, es)
            GS = k.sbuf("fgs", [128, NE, 5], F32, es)
            with ExitStack() as es2:
                es2.enter_context(nc.named_scope('F3_%d' % l))
                AFFT = k.sbuf("fafft", [16, SEQ], F32, es2)
                JK = k.sbuf("fjk", [16, SEQ], F32, es2)
                BS = k.sbuf("fbs", [16, 8], F32, es2)
                DG = k.sbuf("fdg", [16, 16], F32, es2)
                THRB = k.sbuf("fthrb", [128, NE], F32, es2)
                MK = k.sbuf("fmk", [128, 32, NE], F32, es2)
                MKB = k.sbuf("fmkb", [128, 32, NE], BF16, es2)
                OFFS = k.sbuf("foffs", [128, 32, NE], F32, es2)
                POS = k.sbuf("fpos", [128, 32, NE], F32, es2)
                RH = k.sbuf("frh", [128, 32, NE, 5], BF16, es2)
                R1 = k.sbuf("fr1", [128, 32, NE], F32, es2)
                R2 = k.sbuf("fr2", [128, 32, NE], F32, es2)
                OH = Ring([k.sbuf("foh%d" % i, [128, 512], BF16, es2) for i in range(6)])
                TF = k.sbuf("ftf", [128, 8], F32, es2)
                for si, (sname, stiles, cap) in enumerate(sets):
                    nt = len(stiles)
                    ntok = nt * 128
                    t0 = stiles[0]
                    nst = (cap + 127) // 128
                    for q in range(nt):
                        P(lambda e: e.transpose(out=PB[0][0:16, (q % 4) * 128:(q % 4 + 1) * 128], in_=AFF[:, t0 + q, :], identity=ID32[:]), r=[AFF, ID32], w=[PB[0]])
                        if q % 4 == 3 or q == nt - 1:
                            q0 = (q // 4) * 4
                            wq = (q - q0 + 1) * 128
                            V(lambda e: e.tensor_copy(out=AFFT[:, q0 * 128:q0 * 128 + wq], in_=PB[0][0:16, 0:wq]), r=[PB[0]], w=[AFFT])
                    V(lambda e: e.memset(BS[:, 0:1], 0.0), w=[BS])
                    V(lambda e: e.memset(BS[:, 1:2], 1.0), w=[BS])
                    for it in range(26):
                        V(lambda e: e.tensor_scalar(out=BS[:, 2:3], in0=BS[:, 0:1], scalar1=BS[:, 1:2], scalar2=0.5, op0=ALU.add, op1=ALU.mult), r=[BS], w=[BS])
                        V(lambda e: e.tensor_scalar(out=JK[:, 0:ntok], in0=AFFT[:, 0:ntok], scalar1=BS[:, 2:3], scalar2=0.0, op0=ALU.is_ge, op1=ALU.add, accum_out=BS[:, 3:4]), r=[AFFT, BS], w=[JK, BS])
                        V(lambda e: e.tensor_scalar(out=BS[:, 4:5], in0=BS[:, 3:4], scalar1=float(cap) - 0.5, scalar2=None, op0=ALU.is_ge), r=[BS], w=[BS])
                        V(lambda e: e.tensor_tensor(out=BS[:, 5:6], in0=BS[:, 2:3], in1=BS[:, 0:1], op=ALU.subtract), r=[BS], w=[BS])
                        V(lambda e: e.tensor_tensor(out=BS[:, 6:7], in0=BS[:, 1:2], in1=BS[:, 2:3], op=ALU.subtract), r=[BS], w=[BS])
                        V(lambda e: e.scalar_tensor_tensor(out=BS[:, 0:1], in0=BS[:, 5:6], scalar=BS[:, 4:5], in1=BS[:, 0:1], op0=ALU.mult, op1=ALU.add), r=[BS], w=[BS])
                        V(lambda e: e.scalar_tensor_tensor(out=BS[:, 1:2], in0=BS[:, 6:7], scalar=BS[:, 4:5], in1=BS[:, 2:3], op0=ALU.mult, op1=ALU.add), r=[BS], w=[BS])
                    V(lambda e: e.tensor_scalar(out=DG[:], in0=ID32[0:16, 0:16], scalar1=BS[:, 0:1], scalar2=None, op0=ALU.mult), r=[ID32, BS], w=[DG])
                    P(lambda e: e.matmul(PB[1][:, 0:NE], lhsT=ONES32[0:16, :], rhs=DG[:], start=True, stop=True), r=[ONES32, DG], w=[PB[1]])
                    V(lambda e: e.tensor_copy(out=THRB[:], in_=PB[1][:, 0:NE]), r=[PB[1]], w=[THRB])
                    for q in range(nt):
                        V(lambda e: e.tensor_tensor(out=MK[:, q, :], in0=AFF[:, t0 + q, :], in1=THRB[:], op=ALU.is_ge), r=[AFF, THRB], w=[MK])
                    ncol = nt * NE
                    mkf = MK[:, 0:nt, :].rearrange("p t e -> p (t e)")
                    V(lambda e: e.tensor_copy(out=MKB[:, 0:nt, :].rearrange("p t e -> p (t e)"), in_=mkf), r=[MK], w=[MKB])
                    P(lambda e: e.matmul(PB[2][:, 0:ncol], lhsT=SUB[:], rhs=MKB[:, 0:nt, :].rearrange("p t e -> p (t e)"), start=True, stop=True), r=[SUB, MKB], w=[PB[2]])
                    P(lambda e: e.matmul(PB[3][:, 0:ncol], lhsT=ONESB[:], rhs=MKB[:, 0:nt, :].rearrange("p t e -> p (t e)"), start=True, stop=True), r=[ONESB, MKB], w=[PB[3]])
                    V(lambda e: e.memset(OFFS[:, 0, :], 0.0), w=[OFFS])
                    for q in range(1, nt):
                        V(lambda e: e.tensor_tensor(out=OFFS[:, q, :], in0=OFFS[:, q - 1, :], in1=PB[3][:, (q - 1) * NE:q * NE], op=ALU.add), r=[OFFS, PB[3]], w=[OFFS])
                    posf = POS[:, 0:nt, :].rearrange("p t e -> p (t e)")
                    V(lambda e: e.tensor_tensor(out=posf, in0=PB[2][:, 0:ncol], in1=OFFS[:, 0:nt, :].rearrange("p t e -> p (t e)"), op=ALU.add), r=[PB[2], OFFS], w=[POS])
                    V(lambda e: e.tensor_scalar(out=R1[:, 0:nt, :].rearrange("p t e -> p (t e)"), in0=posf, scalar1=float(cap) - 0.5, scalar2=None, op0=ALU.is_lt), r=[POS], w=[R1])
                    V(lambda e: e.tensor_tensor(out=mkf, in0=mkf, in1=R1[:, 0:nt, :].rearrange("p t e -> p (t e)"), op=ALU.mult), r=[MK, R1], w=[MK])
                    for q in range(nt):
                        V(lambda e: e.memset(RH[:, q, :, 0], float((t0 + q) * 128)), w=[RH])
                        V(lambda e: e.tensor_scalar(out=RH[:, q, :, 1], in0=ONES32[:, 0:NE], scalar1=PIDX[:, 0:1], scalar2=None, op0=ALU.mult), r=[ONES32, PIDX], w=[RH])
                    afs = AFF[:, t0:t0 + nt, :]
                    V(lambda e: e.tensor_copy(out=RH[:, 0:nt, :, 2], in_=afs), r=[AFF], w=[RH])
                    V(lambda e: e.tensor_tensor(out=R1[:, 0:nt, :], in0=afs, in1=RH[:, 0:nt, :, 2], op=ALU.subtract), r=[AFF, RH], w=[R1])
                    V(lambda e: e.tensor_copy(out=RH[:, 0:nt, :, 3], in_=R1[:, 0:nt, :]), r=[R1], w=[RH])
                    V(lambda e: e.tensor_tensor(out=R2[:, 0:nt, :], in0=R1[:, 0:nt, :], in1=RH[:, 0:nt, :, 3], op=ALU.subtract), r=[R1, RH], w=[R2])
                    V(lambda e: e.tensor_copy(out=RH[:, 0:nt, :, 4], in_=R2[:, 0:nt, :]), r=[R2], w=[RH])
                    PQ = [PB[4], PB[5], PB[6], PB[7]]
                    capw = nst * 128 if cap >= 128 else cap
                    for ex in range(NE):
                        for q in range(nt):
                            oh = OH.next()
                            k.op("dve", lambda e: e.tensor_scalar(out=oh[:, 0:capw], in0=IOTAF[:, 0:capw], scalar1=POS[:, q, ex:ex + 1], scalar2=MK[:, q, ex:ex + 1], op0=ALU.is_equal, op1=ALU.mult), [IOTAF, POS, MK], [oh])
                            for s_ in range(nst):
                                mw = min(128, cap - s_ * 128)
                                P(lambda e: e.matmul(PQ[s_][0:mw, 0:5], lhsT=oh[:, s_ * 128:s_ * 128 + mw], rhs=RH[:, q, ex, :], start=(q == 0), stop=(q == nt - 1)), r=[oh, RH], w=[PQ[s_]])
                        for s_ in range(nst):
                            mw = min(128, cap - s_ * 128)
                            sl = s_ if si == 0 else 4
                            V(lambda e: e.tensor_copy(out=TF[0:mw, 0:5], in_=PQ[s_][0:mw, 0:5]), r=[PQ[s_]], w=[TF])
                            V(lambda e: e.tensor_tensor(out=TF[0:mw, 5:6], in0=TF[0:mw, 0:1], in1=TF[0:mw, 1:2], op=ALU.add), r=[TF], w=[TF])
                            V(lambda e: e.tensor_copy(out=TOKI[0:mw, ex, sl:sl + 1], in_=TF[0:mw, 5:6]), r=[TF], w=[TOKI])
                            V(lambda e: e.tensor_tensor(out=TF[0:mw, 6:7], in0=TF[0:mw, 2:3], in1=TF[0:mw, 3:4], op=ALU.add), r=[TF], w=[TF])
                            V(lambda e: e.tensor_tensor(out=GS[0:mw, ex, sl:sl + 1], in0=TF[0:mw, 6:7], in1=TF[0:mw, 4:5], op=ALU.add), r=[TF], w=[GS])
            k.barrier()

            with ExitStack() as es3:
                es3.enter_context(nc.named_scope('F5_%d' % l))
                WPR = Ring([k.sbuf("fwp%d" % i, [128, 16, 512], BF16, es3) for i in range(3)])
                NS = 544 if need_ctx else 512
                nsl = 5 if need_ctx else 4
                XE2 = [[k.sbuf("fxe%d_%d" % (pp, i), [128, D], BF16, es3) for i in range(nsl)] for pp in range(2)]
                XET2 = [k.sbuf("fxet%d" % pp, [128, 8, NS], BF16, es3) for pp in range(2)]
                HMT = k.sbuf("fhmt", [128, 16, NS], BF16, es3)
                SA = Ring([k.sbuf("fsa%d" % i, [128, NS], F32, es3) for i in range(2)])
                YS = [k.sbuf("fys%d" % i, [128, D], F32, es3) for i in range(5 if need_ctx else 4)]
                slot_tiles = [(s_, 128, s_ * 128) for s_ in range(4)]
                if need_ctx:
                    slot_tiles.append((4, 32, 512))
                pieces = []
                for ex in range(NE):
                    for g in range(4):
                        pieces.append((ex, "g", g))
                    for hf_ in range(2):
                        pieces.append((ex, "d", hf_))
                loaded = {}

                def issue_piece(pi):
                    if pi >= len(pieces) or pi in loaded:
                        return
                    ex, kind, g = pieces[pi]
                    wp = WPR.next()
                    if kind == "g":
                        k.dma("pool", lambda e: e.dma_start(out=wp[:, 0:8, :], in_=wg_d[l, ex, :, g * 512:(g + 1) * 512].rearrange("(kc p) n -> p kc n", p=128)), writes=[wp], sembuf=wp)
                        k.dma("pool", lambda e: e.dma_start(out=wp[:, 8:16, :], in_=wu_d[l, ex, :, g * 512:(g + 1) * 512].rearrange("(kc p) n -> p kc n", p=128)), writes=[], reads=[], sembuf=wp)
                        wp.writes = {("d", wp.dsem): k.dcount[wp.dsem]}
                    else:
                        k.dma("pool", lambda e: e.dma_start(out=wp[:], in_=wd_d[l, ex, :, g * 512:(g + 1) * 512].rearrange("(fc p) n -> p fc n", p=128)), writes=[wp], sembuf=wp)
                    loaded[pi] = wp

                def f5_gather(ex):
                    for (s_, mw, off) in slot_tiles:
                        xe = XE2[ex % 2][s_]
                        k.dma("pool", lambda e: e.indirect_dma_start(out=xe[0:mw, :], out_offset=None, in_=H2d[:, :], in_offset=bass.IndirectOffsetOnAxis(ap=TOKI[0:mw, ex, s_:s_ + 1], axis=0)), reads=[TOKI], writes=[xe], sembuf=xe)

                def f5_xet(ex):
                    for (s_, mw, off) in slot_tiles:
                        xe = XE2[ex % 2][s_]
                        for c in range(8):
                            P(lambda e: e.transpose(out=pbf(6)[:, c * 128:c * 128 + mw], in_=xe[0:mw, c * 128:(c + 1) * 128], identity=IDB[0:mw, 0:mw]), r=[xe, IDB], w=[PB[6]])
                        V(lambda e: e.tensor_copy(out=XET2[ex % 2][:, :, off:off + mw], in_=pbf(6)[:, 0:1024].rearrange("p (c t) -> p c t", c=8)[:, :, 0:mw]), r=[PB[6]], w=[XET2[ex % 2]])

                PA = Ring([PB[0], PB[1]])
                PUu = Ring([PB[2], PB[3]])
                PYr = Ring([PB[4], PB[5]])
                issue_piece(0)
                issue_piece(1)
                pi = 0
                prev_tokens = {}
                for ex in range(NE):
                    XET = XET2[ex % 2]
                    f5_gather(ex)
                    f5_xet(ex)
                    for g in range(4):
                        wp = loaded.pop(pi)
                        issue_piece(pi + 2)
                        pi += 1
                        for f in range(4):
                            fc = g * 4 + f
                            pa = PA.next(); pu = PUu.next()
                            for (wsel, pp) in ((0, pa), (8, pu)):
                                for kc in range(8):
                                    P(lambda e: e.matmul(pp[:, 0:512], lhsT=wp[:, wsel + kc, f * 128:(f + 1) * 128], rhs=XET[:, kc, 0:512], start=(kc == 0), stop=(kc == 7)), r=[wp, XET], w=[pp])
                            sa = SA.next()
                            A(lambda e: e.activation(out=sa[:, 0:512], in_=pa[:, 0:512], func=AF.Silu), r=[pa], w=[sa])
                            V(lambda e: e.tensor_tensor(out=HMT[:, fc, 0:512], in0=sa[:, 0:512], in1=pu[:, 0:512], op=ALU.mult), r=[sa, pu], w=[HMT])
                            if need_ctx:
                                for (wsel, c0_) in ((0, 0), (8, 32)):
                                    for kc in range(8):
                                        P(lambda e: e.matmul(PB[7][:, c0_:c0_ + 32], lhsT=wp[:, wsel + kc, f * 128:(f + 1) * 128], rhs=XET[:, kc, 512:544], start=(kc == 0), stop=(kc == 7)), r=[wp, XET], w=[PB[7]])
                                A(lambda e: e.activation(out=sa[:, 512:544], in_=PB[7][:, 0:32], func=AF.Silu), r=[PB[7]], w=[sa])
                                V(lambda e: e.tensor_tensor(out=HMT[:, fc, 512:544], in0=sa[:, 512:544], in1=PB[7][:, 32:64], op=ALU.mult), r=[sa, PB[7]], w=[HMT])
                    wpd = []
                    wpd.append(loaded.pop(pi))
                    issue_piece(pi + 2)
                    pi += 1
                    wpd.append(loaded.pop(pi))
                    pi += 1
                    cur_tokens = {}
                    for (s_, mw, off) in slot_tiles:
                        for hf_ in range(2):
                            py = PYr.next()
                            for fc in range(16):
                                P(lambda e: e.matmul(py[0:mw, 0:512], lhsT=HMT[:, fc, off:off + mw], rhs=wpd[hf_][:, fc, :], start=(fc == 0), stop=(fc == 15)), r=[HMT, wpd[hf_]], w=[py])
                            A(lambda e: e.activation(out=YS[s_][0:mw, hf_ * 512:(hf_ + 1) * 512], in_=py[0:mw, 0:512], func=AF.Identity, scale=GS[0:mw, ex, s_:s_ + 1]), r=[py, GS], w=[YS[s_]])
                        MACC.writes = dict(prev_tokens)
                        MACC.readers = {}
                        k.dma("pool", lambda e: e.indirect_dma_start(out=MACC[:, :], out_offset=bass.IndirectOffsetOnAxis(ap=TOKI[0:mw, ex, s_:s_ + 1], axis=0), in_=YS[s_][0:mw, :], in_offset=None, compute_op=ALU.add), reads=[TOKI, YS[s_]], writes=[MACC], sembuf=YS[s_])
                        _merge(cur_tokens, MACC.writes)
                    prev_tokens = cur_tokens
                    MACC.writes = dict(prev_tokens)
                    issue_piece(pi + 1)
            k.barrier()

            with ExitStack() as es4:
                es4.enter_context(nc.named_scope('F6_%d' % l))
                G2R = [k.sbuf("fg2r%d" % j, [128, D], F32, es4) for j in range(len(sets))]
                for j in range(len(sets)):
                    bcast_row(G2R[j], MODd[j, 5 * D:6 * D])
                XL = Ring([k.sbuf("gxt%d" % i, [128, D], F32, es4) for i in range(3)])
                ML = Ring([k.sbuf("gml%d" % i, [128, D], F32, es4) for i in range(3)])
                XO = Ring([k.sbuf("gxo%d" % i, [128, D], F32, es4) for i in range(3)])
                junk = k.sbuf("gjunk", [128, D], BF16, es4)
                SSQR = Ring([k.sbuf("gssq%d" % i, [128, 1], F32, es4) for i in range(2)])
                RTR = Ring([k.sbuf("grt%d" % i, [128, 1], F32, es4) for i in range(2)])
                if last:
                    FNW = k.sbuf("gfnw", [128, D], F32, es4)
                    LD(FNW[:], fnw_d.t.partition_broadcast(128), FNW)
                for ti in tiles_out:
                    j = 1 if ti < 2 else 0
                    xt = XL.next(); ml = ML.next(); xo = XO.next()
                    LD(xt[:], xs_oth[ti * 128:(ti + 1) * 128, :], xt)
                    LD(ml[:], MACC[ti * 128:(ti + 1) * 128, :], ml)
                    V(lambda e: e.tensor_tensor(out=ml[:], in0=ml[:], in1=G2R[j][:], op=ALU.mult), r=[ml, G2R[j]], w=[ml])
                    if not last:
                        V(lambda e: e.tensor_tensor(out=xo[:], in0=ml[:], in1=xt[:], op=ALU.add), r=[ml, xt], w=[xo])
                        ST(xs_cur[ti * 128:(ti + 1) * 128, :], xo[:], xo)
                    else:
                        V(lambda e: e.tensor_tensor(out=xt[:], in0=ml[:], in1=xt[:], op=ALU.add), r=[ml, xt], w=[xt])
                        rms_to_xn(xt, ml, junk, SSQR.next(), RTR.next())
                        V(lambda e: e.tensor_tensor(out=xo[:], in0=ml[:], in1=FNW[:], op=ALU.mult), r=[ml, FNW], w=[xo])
                        ST(out_d[(ti - 2) * 128:(ti - 1) * 128, :], xo[:], xo)
        k.barrier()
    k.finish()
    return nc, k


_CACHE = {}


def kernel(**inputs):
    n = 8
    if "nc" not in _CACHE:
        _CACHE["nc"] = build_program()[0]
    nc = _CACHE["nc"]
    shared = {kk: np.ascontiguousarray(v, dtype=np.float32) for kk, v in inputs.items() if kk not in ("x", "c", "ctx")}
    in_maps = []
    for b in range(n):
        m = dict(shared)
        m["x"] = np.ascontiguousarray(inputs["x"][b], dtype=np.float32)
        m["c"] = np.ascontiguousarray(inputs["c"][b], dtype=np.float32)
        m["ctx"] = np.ascontiguousarray(inputs["ctx"][b], dtype=np.float32)
        in_maps.append(m)
    res = run_bass_kernel_spmd(nc, in_maps, core_ids=list(range(n)))
    return np.stack([np.asarray(r["out"], dtype=np.float32) for r in res.results], axis=0)
```

```python
import numpy as np
from contextlib import ExitStack
import concourse.bass as bass
import concourse.mybir as mybir
from concourse.bass_utils import run_bass_kernel_spmd

F32 = mybir.dt.float32
BF16 = mybir.dt.bfloat16
I32 = mybir.dt.int32
AF = mybir.ActivationFunctionType
ALU = mybir.AluOpType

D = 1024
SEQ = 4096
CTX = 256
DEPTH = 2
NT = 34
NROW = NT * 128
NE = 16
FF = 2048
N_IN = 9232
NST = 3088
EPS = 1e-6
ZR_OG, ZR_U, ZR_ZS, ZR_GM, ZR_W = 0, 1024, 1536, 2560, 5632


class Buf:
    __slots__ = ("t", "name", "writes", "readers", "dsem", "dcnt")

    def __init__(self, t, name):
        self.t = t
        self.name = name
        self.writes = {}
        self.readers = {}
        self.dsem = None
        self.dcnt = 0

    def __getitem__(self, key):
        return self.t[key]


def _merge(d, s):
    for k, v in s.items():
        if d.get(k, 0) < v:
            d[k] = v


class KB:
    def __init__(self, nc):
        self.nc = nc
        self.es = ExitStack()
        self.eng = {"pe": nc.tensor, "dve": nc.vector, "act": nc.scalar, "pool": nc.gpsimd, "sp": nc.sync}
        self.semh = {}
        self.cnt = {}
        self.seen = {k: {} for k in self.eng}
        for k in self.eng:
            self.semh[k] = self.es.enter_context(nc.semaphore("s_" + k))
            self.cnt[k] = 0
        self.dfree = []
        self.dcount = []
        self.nop = 0

    def sbuf(self, name, shape, dtype, es=None):
        self.uid = getattr(self, "uid", 0) + 1
        name = "%s_u%d" % (name, self.uid)
        t = (es or self.es).enter_context(self.nc.sbuf_tensor(name, list(shape), dtype))
        b = Buf(t, name)
        if es is not None:
            es.callback(self._release, b)
        return b

    def _release(self, b):
        if b.dsem is not None:
            self.dfree.append(b.dsem)
            b.dsem = None

    def _dsem_for(self, b):
        if b.dsem is None:
            if self.dfree:
                b.dsem = self.dfree.pop()
            else:
                idx = len(self.dcount)
                h = self.es.enter_context(self.nc.semaphore("dq%d" % idx))
                self.dcount.append(0)
                self.semh[("d", idx)] = h
                b.dsem = idx
        return b.dsem

    def psum(self, name, shape, dtype, es=None):
        t = (es or self.es).enter_context(self.nc.psum_tensor(name, list(shape), dtype))
        return Buf(t, name)

    def dram(self, name, shape, dtype, kind="Internal"):
        t = self.nc.dram_tensor(name, list(shape), dtype, kind=kind)
        return Buf(t.ap(), name)

    def _wait(self, e, need):
        eng = self.eng[e]
        seen = self.seen[e]
        for key, v in need.items():
            if seen.get(key, 0) < v:
                eng.wait_ge(self.semh[key], v)
                seen[key] = v

    def op(self, e, fn, reads=(), writes=()):
        raw = {}
        oth = {}
        for b in reads:
            _merge(raw, b.writes)
        for b in writes:
            _merge(oth, b.readers)
            _merge(oth, b.writes)
        need = dict(raw)
        for key, v in oth.items():
            if key == e:
                continue
            if need.get(key, 0) < v:
                need[key] = v
        if e == "pe":
            need.pop("pe", None)
        self._wait(e, need)
        ins = fn(self.eng[e])
        self.cnt[e] += 1
        ins.then_inc(self.semh[e], 1)
        tok = {e: self.cnt[e]}
        for b in reads:
            _merge(b.readers, tok)
        for b in writes:
            b.writes = dict(tok)
            b.readers = {}
        self.nop += 1
        return ins

    def dma(self, q, fn, reads=(), writes=(), sembuf=None):
        need = {}
        for b in reads:
            _merge(need, b.writes)
        for b in writes:
            _merge(need, b.readers)
            _merge(need, b.writes)
        self._wait(q, need)
        idx = self._dsem_for(sembuf)
        key = ("d", idx)
        ins = fn(self.eng[q])
        self.dcount[idx] += 16
        ins.then_inc(self.semh[key], 16)
        tok = {key: self.dcount[idx]}
        for b in reads:
            _merge(b.readers, tok)
        for b in writes:
            b.writes = dict(tok)
            b.readers = {}
        self.nop += 1
        return ins

    def barrier(self):
        need = {k: c for k, c in self.cnt.items() if c > 0}
        for idx, c in enumerate(self.dcount):
            if c > 0:
                need[("d", idx)] = c
        for e in self.eng:
            self._wait(e, dict(need))

    def finish(self):
        self.barrier()
        self.es.close()


class Ring:
    def __init__(self, bufs):
        self.bufs = bufs
        self.i = 0

    def next(self):
        b = self.bufs[self.i % len(self.bufs)]
        self.i += 1
        return b


def interleave(gens, skew=None):
    gens = list(gens)
    delay = {id(g): (skew[i] if skew else 0) for i, g in enumerate(gens)}
    while gens:
        for g in list(gens):
            if delay[id(g)] > 0:
                delay[id(g)] -= 1
                continue
            try:
                next(g)
            except StopIteration:
                gens.remove(g)


def build_program(layers=DEPTH, upto="all", dbg=False):
    nc = bass.Bass("TRN2", target_bir_lowering=False)
    k = KB(nc)
    ins_ = {}

    def din(name, shape):
        ins_[name] = k.dram(name, shape, F32, kind="ExternalInput")
        return ins_[name]

    x_d = din("x", [SEQ, D]); c_d = din("c", [D]); ctx_d = din("ctx", [CTX, D]); cctx_d = din("c_ctx", [D])
    ada_w_d = din("ada_w", [DEPTH, D, 6 * D]); ada_b_d = din("ada_b", [DEPTH, 6 * D])
    n1_d = din("norm1_w", [DEPTH, D]); n2_d = din("norm2_w", [DEPTH, D])
    w_in_d = din("w_in", [DEPTH, D, N_IN]); gb_d = din("mlstm_gate_b", [DEPTH, 16])
    mnw_d = din("mlstm_norm_w", [DEPTH, D]); wmo_d = din("w_mlstm_out", [DEPTH, D, D])
    cdw_d = din("conv_dw_w", [DEPTH, 31, 512]); cdb_d = din("conv_dw_b", [DEPTH, 512])
    clw_d = din("conv_ln_w", [DEPTH, 512]); clb_d = din("conv_ln_b", [DEPTH, 512])
    wco_d = din("w_conv_out", [DEPTH, 512, D])
    slw_d = din("sg_ln_w", [DEPTH, 512]); slb_d = din("sg_ln_b", [DEPTH, 512])
    sgw_d = din("sg_w", [DEPTH, 4, 128, 128]); sgb_d = din("sg_b", [DEPTH, 4, 128])
    wso_d = din("w_sg_out", [DEPTH, 512, D]); wo_d = din("w_o", [DEPTH, D, D])
    rw_d = din("router_w", [DEPTH, D, NE]); rb_d = din("router_b", [DEPTH, NE])
    wg_d = din("expert_w_gate", [DEPTH, NE, D, FF]); wu_d = din("expert_w_up", [DEPTH, NE, D, FF])
    wd_d = din("expert_w_down", [DEPTH, NE, FF, D]); fnw_d = din("final_norm_w", [D])
    out_d = k.dram("out", [SEQ, D], F32, kind="ExternalOutput")

    sk = "ExternalOutput" if dbg else "Internal"
    XA = k.dram("XA", [NROW, D], F32, kind=sk)
    XB = k.dram("XB", [NROW, D], F32, kind=sk)
    ZQ = k.dram("ZQ", [NT, 128, 3072], BF16, kind=sk)
    GGd = k.dram("GGd", [NT, 128, 32], F32, kind=sk)
    ZR = k.dram("ZR", [NT, 128, ZR_W], BF16, kind=sk)
    HD = [k.dram("HF", [NT, 128, D], F32, kind=sk), k.dram("HB", [NT, 128, D], F32, kind=sk)]
    H2d = k.dram("H2d", [NROW, D], BF16, kind=sk)
    MACC = k.dram("MACC", [NROW, D], F32, kind=sk)
    MODd = k.dram("MODd", [2, 6 * D], F32, kind=sk)
    WSd = k.dram("WSd", [2, 2, D], F32, kind=sk)
    dbg_d = {}

    def V(fn, r=(), w=()):
        return k.op("dve", fn, r, w)

    def A(fn, r=(), w=()):
        return k.op("act", fn, r, w)

    def P(fn, r=(), w=()):
        return k.op("pe", fn, r, w)

    def G(fn, r=(), w=()):
        return k.op("pool", fn, r, w)

    def LD(out_ap, in_ap, buf, q="sp"):
        return k.dma(q, lambda e: e.dma_start(out=out_ap, in_=in_ap), writes=[buf], sembuf=buf)

    def ST(out_ap, in_ap, buf, q="pool"):
        return k.dma(q, lambda e: e.dma_start(out=out_ap, in_=in_ap), reads=[buf], sembuf=buf)

    PB = [k.psum("pb%d" % i, [128, 512], F32) for i in range(8)]

    def pbf(i):
        return PB[i][:].bitcast(BF16)

    ONES32 = k.sbuf("ones32", [128, 128], F32)
    ID32 = k.sbuf("id32", [128, 128], F32)
    IDB = k.sbuf("idb", [128, 128], BF16)
    ONESB = k.sbuf("onesb", [128, 128], BF16)
    U32 = k.sbuf("u32", [128, 128], F32)
    L32 = k.sbuf("l32", [128, 128], F32)
    SUB = k.sbuf("sub", [128, 128], BF16)
    MASK4 = k.sbuf("mask4", [128, 2, 4, 128], BF16)
    MEAN32 = k.sbuf("mean32", [128, 128], F32)
    EPSC = k.sbuf("epsc", [128, 1], F32)
    MHALF = k.sbuf("mhalf", [128, 128], F32)
    XINIT = k.sbuf("xinit", [128, 4], F32)
    PIDX = k.sbuf("pidx", [128, 1], F32)
    PIDXI = k.sbuf("pidxi", [128, 1], I32)
    TMPC = k.sbuf("tmpc", [128, 128], F32)

    G(lambda e: e.memset(ONES32[:], 1.0), w=[ONES32])
    G(lambda e: e.memset(EPSC[:], EPS), w=[EPSC])
    G(lambda e: e.memset(MHALF[:], -0.5), w=[MHALF])
    G(lambda e: e.memset(MEAN32[:], 1.0 / 512.0), w=[MEAN32])
    G(lambda e: e.affine_select(out=ID32[:], in_=ONES32[:], pattern=[[-1, 128]], compare_op=ALU.is_equal, fill=0.0, base=0, channel_multiplier=1), r=[ONES32], w=[ID32])
    G(lambda e: e.affine_select(out=U32[:], in_=ONES32[:], pattern=[[1, 128]], compare_op=ALU.is_ge, fill=0.0, base=0, channel_multiplier=-1), r=[ONES32], w=[U32])
    G(lambda e: e.affine_select(out=L32[:], in_=ONES32[:], pattern=[[-1, 128]], compare_op=ALU.is_ge, fill=0.0, base=0, channel_multiplier=1), r=[ONES32], w=[L32])
    G(lambda e: e.affine_select(out=TMPC[:], in_=ONES32[:], pattern=[[1, 128]], compare_op=ALU.is_gt, fill=0.0, base=0, channel_multiplier=-1), r=[ONES32], w=[TMPC])
    V(lambda e: e.tensor_copy(out=SUB[:], in_=TMPC[:]), r=[TMPC], w=[SUB])
    V(lambda e: e.tensor_copy(out=IDB[:], in_=ID32[:]), r=[ID32], w=[IDB])
    V(lambda e: e.tensor_copy(out=ONESB[:], in_=ONES32[:]), r=[ONES32], w=[ONESB])
    for h in range(4):
        V(lambda e: e.tensor_copy(out=MASK4[:, 0, h, :], in_=U32[:]), r=[U32], w=[MASK4])
        V(lambda e: e.tensor_copy(out=MASK4[:, 1, h, :], in_=L32[:]), r=[L32], w=[MASK4])
    G(lambda e: e.iota(PIDXI[:], pattern=[[0, 1]], base=0, channel_multiplier=1), w=[PIDXI])
    V(lambda e: e.tensor_copy(out=PIDX[:], in_=PIDXI[:]), r=[PIDXI], w=[PIDX])

    k.dma("sp", lambda e: e.dma_start(out=XA[0:CTX, :], in_=ctx_d[:, :]), sembuf=XINIT)
    for i in range(4):
        k.dma("sp", lambda e: e.dma_start(out=XA[CTX + i * 1024:CTX + (i + 1) * 1024, :], in_=x_d[i * 1024:(i + 1) * 1024, :]), sembuf=XINIT)

    CROW = k.sbuf("crow", [16, 128], F32)
    CSB = k.sbuf("csb", [128, 8, 2], BF16)
    LD(CROW[0:8, :], c_d.t.rearrange("(r p) -> r p", p=128), CROW)
    LD(CROW[8:16, :], cctx_d.t.rearrange("(r p) -> r p", p=128), CROW)
    P(lambda e: e.transpose(out=PB[0][:, 0:16], in_=CROW[:, :], identity=ID32[0:16, 0:16]), r=[CROW, ID32], w=[PB[0]])
    for j in range(2):
        A(lambda e: e.activation(out=CSB[:, :, j], in_=PB[0][:, j * 8:(j + 1) * 8], func=AF.Silu), r=[PB[0]], w=[CSB])

    FT = k.sbuf("ft", [128, 4, 8, 2], F32)
    GBR = k.sbuf("gbr", [128, 16], F32)

    def bcast_row(dst, src_ap):
        LD(dst[:], src_ap.partition_broadcast(128), dst)

    xs_cur, xs_oth = XA, XB

    for l in range(layers):
        need_ctx = l < DEPTH - 1
        last = l == DEPTH - 1
        with ExitStack() as es:
            es.enter_context(nc.named_scope('A%d' % l))
            AWR = Ring([k.sbuf("aw%d" % i, [128, 8, 512], BF16, es) for i in range(2)])
            MODROW = k.sbuf("modrow", [2, 6 * D], F32, es)
            WSROW = k.sbuf("wsrow", [2, 2, D], F32, es)
            NROWS = k.sbuf("nrows", [2, 2, D], F32, es)
            for g in range(12):
                aw = AWR.next()
                k.dma("pool", lambda e: e.dma_start(out=aw[:], in_=ada_w_d[l, :, g * 512:(g + 1) * 512].rearrange("(kc p) n -> p kc n", p=128)), writes=[aw], sembuf=aw)
                for kc in range(8):
                    P(lambda e: e.matmul(PB[0][0:2, 0:512], lhsT=CSB[:, kc, :], rhs=aw[:, kc, :], start=(kc == 0), stop=(kc == 7)), r=[CSB, aw], w=[PB[0]])
                V(lambda e: e.tensor_copy(out=MODROW[0:2, g * 512:(g + 1) * 512], in_=PB[0][0:2, 0:512]), r=[PB[0]], w=[MODROW])
            BROW = k.sbuf("brow", [2, 6 * D], F32, es)
            LD(BROW[:], ada_b_d[l, :].partition_broadcast(2), BROW)
            V(lambda e: e.tensor_tensor(out=MODROW[:], in0=MODROW[:], in1=BROW[:], op=ALU.add), r=[MODROW, BROW], w=[MODROW])
            LD(NROWS[:, 0, :], n1_d[l, :].partition_broadcast(2), NROWS)
            LD(NROWS[:, 1, :], n2_d[l, :].partition_broadcast(2), NROWS)
            for w_, sc_set in ((0, 1), (1, 4)):
                V(lambda e: e.tensor_scalar(out=WSROW[:, w_, :], in0=MODROW[:, sc_set * D:(sc_set + 1) * D], scalar1=1.0, scalar2=None, op0=ALU.add), r=[MODROW], w=[WSROW])
                V(lambda e: e.tensor_tensor(out=WSROW[:, w_, :], in0=WSROW[:, w_, :], in1=NROWS[:, w_, :], op=ALU.mult), r=[WSROW, NROWS], w=[WSROW])
            srcs = [(WSROW, lambda c: WSROW[0:2, 0, c * 128:(c + 1) * 128]), (MODROW, lambda c: MODROW[0:2, 0 * D + c * 128:0 * D + (c + 1) * 128]),
                    (WSROW, lambda c: WSROW[0:2, 1, c * 128:(c + 1) * 128]), (MODROW, lambda c: MODROW[0:2, 3 * D + c * 128:3 * D + (c + 1) * 128])]
            for s, (sb, fn) in enumerate(srcs):
                for c in range(8):
                    P(lambda e: e.transpose(out=PB[1][:, (s * 8 + c) * 2:(s * 8 + c) * 2 + 2], in_=fn(c), identity=ID32[0:2, 0:2]), r=[sb, ID32], w=[PB[1]])
            V(lambda e: e.tensor_copy(out=FT[:].rearrange("p s c j -> p (s c j)"), in_=PB[1][:, 0:64]), r=[PB[1]], w=[FT])
            LD(GBR[:], gb_d[l, :].partition_broadcast(128), GBR)
            ST(MODd[:, :], MODROW[:], MODROW)
            ST(WSd[:, :, :], WSROW[:], WSROW)
        k.barrier()

        tiles_all = list(range(NT))
        tiles_out = list(range(NT)) if need_ctx else list(range(2, NT))

        def rsqrt_pool(out_ap, in_ap, scale, w, rbufs, wbuf):
            G(lambda e: e.tensor_scalar(out=out_ap, in0=in_ap, scalar1=scale, scalar2=EPS, op0=ALU.mult, op1=ALU.add), r=rbufs, w=[wbuf])
            G(lambda e: e.tensor_tensor(out=out_ap, in0=out_ap, in1=MHALF[0:out_ap.shape[0], 0:w], op=ALU.pow), r=[wbuf, MHALF], w=[wbuf])

        def rms_to_xn(xt, xn, junk, ssq, rt):
            A(lambda e: e.activation(out=junk[:], in_=xt[:], func=AF.Square, accum_out=ssq[:]), r=[xt], w=[junk, ssq])
            rsqrt_pool(rt[:], ssq[:], 1.0 / D, 1, [ssq], rt)
            V(lambda e: e.tensor_scalar(out=xn[:], in0=xt[:], scalar1=rt[:, 0:1], scalar2=None, op0=ALU.mult), r=[xt, rt], w=[xn])

        def xn_to_hT(xn, hT, j, s_ws, s_sh, pbs=None):
            if pbs is None:
                pbs = (PB[0], PB[1])
            for hh in range(2):
                pb = pbs[hh]
                for q in range(4):
                    c = hh * 4 + q
                    P(lambda e: e.transpose(out=pb[:, q * 128:(q + 1) * 128], in_=xn[:, c * 128:(c + 1) * 128], identity=ID32[:]), r=[xn, ID32], w=[pb])
                for q in range(4):
                    c = hh * 4 + q
                    A(lambda e: e.activation(out=hT[:, c, :], in_=pb[:, q * 128:(q + 1) * 128], func=AF.Identity, bias=FT[:, s_sh, c, j:j + 1], scale=FT[:, s_ws, c, j:j + 1]), r=[pb, FT], w=[hT])

        for part in range(2):
          with ExitStack() as es:
            es.enter_context(nc.named_scope('B%d_%d' % (l, part)))
            wc0, wc1 = (0, NST) if part == 0 else (NST, N_IN)
            WIN = k.sbuf("win%d" % part, [128, 8, wc1 - wc0], BF16, es)
            cc = wc0
            while cc < wc1:
                ce = min(cc + 1024, wc1)
                k.dma("pool", lambda e: e.dma_start(out=WIN[:, :, cc - wc0:ce - wc0], in_=w_in_d[l, :, cc:ce].rearrange("(kc p) n -> p kc n", p=128)), writes=[WIN], sembuf=WIN)
                cc = ce
            junk = k.sbuf("bjunk", [128, D], BF16, es)
            btiles = tiles_all if part == 0 else tiles_out
            BW = []
            for s_ in range(2):
                W = {}
                W["pre"] = []
                for pp in range(2):
                    W["pre"].append({"xt": k.sbuf("bxt%d%d" % (s_, pp), [128, D], F32, es), "xn": k.sbuf("bxn%d%d" % (s_, pp), [128, D], F32, es),
                                     "ssq": k.sbuf("bssq%d%d" % (s_, pp), [128, 1], F32, es), "rt": k.sbuf("brt%d%d" % (s_, pp), [128, 1], F32, es),
                                     "hT": k.sbuf("bht%d%d" % (s_, pp), [128, 8, 128], BF16, es)})
                if part == 0:
                    W["qkv"] = k.sbuf("bqkv%d" % s_, [128, 3072], BF16, es)
                    W["GT"] = k.sbuf("bgt%d" % s_, [128, 16], F32, es)
                    W["SP"] = k.sbuf("bsp%d" % s_, [128, 8], F32, es)
                    W["TM"] = k.sbuf("btm%d" % s_, [128, 16], F32, es)
                    W["gg"] = k.sbuf("bgg%d" % s_, [128, 32], F32, es)
                else:
                    W["zr"] = k.sbuf("bzr%d" % s_, [128, ZR_W], BF16, es)
                    W["SG"] = k.sbuf("bsg%d" % s_, [128, 512], F32, es)
                    W["SIG"] = k.sbuf("bsig%d" % s_, [128, 512], F32, es)
                W["PR"] = Ring([PB[2 + 2 * s_], PB[3 + 2 * s_]] + ([PB[6 + s_]] if part == 1 else []))
                W["pt"] = PB[s_]
                W["pg"] = PB[6 + s_]
                BW.append(W)

            def b_prep1(ti, W, pp):
                pr = W["pre"][pp]
                LD(pr["xt"][:], xs_cur[ti * 128:(ti + 1) * 128, :], pr["xt"])
                rms_to_xn(pr["xt"], pr["xn"], junk, pr["ssq"], pr["rt"])

            def b_prep2(ti, W, pp):
                pr = W["pre"][pp]
                xn_to_hT(pr["xn"], pr["hT"], 1 if ti < 2 else 0, 0, 1, pbs=(W["pt"], W["pt"]))

            def b_tile(ti, W, pp, nxt_ti):
                hT, PR, pg = W["pre"][pp]["hT"], W["PR"], W["pg"]
                gi = 0
                if part == 0:
                    qkv, GT, SP_, TM, gg = W["qkv"], W["GT"], W["SP"], W["TM"], W["gg"]
                else:
                    zr, SG_, SIG = W["zr"], W["SG"], W["SIG"]
                for g in (range(7) if part == 0 else range(7, 19)):
                    c0 = g * 512
                    c1 = c0 + 512
                    if g == 6:
                        c1 = NST
                    if g >= 7:
                        c0 = NST + (g - 7) * 512
                        c1 = c0 + 512
                    w = c1 - c0
                    ps = PR.next()
                    for kc in range(8):
                        P(lambda e: e.matmul(ps[:, 0:w], lhsT=hT[:, kc, :], rhs=WIN[:, kc, c0 - wc0:c1 - wc0], start=(kc == 0), stop=(kc == 7)), r=[hT, WIN], w=[ps])
                    if g < 6:
                        sc = 0.0625 if g in (2, 3) else 1.0
                        if g % 2 == 0:
                            A(lambda e: e.activation(out=qkv[:, c0:c1], in_=ps[:, 0:512], func=AF.Copy, scale=sc), r=[ps], w=[qkv])
                        else:
                            V(lambda e: e.tensor_scalar(out=qkv[:, c0:c1], in0=ps[:, 0:512], scalar1=sc, scalar2=None, op0=ALU.mult), r=[ps], w=[qkv])
                    elif g == 6:
                        V(lambda e: e.tensor_tensor(out=GT[:], in0=ps[:, 0:16], in1=GBR[:], op=ALU.add), r=[ps, GBR], w=[GT])
                    else:
                        r0 = (g - 7) * 512
                        if r0 < 1024:
                            A(lambda e: e.activation(out=zr[:, ZR_OG + r0:ZR_OG + r0 + 512], in_=ps[:, 0:512], func=AF.Sigmoid), r=[ps], w=[zr])
                        elif r0 == 1024:
                            V(lambda e: e.tensor_copy(out=SG_[:], in_=ps[:, 0:512]), r=[ps], w=[SG_])
                        elif r0 == 1536:
                            A(lambda e: e.activation(out=SIG[:], in_=ps[:, 0:512], func=AF.Sigmoid), r=[ps], w=[SIG])
                            V(lambda e: e.tensor_tensor(out=zr[:, ZR_U:ZR_U + 512], in0=SG_[:], in1=SIG[:], op=ALU.mult), r=[SG_, SIG], w=[zr])
                        elif r0 < 3072:
                            o0 = ZR_ZS + (r0 - 2048)
                            V(lambda e: e.tensor_tensor(out=SG_[:], in0=ps[:, 0:512], in1=ps[:, 0:512], op=ALU.mult), r=[ps], w=[SG_]) if False else None
                            A(lambda e: e.activation(out=SG_[:], in_=ps[:, 0:512], func=AF.Square), r=[ps], w=[SG_])
                            V(lambda e: e.tensor_scalar(out=SG_[:], in0=SG_[:], scalar1=0.044715, scalar2=1.0, op0=ALU.mult, op1=ALU.add), r=[SG_], w=[SG_])
                            V(lambda e: e.tensor_tensor(out=SG_[:], in0=SG_[:], in1=ps[:, 0:512], op=ALU.mult), r=[SG_, ps], w=[SG_])
                            A(lambda e: e.activation(out=SIG[:], in_=SG_[:], func=AF.Sigmoid, scale=1.5957691216057308), r=[SG_], w=[SIG])
                            V(lambda e: e.tensor_tensor(out=zr[:, o0:o0 + 512], in0=SIG[:], in1=ps[:, 0:512], op=ALU.mult), r=[SIG, ps], w=[zr])
                        else:
                            o0 = ZR_GM + (r0 - 3072)
                            A(lambda e: e.activation(out=zr[:, o0:o0 + 512], in_=ps[:, 0:512], func=AF.Sigmoid), r=[ps], w=[zr])
                    gi += 1
                    if nxt_ti is not None and gi == 1:
                        b_prep1(nxt_ti, W, 1 - pp)
                    if nxt_ti is not None and gi == (4 if part == 0 else 7):
                        b_prep2(nxt_ti, W, 1 - pp)
                    yield
                if part == 1:
                    ST(ZR[ti, :, :], zr[:], zr)
                    yield
                    return
                for dd in range(2):
                    A(lambda e: e.activation(out=SP_[:, dd * 4:(dd + 1) * 4], in_=GT[:, dd * 8 + 4:dd * 8 + 8], func=AF.Exp, scale=-1.0), r=[GT], w=[SP_])
                yield
                A(lambda e: e.activation(out=SP_[:], in_=SP_[:], func=AF.Ln, bias=1.0, scale=1.0), r=[SP_], w=[SP_])
                yield
                P(lambda e: e.matmul(pg[:, 0:4], lhsT=U32[:], rhs=SP_[:, 0:4], start=True, stop=True), r=[U32, SP_], w=[pg])
                P(lambda e: e.matmul(pg[:, 4:8], lhsT=L32[:], rhs=SP_[:, 4:8], start=True, stop=True), r=[L32, SP_], w=[pg])
                P(lambda e: e.matmul(pg[:, 8:16], lhsT=ONES32[:], rhs=SP_[:, 0:8], start=True, stop=True), r=[ONES32, SP_], w=[pg])
                yield
                A(lambda e: e.activation(out=gg[:, 0:8], in_=pg[:, 0:8], func=AF.Exp, scale=-1.0), r=[pg], w=[gg])
                for dd in range(2):
                    V(lambda e: e.tensor_tensor(out=TM[:, dd * 4:(dd + 1) * 4], in0=GT[:, dd * 8:dd * 8 + 4], in1=pg[:, dd * 4:(dd + 1) * 4], op=ALU.add), r=[GT, pg], w=[TM])
                yield
                A(lambda e: e.activation(out=gg[:, 8:16], in_=TM[:, 0:8], func=AF.Exp), r=[TM], w=[gg])
                V(lambda e: e.tensor_tensor(out=TM[:, 8:16], in0=TM[:, 0:8], in1=pg[:, 8:16], op=ALU.subtract), r=[TM, pg], w=[TM])
                yield
                A(lambda e: e.activation(out=gg[:, 16:24], in_=TM[:, 8:16], func=AF.Exp), r=[TM], w=[gg])
                A(lambda e: e.activation(out=gg[:, 24:32], in_=pg[:, 8:16], func=AF.Exp, scale=-1.0), r=[pg], w=[gg])
                yield
                ST(ZQ[ti, :, :], qkv[:], qkv)
                ST(GGd[ti, :, :], gg[:], gg)
                yield

            def b_stream(s_):
                mine = btiles[s_::2]
                b_prep1(mine[0], BW[s_], 0)
                b_prep2(mine[0], BW[s_], 0)
                yield
                for n_, ti in enumerate(mine):
                    yield from b_tile(ti, BW[s_], n_ % 2, mine[n_ + 1] if n_ + 1 < len(mine) else None)

            interleave([b_stream(0), b_stream(1)], skew=[0, 9 if part == 0 else 8])
          k.barrier()
        if upto == "B" and l == layers - 1:
            break

        with ExitStack() as es:
            es.enter_context(nc.named_scope('C%d' % l))
            Cst = []
            for dd in range(2):
                c32 = k.sbuf("c32_%d" % dd, [128, 2, 4, 257], F32, es)
                cbf = k.sbuf("cbf_%d" % dd, [128, 2, 4, 257], BF16, es)
                G(lambda e: e.memset(c32[:], 0.0), w=[c32])
                G(lambda e: e.memset(cbf[:], 0.0), w=[cbf])
                Cst.append((c32, cbf))
            order = [tiles_all, [1, 0] + list(range(NT - 1, 1, -1))]
            SW = []
            for dd in range(2):
                W = {"q": k.sbuf("sq%d" % dd, [128, D], BF16, es), "kk": k.sbuf("sk%d" % dd, [128, D], BF16, es),
                     "qs": k.sbuf("sqs%d" % dd, [128, D], BF16, es), "ks": k.sbuf("sks%d" % dd, [128, D], BF16, es),
                     "kst": k.sbuf("skst%d" % dd, [128, 8, 128], BF16, es), "dn": k.sbuf("sdn%d" % dd, [128, 8], F32, es),
                     "ho": [k.sbuf("sho%d_%d" % (dd, i), [128, D], F32, es) for i in range(2)], "par": []}
                for pp in range(2):
                    va = k.sbuf("sv%d_%d" % (dd, pp), [128, 4, 257], BF16, es)
                    G(lambda e: e.memset(va[:], 1.0), w=[va])
                    W["par"].append({"va": va, "gg": k.sbuf("sg%d_%d" % (dd, pp), [128, 32], F32, es),
                                     "kss": k.sbuf("skss%d_%d" % (dd, pp), [128, D], BF16, es),
                                     "qst": k.sbuf("sqst%d_%d" % (dd, pp), [128, 8, 128], BF16, es),
                                     "stm": k.sbuf("sst%d_%d" % (dd, pp), [128, 4, 128], BF16, es)})
                W["T"] = PB[dd]
                W["S"] = PB[dd]
                W["PN"] = Ring([PB[3 + dd]])
                W["PU"] = Ring([PB[5 + dd], PB[2] if dd == 0 else PB[7]])
                SW.append(W)

            def scan_stage1(dd, ti, pp):
                W = SW[dd]
                P_ = W["par"][pp]
                q, kk, qs, ks, kst = W["q"], W["kk"], W["qs"], W["ks"], W["kst"]
                va, gg, kss, qst, stm = P_["va"], P_["gg"], P_["kss"], P_["qst"], P_["stm"]
                T, S = W["T"], W["S"]
                Tb = T[:].bitcast(BF16)
                LD(gg[:], GGd[ti, :, :], gg)
                LD(q[:], ZQ[ti, :, 0:1024], q)
                LD(kk[:], ZQ[ti, :, 1024:2048], kk)
                LD(va[:, :, 0:256], ZQ[ti, :, 2048:3072].rearrange("p (h d) -> p h d", h=4), va)
                yield
                for h in range(4):
                    hs = slice(h * 256, (h + 1) * 256)
                    A(lambda e: e.activation(out=qs[:, hs], in_=q[:, hs], func=AF.Identity, scale=gg[:, dd * 4 + h:dd * 4 + h + 1]), r=[q, gg], w=[qs])
                    V(lambda e: e.tensor_scalar(out=ks[:, hs], in0=kk[:, hs], scalar1=gg[:, 8 + dd * 4 + h:8 + dd * 4 + h + 1], scalar2=None, op0=ALU.mult), r=[kk, gg], w=[ks])
                yield
                for c in range(8):
                    P(lambda e: e.transpose(out=Tb[:, c * 128:(c + 1) * 128], in_=qs[:, c * 128:(c + 1) * 128], identity=IDB[:]), r=[qs, IDB], w=[T])
                A(lambda e: e.activation(out=qst[:].rearrange("p c t -> p (c t)"), in_=Tb[:, 0:1024], func=AF.Copy), r=[T], w=[qst])
                yield
                for h in range(4):
                    hs = slice(h * 256, (h + 1) * 256)
                    eng_ = "act" if h % 2 == 0 else "dve"
                    if eng_ == "act":
                        A(lambda e: e.activation(out=kss[:, hs], in_=kk[:, hs], func=AF.Identity, scale=gg[:, 16 + dd * 4 + h:16 + dd * 4 + h + 1]), r=[kk, gg], w=[kss])
                    else:
                        V(lambda e: e.tensor_scalar(out=kss[:, hs], in0=kk[:, hs], scalar1=gg[:, 16 + dd * 4 + h:16 + dd * 4 + h + 1], scalar2=None, op0=ALU.mult), r=[kk, gg], w=[kss])
                yield
                for c in range(8):
                    P(lambda e: e.transpose(out=Tb[:, c * 128:(c + 1) * 128], in_=ks[:, c * 128:(c + 1) * 128], identity=IDB[:]), r=[ks, IDB], w=[T])
                V(lambda e: e.tensor_copy(out=kst[:].rearrange("p c t -> p (c t)"), in_=Tb[:, 0:1024]), r=[T], w=[kst])
                yield
                for h in range(4):
                    for jj in range(2):
                        P(lambda e: e.matmul(S[:, h * 128:(h + 1) * 128], lhsT=kst[:, 2 * h + jj, :], rhs=qst[:, 2 * h + jj, :], start=(jj == 0), stop=(jj == 1)), r=[kst, qst], w=[S])
                V(lambda e: e.tensor_tensor(out=stm[:].rearrange("p h t -> p (h t)"), in0=S[:, 0:512], in1=MASK4[:, dd, :, :].rearrange("p h t -> p (h t)"), op=ALU.mult), r=[S, MASK4], w=[stm])
                yield

            def scan_stage2(dd, ti, pp, n_):
                W = SW[dd]
                P_ = W["par"][pp]
                va, gg, kss, qst, stm = P_["va"], P_["gg"], P_["kss"], P_["qst"], P_["stm"]
                c32, cbf = Cst[dd]
                dn = W["dn"]
                ho = W["ho"][n_ % 2]
                for h in range(4):
                    for jj in range(2):
                        pu = W["PU"].next()
                        P(lambda e: e.matmul(pu[:, 0:257], lhsT=kss[:, h * 256 + jj * 128:h * 256 + (jj + 1) * 128], rhs=va[:, h, :], start=True, stop=True), r=[kss, va], w=[pu])
                        V(lambda e: e.scalar_tensor_tensor(out=c32[:, jj, h, :], in0=c32[:, jj, h, :], scalar=gg[:, 24 + dd * 4 + h:24 + dd * 4 + h + 1], in1=pu[:, 0:257], op0=ALU.mult, op1=ALU.add), r=[c32, gg, pu], w=[c32])
                    pn = W["PN"].next()
                    P(lambda e: e.matmul(pn[:, 0:257], lhsT=stm[:, h, :], rhs=va[:, h, :], start=True, stop=False), r=[stm, va], w=[pn])
                    for jj in range(2):
                        P(lambda e: e.matmul(pn[:, 0:257], lhsT=qst[:, 2 * h + jj, :], rhs=cbf[:, jj, h, :], start=False, stop=(jj == 1)), r=[qst, cbf], w=[pn])
                    V(lambda e: e.tensor_scalar(out=dn[:, 4 + h:5 + h], in0=pn[:, 256:257], scalar1=-1.0, scalar2=1.0, op0=ALU.mult, op1=ALU.max), r=[pn], w=[dn])
                    V(lambda e: e.tensor_tensor(out=dn[:, h:h + 1], in0=dn[:, 4 + h:5 + h], in1=pn[:, 256:257], op=ALU.max), r=[dn, pn], w=[dn])
                    V(lambda e: e.reciprocal(out=dn[:, h:h + 1], in_=dn[:, h:h + 1]), r=[dn], w=[dn])
                    A(lambda e: e.activation(out=ho[:, h * 256:(h + 1) * 256], in_=pn[:, 0:256], func=AF.Identity, scale=dn[:, h:h + 1]), r=[pn, dn], w=[ho])
                    yield
                A(lambda e: e.activation(out=cbf[:, 0, :, :], in_=c32[:, 0, :, :], func=AF.Copy), r=[c32], w=[cbf])
                V(lambda e: e.tensor_copy(out=cbf[:, 1, :, :], in_=c32[:, 1, :, :]), r=[c32], w=[cbf])
                if ti in tiles_out:
                    ST(HD[dd][ti, :, :], ho[:], ho)
                yield

            def scan_stream(dd):
                od = order[dd]
                yield from scan_stage1(dd, od[0], 0)
                for n_, ti in enumerate(od):
                    if n_ + 1 < len(od):
                        yield from scan_stage1(dd, od[n_ + 1], (n_ + 1) % 2)
                    yield from scan_stage2(dd, ti, n_ % 2, n_)

            interleave([scan_stream(0), scan_stream(1)], skew=[0, 3])
        k.barrier()
        if upto == "C" and l == layers - 1:
            break

        with ExitStack() as es:
            es.enter_context(nc.named_scope('E%d' % l))
            WMO = k.sbuf("wmo", [128, 8, D], BF16, es)
            WCO = k.sbuf("wco", [128, 4, D], BF16, es)
            WSO = k.sbuf("wso", [128, 4, D], BF16, es)
            WO = k.sbuf("wo", [128, 8, D], BF16, es)
            for (wb, wd_) in ((WMO, wmo_d), (WCO, wco_d), (WSO, wso_d), (WO, wo_d)):
                k.dma("pool", lambda e: e.dma_start(out=wb[:], in_=wd_[l, :, :].rearrange("(kc p) n -> p kc n", p=128)), writes=[wb], sembuf=wb)
            ROWS = k.sbuf("erows", [64, 128], F32, es)
            CW = k.sbuf("ecw", [128, 4, 32], F32, es)
            SM = k.sbuf("esm", [128, 16], F32, es)
            for c in range(4):
                LD(ROWS[0:31, :], cdw_d[l, :, c * 128:(c + 1) * 128], ROWS)
                P(lambda e: e.transpose(out=PB[0][:, 0:31], in_=ROWS[0:31, :], identity=ID32[0:31, 0:31]), r=[ROWS, ID32], w=[PB[0]])
                V(lambda e: e.tensor_copy(out=CW[:, c, 0:31], in_=PB[0][:, 0:31]), r=[PB[0]], w=[CW])
            LD(ROWS[0:4, :], cdb_d[l, :].rearrange("(r p) -> r p", p=128), ROWS)
            LD(ROWS[4:8, :], clw_d[l, :].rearrange("(r p) -> r p", p=128), ROWS)
            LD(ROWS[8:12, :], clb_d[l, :].rearrange("(r p) -> r p", p=128), ROWS)
            LD(ROWS[12:16, :], sgb_d[l, :, :], ROWS)
            P(lambda e: e.transpose(out=PB[0][:, 0:16], in_=ROWS[0:16, :], identity=ID32[0:16, 0:16]), r=[ROWS, ID32], w=[PB[0]])
            V(lambda e: e.tensor_copy(out=SM[:], in_=PB[0][:, 0:16]), r=[PB[0]], w=[SM])
            SGT = k.sbuf("esgt", [128, 4, 128], BF16, es)
            SGL = k.sbuf("esgl", [128, 128], F32, es)
            for g in range(4):
                LD(SGL[:], sgw_d[l, g, :, :], SGL)
                P(lambda e: e.transpose(out=PB[0][:, 0:128], in_=SGL[:], identity=ID32[:]), r=[SGL, ID32], w=[PB[0]])
                V(lambda e: e.tensor_copy(out=SGT[:, g, :], in_=PB[0][:, 0:128]), r=[PB[0]], w=[SGT])
            MNW = k.sbuf("emnw", [128, D], F32, es)
            SLW = k.sbuf("eslw", [128, 512], F32, es)
            SLB = k.sbuf("eslb", [128, 512], F32, es)
            LD(MNW[:], mnw_d[l, :].partition_broadcast(128), MNW)
            LD(SLW[:], slw_d[l, :].partition_broadcast(128), SLW)
            LD(SLB[:], slb_d[l, :].partition_broadcast(128), SLB)
            G1R = [k.sbuf("eg1r%d" % j, [128, D], F32, es) for j in range(2 if need_ctx else 1)]
            for j in range(len(G1R)):
                bcast_row(G1R[j], MODd[j, 2 * D:3 * D])

            DIAG = k.sbuf("ediag", [128, 4, 31, 128], BF16, es)
            for c in range(4):
                for jt in range(31):
                    if (c * 31 + jt) % 2 == 0:
                        V(lambda e: e.tensor_scalar(out=DIAG[:, c, jt, :], in0=ID32[:], scalar1=CW[:, c, jt:jt + 1], scalar2=None, op0=ALU.mult), r=[ID32, CW], w=[DIAG])
                    else:
                        A(lambda e: e.activation(out=DIAG[:, c, jt, :], in_=ID32[:], func=AF.Identity, scale=CW[:, c, jt:jt + 1]), r=[ID32, CW], w=[DIAG])
            UPC = k.sbuf("eupc", [128, 4, 286], BF16, es)
            G(lambda e: e.memset(UPC[:], 0.0), w=[UPC])
            UB = k.sbuf("eub", [128, 512], BF16, es)
            junk = k.sbuf("ejunk", [128, D], BF16, es)
            TMP = k.sbuf("etmp", [128, 512], F32, es)
            WS = []
            for s_ in range(2):
                W = {}
                W["zr"] = k.sbuf("ezr%d" % s_, [128, ZR_W], BF16, es)
                W["hf"] = k.sbuf("ehf%d" % s_, [128, D], F32, es)
                W["xt"] = k.sbuf("ext%d" % s_, [128, D], F32, es)
                W["T1"] = k.sbuf("et1%d" % s_, [128, D], BF16, es)
                W["SS4"] = k.sbuf("ess4%d" % s_, [128, 4], F32, es)
                W["YN"] = k.sbuf("eyn%d" % s_, [128, D], BF16, es)
                W["YNT"] = k.sbuf("eynt%d" % s_, [128, 8, 128], BF16, es)
                W["MG"] = k.sbuf("emg%d" % s_, [128, D], F32, es)
                W["hb"] = W["MG"]
                W["UPL"] = k.sbuf("eupl%d" % s_, [128, 4, 2, 94], BF16, es)
                G(lambda e: e.memset(W["UPL"][:], 0.0), w=[W["UPL"]])
                W["CV"] = k.sbuf("ecv%d" % s_, [128, 4, 128], F32, es)
                W["CSQ"] = k.sbuf("ecsq%d" % s_, [128, 4, 128], F32, es)
                W["M2"] = k.sbuf("em2%d" % s_, [128, 128], F32, es)
                W["RSTD"] = k.sbuf("erstd%d" % s_, [128, 128], F32, es)
                W["CA"] = k.sbuf("eca%d" % s_, [128, 4, 128], BF16, es)
                W["ST2"] = k.sbuf("est2%d" % s_, [128, 4], F32, es)
                W["VNf"] = k.sbuf("evnf%d" % s_, [128, 512], F32, es)
                W["VNb"] = k.sbuf("evnb%d" % s_, [128, 512], BF16, es)
                W["SGO"] = k.sbuf("esgo%d" % s_, [128, 512], BF16, es)
                W["SGOT"] = k.sbuf("esgot%d" % s_, [128, 4, 128], BF16, es)
                W["MGB"] = k.sbuf("emgb%d" % s_, [128, D], BF16, es)
                W["MGT"] = k.sbuf("emgt%d" % s_, [128, 8, 128], BF16, es)
                W["pb"] = [PB[s_ * 4 + i] for i in range(4)]
                W["PY"] = Ring([PB[s_ * 4 + 2], PB[s_ * 4 + 3]])
                WS.append(W)

            if need_ctx:
                for ti in range(2):
                    LD(UB[:], ZR[ti, :, ZR_U:ZR_U + 512], UB)
                    for c in range(4):
                        P(lambda e: e.transpose(out=pbf(0)[:, c * 128:(c + 1) * 128], in_=UB[:, c * 128:(c + 1) * 128], identity=IDB[:]), r=[UB, IDB], w=[PB[0]])
                    V(lambda e: e.tensor_copy(out=UPC[:, :, 15 + ti * 128:15 + (ti + 1) * 128], in_=pbf(0)[:, 0:512].rearrange("p (c t) -> p c t", c=4)), r=[PB[0]], w=[UPC])

            def e_tile(ti, W):
                zr, hf, hb, xt = W["zr"], W["hf"], W["hb"], W["xt"]
                T1, SS4, YN, YNT, MG, UPL, CV, CSQ = W["T1"], W["SS4"], W["YN"], W["YNT"], W["MG"], W["UPL"], W["CV"], W["CSQ"]
                M2, RSTD, CA, ST2, VNf, VNb, SGO, SGOT, MGB, MGT = W["M2"], W["RSTD"], W["CA"], W["ST2"], W["VNf"], W["VNb"], W["SGO"], W["SGOT"], W["MGB"], W["MGT"]
                pT, pS = W["pb"][0], W["pb"][1]
                pTb = pT[:].bitcast(BF16)
                PY = W["PY"]
                j = 1 if ti < 2 else 0
                LD(zr[:], ZR[ti, :, :], zr)
                LD(hf[:], HD[0][ti, :, :], hf)
                LD(hb[:], HD[1][ti, :, :], hb)
                LD(xt[:], xs_cur[ti * 128:(ti + 1) * 128, :], xt)
                yield

                def gate_acc(py, half, goff, first, final):
                    gsl = zr[:, ZR_GM + goff + half * 512:ZR_GM + goff + (half + 1) * 512]
                    hs = slice(half * 512, (half + 1) * 512)
                    if first:
                        V(lambda e: e.tensor_tensor(out=MG[:, hs], in0=py[:, 0:512], in1=gsl, op=ALU.mult), r=[py, zr], w=[MG])
                    else:
                        V(lambda e: e.tensor_tensor(out=TMP[:], in0=py[:, 0:512], in1=gsl, op=ALU.mult), r=[py, zr], w=[TMP])
                        if final:
                            V(lambda e: e.tensor_tensor(out=MGB[:, hs], in0=MG[:, hs], in1=TMP[:], op=ALU.add), r=[MG, TMP], w=[MGB])
                        else:
                            V(lambda e: e.tensor_tensor(out=MG[:, hs], in0=MG[:, hs], in1=TMP[:], op=ALU.add), r=[MG, TMP], w=[MG])

                if ti >= 2:
                    for c in range(4):
                        P(lambda e: e.transpose(out=pTb[:, c * 128:(c + 1) * 128], in_=zr[:, ZR_U + c * 128:ZR_U + (c + 1) * 128], identity=IDB[:]), r=[zr, IDB], w=[pT])
                    for c in range(4):
                        A(lambda e: e.activation(out=UPL[:, c, :, 15:79], in_=pTb[:, c * 128:(c + 1) * 128].rearrange("p (r w) -> p r w", r=2), func=AF.Copy), r=[pT], w=[UPL])

                    def win(c, jt):
                        return UPL[:, c, :, jt:jt + 64]
                    ubuf = UPL
                else:
                    def win(c, jt):
                        return UPC[:, c, ti * 128 + jt:ti * 128 + jt + 128]
                    ubuf = UPC
                yield
                V(lambda e: e.tensor_tensor(out=hf[:], in0=hf[:], in1=hb[:], op=ALU.add), r=[hf, hb], w=[hf])
                for h in range(4):
                    A(lambda e: e.activation(out=junk[:, h * 256:(h + 1) * 256], in_=hf[:, h * 256:(h + 1) * 256], func=AF.Square, accum_out=SS4[:, h:h + 1]), r=[hf], w=[junk, SS4])
                G(lambda e: e.tensor_tensor(out=T1[:], in0=zr[:, ZR_OG:ZR_OG + 1024], in1=MNW[:], op=ALU.mult), r=[zr, MNW], w=[T1])
                yield
                for c in range(4):
                    for jt in range(31):
                        P(lambda e: e.matmul(pS[:, c * 128:(c + 1) * 128], lhsT=DIAG[:, c, jt, :], rhs=win(c, jt), start=(jt == 0), stop=(jt == 30)), r=[DIAG, ubuf], w=[pS])
                    if c % 2 == 1:
                        yield
                A(lambda e: e.activation(out=SS4[:], in_=SS4[:], func=AF.Sqrt, bias=EPSC[:], scale=1.0 / 256.0), r=[SS4, EPSC], w=[SS4])
                V(lambda e: e.reciprocal(out=SS4[:], in_=SS4[:]), r=[SS4], w=[SS4])
                for h in range(4):
                    hs = slice(h * 256, (h + 1) * 256)
                    V(lambda e: e.scalar_tensor_tensor(out=YN[:, hs], in0=hf[:, hs], scalar=SS4[:, h:h + 1], in1=T1[:, hs], op0=ALU.mult, op1=ALU.mult), r=[hf, SS4, T1], w=[YN])
                yield
                for c in range(4):
                    A(lambda e: e.activation(out=CV[:, c, :], in_=pS[:, c * 128:(c + 1) * 128], func=AF.Identity, bias=SM[:, c:c + 1], scale=1.0), r=[pS, SM], w=[CV])
                A(lambda e: e.activation(out=CSQ[:], in_=CV[:], func=AF.Square), r=[CV], w=[CSQ])
                yield
                for c in range(8):
                    P(lambda e: e.transpose(out=pTb[:, c * 128:(c + 1) * 128], in_=YN[:, c * 128:(c + 1) * 128], identity=IDB[:]), r=[YN, IDB], w=[pT])
                A(lambda e: e.activation(out=YNT[:].rearrange("p c t -> p (c t)"), in_=pTb[:, 0:1024], func=AF.Copy), r=[pT], w=[YNT])
                yield
                for c in range(4):
                    P(lambda e: e.matmul(pS[:, 0:128], lhsT=MEAN32[:], rhs=CV[:, c, :], start=(c == 0), stop=(c == 3)), r=[MEAN32, CV], w=[pS])
                for c in range(4):
                    P(lambda e: e.matmul(pS[:, 128:256], lhsT=MEAN32[:], rhs=CSQ[:, c, :], start=(c == 0), stop=(c == 3)), r=[MEAN32, CSQ], w=[pS])
                yield
                for half in range(2):
                    py = PY.next()
                    for kc in range(8):
                        P(lambda e: e.matmul(py[:, 0:512], lhsT=YNT[:, kc, :], rhs=WMO[:, kc, half * 512:(half + 1) * 512], start=(kc == 0), stop=(kc == 7)), r=[YNT, WMO], w=[py])
                    gate_acc(py, half, 0, True, False)
                    yield
                A(lambda e: e.activation(out=M2[:], in_=pS[:, 0:128], func=AF.Square), r=[pS], w=[M2])
                V(lambda e: e.tensor_tensor(out=RSTD[:], in0=pS[:, 128:256], in1=M2[:], op=ALU.subtract), r=[pS, M2], w=[RSTD])
                A(lambda e: e.activation(out=RSTD[:], in_=RSTD[:], func=AF.Sqrt, bias=EPSC[:], scale=1.0), r=[RSTD, EPSC], w=[RSTD])
                V(lambda e: e.reciprocal(out=RSTD[:], in_=RSTD[:]), r=[RSTD], w=[RSTD])
                V(lambda e: e.tensor_copy(out=M2[:], in_=pS[:, 0:128]), r=[pS], w=[M2])
                yield
                for c in range(4):
                    eng_ = "dve"
                    k.op(eng_, lambda e: e.tensor_tensor(out=CSQ[:, c, :], in0=CV[:, c, :], in1=M2[:], op=ALU.subtract), [CV, M2], [CSQ])
                    k.op(eng_, lambda e: e.tensor_tensor(out=CSQ[:, c, :], in0=CSQ[:, c, :], in1=RSTD[:], op=ALU.mult), [CSQ, RSTD], [CSQ])
                yield
                for c in range(4):
                    A(lambda e: e.activation(out=CA[:, c, :], in_=CSQ[:, c, :], func=AF.Silu, bias=SM[:, 8 + c:9 + c], scale=SM[:, 4 + c:5 + c]), r=[CSQ, SM], w=[CA])
                yield
                vv = zr[:, ZR_ZS + 512:ZR_ZS + 1024]
                A(lambda e: e.activation(out=junk[:, 0:512], in_=vv, func=AF.Identity, accum_out=ST2[:, 0:1]), r=[zr], w=[junk, ST2])
                A(lambda e: e.activation(out=junk[:, 512:1024], in_=vv, func=AF.Square, accum_out=ST2[:, 1:2]), r=[zr], w=[junk, ST2])
                yield
                for half in range(2):
                    py = PY.next()
                    for c in range(4):
                        P(lambda e: e.matmul(py[:, 0:512], lhsT=CA[:, c, :], rhs=WCO[:, c, half * 512:(half + 1) * 512], start=(c == 0), stop=(c == 3)), r=[CA, WCO], w=[py])
                    gate_acc(py, half, 1024, False, False)
                yield
                V(lambda e: e.tensor_scalar(out=ST2[:, 0:2], in0=ST2[:, 0:2], scalar1=1.0 / 512.0, scalar2=None, op0=ALU.mult), r=[ST2], w=[ST2])
                V(lambda e: e.tensor_tensor(out=ST2[:, 2:3], in0=ST2[:, 0:1], in1=ST2[:, 0:1], op=ALU.mult), r=[ST2], w=[ST2])
                yield
                V(lambda e: e.tensor_tensor(out=ST2[:, 2:3], in0=ST2[:, 1:2], in1=ST2[:, 2:3], op=ALU.subtract), r=[ST2], w=[ST2])
                A(lambda e: e.activation(out=ST2[:, 2:3], in_=ST2[:, 2:3], func=AF.Sqrt, bias=EPSC[:], scale=1.0), r=[ST2, EPSC], w=[ST2])
                yield
                V(lambda e: e.reciprocal(out=ST2[:, 2:3], in_=ST2[:, 2:3]), r=[ST2], w=[ST2])
                yield
                V(lambda e: e.tensor_scalar(out=VNf[:], in0=vv, scalar1=ST2[:, 0:1], scalar2=ST2[:, 2:3], op0=ALU.subtract, op1=ALU.mult), r=[zr, ST2], w=[VNf])
                yield
                V(lambda e: e.tensor_tensor(out=VNf[:], in0=VNf[:], in1=SLW[:], op=ALU.mult), r=[VNf, SLW], w=[VNf])
                yield
                V(lambda e: e.tensor_tensor(out=VNb[:], in0=VNf[:], in1=SLB[:], op=ALU.add), r=[VNf, SLB], w=[VNb])
                yield
                for g in range(4):
                    P(lambda e: e.matmul(pS[:, g * 128:(g + 1) * 128], lhsT=SGT[:, g, :], rhs=VNb[:, g * 128:(g + 1) * 128], start=True, stop=True), r=[SGT, VNb], w=[pS])
                yield
                for g in range(4):
                    V(lambda e: e.scalar_tensor_tensor(out=SGO[:, g * 128:(g + 1) * 128], in0=pS[:, g * 128:(g + 1) * 128], scalar=SM[:, 12 + g:13 + g], in1=zr[:, ZR_ZS + g * 128:ZR_ZS + (g + 1) * 128], op0=ALU.add, op1=ALU.mult), r=[pS, SM, zr], w=[SGO])
                yield
                for c in range(4):
                    P(lambda e: e.transpose(out=pTb[:, c * 128:(c + 1) * 128], in_=SGO[:, c * 128:(c + 1) * 128], identity=IDB[:]), r=[SGO, IDB], w=[pT])
                A(lambda e: e.activation(out=SGOT[:].rearrange("p c t -> p (c t)"), in_=pTb[:, 0:512], func=AF.Copy), r=[pT], w=[SGOT])
                yield
                for half in range(2):
                    py = PY.next()
                    for c in range(4):
                        P(lambda e: e.matmul(py[:, 0:512], lhsT=SGOT[:, c, :], rhs=WSO[:, c, half * 512:(half + 1) * 512], start=(c == 0), stop=(c == 3)), r=[SGOT, WSO], w=[py])
                    gate_acc(py, half, 2048, False, True)
                yield
                for c in range(8):
                    P(lambda e: e.transpose(out=pTb[:, c * 128:(c + 1) * 128], in_=MGB[:, c * 128:(c + 1) * 128], identity=IDB[:]), r=[MGB, IDB], w=[pT])
                A(lambda e: e.activation(out=MGT[:].rearrange("p c t -> p (c t)"), in_=pTb[:, 0:1024], func=AF.Copy), r=[pT], w=[MGT])
                yield
                for half in range(2):
                    py = PY.next()
                    hs = slice(half * 512, (half + 1) * 512)
                    for kc in range(8):
                        P(lambda e: e.matmul(py[:, 0:512], lhsT=MGT[:, kc, :], rhs=WO[:, kc, hs], start=(kc == 0), stop=(kc == 7)), r=[MGT, WO], w=[py])
                    V(lambda e: e.tensor_tensor(out=MG[:, hs], in0=py[:, 0:512], in1=G1R[j][:, hs], op=ALU.mult), r=[py, G1R[j]], w=[MG])
                    yield
                    G(lambda e: e.tensor_tensor(out=xt[:, hs], in0=MG[:, hs], in1=xt[:, hs], op=ALU.add), r=[MG, xt], w=[xt])
                    yield
                ST(xs_oth[ti * 128:(ti + 1) * 128, :], xt[:], xt)
                yield

            def e_stream(s_):
                for ti in tiles_out[s_::2]:
                    yield from e_tile(ti, WS[s_])

            interleave([e_stream(0), e_stream(1)], skew=[0, 18])
        k.barrier()
        if upto == "E" and l == layers - 1:
            break

        sets = [("lat", list(range(2, NT)), 512)]
        if need_ctx:
            sets.append(("ctx", [0, 1], 32))
        with ExitStack() as es:
            ZERO = k.sbuf("zero", [128, 1024], F32, es)
            IOTAF = k.sbuf("iotaf", [128, 512], F32, es)
            IOTAI = k.sbuf("iotai", [128, 512], I32, es)
            G(lambda e: e.memset(ZERO[:], 0.0), w=[ZERO])
            G(lambda e: e.iota(IOTAI[:], pattern=[[1, 512]], base=0, channel_multiplier=0), w=[IOTAI])
            V(lambda e: e.tensor_copy(out=IOTAF[:], in_=IOTAI[:]), r=[IOTAI], w=[IOTAF])
            RW = k.sbuf("frw", [128, 8, NE], F32, es)
            LD(RW[:], rw_d[l, :, :].rearrange("(kc p) n -> p kc n", p=128), RW)
            RBR = k.sbuf("frbr", [128, NE], F32, es)
            LD(RBR[:], rb_d[l, :].partition_broadcast(128), RBR)
            AFF = k.sbuf("faff", [128, NT, NE], F32, es)
            W2R = [k.sbuf("fw2r%d" % j, [128, D], F32, es) for j in range(len(sets))]
            S2R = [k.sbuf("fs2r%d" % j, [128, D], F32, es) for j in range(len(sets))]
            for j in range(len(sets)):
                bcast_row(W2R[j], WSd[j, 1, :])
                bcast_row(S2R[j], MODd[j, 3 * D:4 * D])

            with ExitStack() as es1:
                es1.enter_context(nc.named_scope('F1_%d' % l))
                junk = k.sbuf("fjunk", [128, D], BF16, es1)
                FW = []
                for s_ in range(2):
                    FW.append({"xt": k.sbuf("fxt%d" % s_, [128, D], F32, es1), "xn": k.sbuf("fxn%d" % s_, [128, D], F32, es1),
                               "tmp": k.sbuf("ftmp%d" % s_, [128, D], F32, es1),
                               "ssq": k.sbuf("fssq%d" % s_, [128, 1], F32, es1), "rt": k.sbuf("frt%d" % s_, [128, 1], F32, es1),
                               "h2T": k.sbuf("fh2t%d" % s_, [128, 8, 128], F32, es1), "h2r": k.sbuf("fh2r%d" % s_, [128, D], BF16, es1),
                               "LG": k.sbuf("flg%d" % s_, [128, NE], F32, es1), "MX": k.sbuf("fmx%d" % s_, [128, 2], F32, es1),
                               "pt": PB[s_], "pr": PB[2 + s_]})

                def f1_tile(ti, W):
                    xt, xn, tmp, ssq, rt, h2T, h2r, LG, MX, pr = W["xt"], W["xn"], W["tmp"], W["ssq"], W["rt"], W["h2T"], W["h2r"], W["LG"], W["MX"], W["pr"]
                    j = 1 if ti < 2 else 0
                    LD(xt[:], xs_oth[ti * 128:(ti + 1) * 128, :], xt)
                    k.dma("pool", lambda e: e.dma_start(out=MACC[ti * 128:(ti + 1) * 128, :], in_=ZERO[:]), reads=[ZERO], sembuf=ZERO)
                    yield
                    rms_to_xn(xt, xn, junk, ssq, rt)
                    yield
                    xn_to_hT(xn, h2T, j, 2, 3, pbs=(W["pt"], W["pt"]))
                    yield
                    V(lambda e: e.tensor_tensor(out=tmp[:], in0=xn[:], in1=W2R[j][:], op=ALU.mult), r=[xn, W2R[j]], w=[tmp])
                    yield
                    V(lambda e: e.tensor_tensor(out=h2r[:], in0=tmp[:], in1=S2R[j][:], op=ALU.add), r=[tmp, S2R[j]], w=[h2r])
                    ST(H2d[ti * 128:(ti + 1) * 128, :], h2r[:], h2r)
                    yield
                    for kc in range(8):
                        P(lambda e: e.matmul(pr[:, 0:NE], lhsT=h2T[:, kc, :], rhs=RW[:, kc, :], start=(kc == 0), stop=(kc == 7)), r=[h2T, RW], w=[pr])
                    yield
                    V(lambda e: e.tensor_tensor(out=LG[:], in0=pr[:, 0:NE], in1=RBR[:], op=ALU.add), r=[pr, RBR], w=[LG])
                    yield
                    V(lambda e: e.reduce_max(out=MX[:, 0:1], in_=LG[:], axis=mybir.AxisListType.X), r=[LG], w=[MX])
                    yield
                    V(lambda e: e.tensor_scalar(out=MX[:, 0:1], in0=MX[:, 0:1], scalar1=-1.0, scalar2=None, op0=ALU.mult), r=[MX], w=[MX])
                    yield
                    A(lambda e: e.activation(out=LG[:], in_=LG[:], func=AF.Exp, bias=MX[:, 0:1], scale=1.0, accum_out=MX[:, 1:2]), r=[LG, MX], w=[LG, MX])
                    yield
                    V(lambda e: e.reciprocal(out=MX[:, 1:2], in_=MX[:, 1:2]), r=[MX], w=[MX])
                    yield
                    V(lambda e: e.tensor_scalar(out=AFF[:, ti, :], in0=LG[:], scalar1=MX[:, 1:2], scalar2=None, op0=ALU.mult), r=[LG, MX], w=[AFF])
                    yield

                def f1_stream(s_):
                    for ti in tiles_out[s_::2]:
                        yield from f1_tile(ti, FW[s_])

                interleave([f1_stream(0), f1_stream(1)], skew=[0, 6])
            k.barrier()

            TOKI = k.sbuf("ftoki", [128, NE, 5], I32, es)
            GS = k.sbuf("fgs", [128, NE, 5], F32, es)
            with ExitStack() as es2:
                es2.enter_context(nc.named_scope('F3_%d' % l))
                AFFT = k.sbuf("fafft", [16, SEQ], F32, es2)
                JK = k.sbuf("fjk", [16, SEQ], F32, es2)
                BS = k.sbuf("fbs", [16, 8], F32, es2)
                DG = k.sbuf("fdg", [16, 16], F32, es2)
                THRB = k.sbuf("fthrb", [128, NE], F32, es2)
                MK = k.sbuf("fmk", [128, 32, NE], F32, es2)
                MKB = k.sbuf("fmkb", [128, 32, NE], BF16, es2)
                OFFS = k.sbuf("foffs", [128, 32, NE], F32, es2)
                POS = k.sbuf("fpos", [128, 32, NE], F32, es2)
                RH = k.sbuf("frh", [128, 32, NE, 5], BF16, es2)
                R1 = k.sbuf("fr1", [128, 32, NE], F32, es2)
                R2 = k.sbuf("fr2", [128, 32, NE], F32, es2)
                OH = Ring([k.sbuf("foh%d" % i, [128, 512], BF16, es2) for i in range(6)])
                TF = k.sbuf("ftf", [128, 8], F32, es2)
                NPOS = k.sbuf("fnpos", [128, 32, NE], F32, es2)
                NMK = k.sbuf("fnmk", [128, 32, NE], F32, es2)
                ABR = Ring([k.sbuf("fab%d" % i, [128, 512], F32, es2) for i in range(2)])
                for si, (sname, stiles, cap) in enumerate(sets):
                    nt = len(stiles)
                    ntok = nt * 128
                    t0 = stiles[0]
                    nst = (cap + 127) // 128
                    for q in range(nt):
                        P(lambda e: e.transpose(out=PB[0][0:16, (q % 4) * 128:(q % 4 + 1) * 128], in_=AFF[:, t0 + q, :], identity=ID32[:]), r=[AFF, ID32], w=[PB[0]])
                        if q % 4 == 3 or q == nt - 1:
                            q0 = (q // 4) * 4
                            wq = (q - q0 + 1) * 128
                            V(lambda e: e.tensor_copy(out=AFFT[:, q0 * 128:q0 * 128 + wq], in_=PB[0][0:16, 0:wq]), r=[PB[0]], w=[AFFT])
                    V(lambda e: e.memset(BS[:, 0:1], 0.0), w=[BS])
                    V(lambda e: e.memset(BS[:, 1:2], 1.0), w=[BS])
                    for it in range(26):
                        V(lambda e: e.tensor_scalar(out=BS[:, 2:3], in0=BS[:, 0:1], scalar1=BS[:, 1:2], scalar2=0.5, op0=ALU.add, op1=ALU.mult), r=[BS], w=[BS])
                        V(lambda e: e.tensor_scalar(out=JK[:, 0:ntok], in0=AFFT[:, 0:ntok], scalar1=BS[:, 2:3], scalar2=0.0, op0=ALU.is_ge, op1=ALU.add, accum_out=BS[:, 3:4]), r=[AFFT, BS], w=[JK, BS])
                        V(lambda e: e.tensor_scalar(out=BS[:, 4:5], in0=BS[:, 3:4], scalar1=float(cap) - 0.5, scalar2=None, op0=ALU.is_ge), r=[BS], w=[BS])
                        V(lambda e: e.tensor_tensor(out=BS[:, 5:6], in0=BS[:, 2:3], in1=BS[:, 0:1], op=ALU.subtract), r=[BS], w=[BS])
                        V(lambda e: e.tensor_tensor(out=BS[:, 6:7], in0=BS[:, 1:2], in1=BS[:, 2:3], op=ALU.subtract), r=[BS], w=[BS])
                        V(lambda e: e.scalar_tensor_tensor(out=BS[:, 0:1], in0=BS[:, 5:6], scalar=BS[:, 4:5], in1=BS[:, 0:1], op0=ALU.mult, op1=ALU.add), r=[BS], w=[BS])
                        V(lambda e: e.scalar_tensor_tensor(out=BS[:, 1:2], in0=BS[:, 6:7], scalar=BS[:, 4:5], in1=BS[:, 2:3], op0=ALU.mult, op1=ALU.add), r=[BS], w=[BS])
                    V(lambda e: e.tensor_scalar(out=DG[:], in0=ID32[0:16, 0:16], scalar1=BS[:, 0:1], scalar2=None, op0=ALU.mult), r=[ID32, BS], w=[DG])
                    P(lambda e: e.matmul(PB[1][:, 0:NE], lhsT=ONES32[0:16, :], rhs=DG[:], start=True, stop=True), r=[ONES32, DG], w=[PB[1]])
                    V(lambda e: e.tensor_copy(out=THRB[:], in_=PB[1][:, 0:NE]), r=[PB[1]], w=[THRB])
                    for q in range(nt):
                        V(lambda e: e.tensor_tensor(out=MK[:, q, :], in0=AFF[:, t0 + q, :], in1=THRB[:], op=ALU.is_ge), r=[AFF, THRB], w=[MK])
                    ncol = nt * NE
                    mkf = MK[:, 0:nt, :].rearrange("p t e -> p (t e)")
                    V(lambda e: e.tensor_copy(out=MKB[:, 0:nt, :].rearrange("p t e -> p (t e)"), in_=mkf), r=[MK], w=[MKB])
                    P(lambda e: e.matmul(PB[2][:, 0:ncol], lhsT=SUB[:], rhs=MKB[:, 0:nt, :].rearrange("p t e -> p (t e)"), start=True, stop=True), r=[SUB, MKB], w=[PB[2]])
                    P(lambda e: e.matmul(PB[3][:, 0:ncol], lhsT=ONESB[:], rhs=MKB[:, 0:nt, :].rearrange("p t e -> p (t e)"), start=True, stop=True), r=[ONESB, MKB], w=[PB[3]])
                    V(lambda e: e.memset(OFFS[:, 0, :], 0.0), w=[OFFS])
                    for q in range(1, nt):
                        V(lambda e: e.tensor_tensor(out=OFFS[:, q, :], in0=OFFS[:, q - 1, :], in1=PB[3][:, (q - 1) * NE:q * NE], op=ALU.add), r=[OFFS, PB[3]], w=[OFFS])
                    posf = POS[:, 0:nt, :].rearrange("p t e -> p (t e)")
                    V(lambda e: e.tensor_tensor(out=posf, in0=PB[2][:, 0:ncol], in1=OFFS[:, 0:nt, :].rearrange("p t e -> p (t e)"), op=ALU.add), r=[PB[2], OFFS], w=[POS])
                    V(lambda e: e.tensor_scalar(out=R1[:, 0:nt, :].rearrange("p t e -> p (t e)"), in0=posf, scalar1=float(cap) - 0.5, scalar2=None, op0=ALU.is_lt), r=[POS], w=[R1])
                    V(lambda e: e.tensor_tensor(out=mkf, in0=mkf, in1=R1[:, 0:nt, :].rearrange("p t e -> p (t e)"), op=ALU.mult), r=[MK, R1], w=[MK])
                    for q in range(nt):
                        V(lambda e: e.memset(RH[:, q, :, 0], float((t0 + q) * 128)), w=[RH])
                        V(lambda e: e.tensor_scalar(out=RH[:, q, :, 1], in0=ONES32[:, 0:NE], scalar1=PIDX[:, 0:1], scalar2=None, op0=ALU.mult), r=[ONES32, PIDX], w=[RH])
                    afs = AFF[:, t0:t0 + nt, :]
                    V(lambda e: e.tensor_copy(out=RH[:, 0:nt, :, 2], in_=afs), r=[AFF], w=[RH])
                    V(lambda e: e.tensor_tensor(out=R1[:, 0:nt, :], in0=afs, in1=RH[:, 0:nt, :, 2], op=ALU.subtract), r=[AFF, RH], w=[R1])
                    V(lambda e: e.tensor_copy(out=RH[:, 0:nt, :, 3], in_=R1[:, 0:nt, :]), r=[R1], w=[RH])
                    V(lambda e: e.tensor_tensor(out=R2[:, 0:nt, :], in0=R1[:, 0:nt, :], in1=RH[:, 0:nt, :, 3], op=ALU.subtract), r=[R1, RH], w=[R2])
                    V(lambda e: e.tensor_copy(out=RH[:, 0:nt, :, 4], in_=R2[:, 0:nt, :]), r=[R2], w=[RH])
                    V(lambda e: e.tensor_scalar(out=NPOS[:, 0:nt, :].rearrange("p t e -> p (t e)"), in0=posf, scalar1=-1.0, scalar2=None, op0=ALU.mult), r=[POS], w=[NPOS])
                    V(lambda e: e.tensor_scalar(out=NMK[:, 0:nt, :].rearrange("p t e -> p (t e)"), in0=mkf, scalar1=-1.0, scalar2=None, op0=ALU.mult), r=[MK], w=[NMK])
                    PQ = [PB[4], PB[5], PB[6], PB[7]]
                    capw = nst * 128 if cap >= 128 else cap
                    for ex in range(NE):
                        for q in range(nt):
                            oh = OH.next()
                            if q % 3 == 2:
                                ab = ABR.next()
                                A(lambda e: e.activation(out=ab[:, 0:capw], in_=IOTAF[:, 0:capw], func=AF.Abs, bias=NPOS[:, q, ex:ex + 1], scale=1.0), r=[IOTAF, NPOS], w=[ab])
                                A(lambda e: e.activation(out=oh[:, 0:capw], in_=ab[:, 0:capw], func=AF.Relu, bias=MK[:, q, ex:ex + 1], scale=NMK[:, q, ex:ex + 1]), r=[ab, MK, NMK], w=[oh])
                            else:
                                k.op("dve", lambda e: e.tensor_scalar(out=oh[:, 0:capw], in0=IOTAF[:, 0:capw], scalar1=POS[:, q, ex:ex + 1], scalar2=MK[:, q, ex:ex + 1], op0=ALU.is_equal, op1=ALU.mult), [IOTAF, POS, MK], [oh])
                            for s_ in range(nst):
                                mw = min(128, cap - s_ * 128)
                                P(lambda e: e.matmul(PQ[s_][0:mw, 0:5], lhsT=oh[:, s_ * 128:s_ * 128 + mw], rhs=RH[:, q, ex, :], start=(q == 0), stop=(q == nt - 1)), r=[oh, RH], w=[PQ[s_]])
                        for s_ in range(nst):
                            mw = min(128, cap - s_ * 128)
                            sl = s_ if si == 0 else 4
                            V(lambda e: e.tensor_copy(out=TF[0:mw, 0:5], in_=PQ[s_][0:mw, 0:5]), r=[PQ[s_]], w=[TF])
                            V(lambda e: e.tensor_tensor(out=TF[0:mw, 5:6], in0=TF[0:mw, 0:1], in1=TF[0:mw, 1:2], op=ALU.add), r=[TF], w=[TF])
                            V(lambda e: e.tensor_copy(out=TOKI[0:mw, ex, sl:sl + 1], in_=TF[0:mw, 5:6]), r=[TF], w=[TOKI])
                            V(lambda e: e.tensor_tensor(out=TF[0:mw, 6:7], in0=TF[0:mw, 2:3], in1=TF[0:mw, 3:4], op=ALU.add), r=[TF], w=[TF])
                            V(lambda e: e.tensor_tensor(out=GS[0:mw, ex, sl:sl + 1], in0=TF[0:mw, 6:7], in1=TF[0:mw, 4:5], op=ALU.add), r=[TF], w=[GS])
            k.barrier()

            with ExitStack() as es3:
                es3.enter_context(nc.named_scope('F5_%d' % l))
                WPR = Ring([k.sbuf("fwp%d" % i, [128, 16, 512], BF16, es3) for i in range(3)])
                NS = 544 if need_ctx else 512
                nsl = 5 if need_ctx else 4
                XE2 = [[k.sbuf("fxe%d_%d" % (pp, i), [128, D], BF16, es3) for i in range(nsl)] for pp in range(2)]
                XET2 = [k.sbuf("fxet%d" % pp, [128, 8, NS], BF16, es3) for pp in range(2)]
                HMT = k.sbuf("fhmt", [128, 16, NS], BF16, es3)
                SA = Ring([k.sbuf("fsa%d" % i, [128, NS], F32, es3) for i in range(2)])
                YS = [k.sbuf("fys%d" % i, [128, D], F32, es3) for i in range(5 if need_ctx else 4)]
                slot_tiles = [(s_, 128, s_ * 128) for s_ in range(4)]
                if need_ctx:
                    slot_tiles.append((4, 32, 512))
                pieces = []
                for ex in range(NE):
                    for g in range(4):
                        pieces.append((ex, "g", g))
                    for hf_ in range(2):
                        pieces.append((ex, "d", hf_))
                loaded = {}

                def issue_piece(pi):
                    if pi >= len(pieces) or pi in loaded:
                        return
                    ex, kind, g = pieces[pi]
                    wp = WPR.next()
                    if kind == "g":
                        k.dma("pool", lambda e: e.dma_start(out=wp[:, 0:8, :], in_=wg_d[l, ex, :, g * 512:(g + 1) * 512].rearrange("(kc p) n -> p kc n", p=128)), writes=[wp], sembuf=wp)
                        k.dma("pool", lambda e: e.dma_start(out=wp[:, 8:16, :], in_=wu_d[l, ex, :, g * 512:(g + 1) * 512].rearrange("(kc p) n -> p kc n", p=128)), writes=[], reads=[], sembuf=wp)
                        wp.writes = {("d", wp.dsem): k.dcount[wp.dsem]}
                    else:
                        k.dma("pool", lambda e: e.dma_start(out=wp[:], in_=wd_d[l, ex, :, g * 512:(g + 1) * 512].rearrange("(fc p) n -> p fc n", p=128)), writes=[wp], sembuf=wp)
                    loaded[pi] = wp

                def f5_gather(ex):
                    for (s_, mw, off) in slot_tiles:
                        xe = XE2[ex % 2][s_]
                        k.dma("pool", lambda e: e.indirect_dma_start(out=xe[0:mw, :], out_offset=None, in_=H2d[:, :], in_offset=bass.IndirectOffsetOnAxis(ap=TOKI[0:mw, ex, s_:s_ + 1], axis=0)), reads=[TOKI], writes=[xe], sembuf=xe)

                def f5_xet(ex):
                    for (s_, mw, off) in slot_tiles:
                        xe = XE2[ex % 2][s_]
                        for c in range(8):
                            P(lambda e: e.transpose(out=pbf(6)[:, c * 128:c * 128 + mw], in_=xe[0:mw, c * 128:(c + 1) * 128], identity=IDB[0:mw, 0:mw]), r=[xe, IDB], w=[PB[6]])
                        V(lambda e: e.tensor_copy(out=XET2[ex % 2][:, :, off:off + mw], in_=pbf(6)[:, 0:1024].rearrange("p (c t) -> p c t", c=8)[:, :, 0:mw]), r=[PB[6]], w=[XET2[ex % 2]])

                PA = Ring([PB[0], PB[1]])
                PUu = Ring([PB[2], PB[3]])
                PYr = Ring([PB[4], PB[5]])
                issue_piece(0)
                issue_piece(1)
                pi = 0
                prev_tokens = {}
                for ex in range(NE):
                    XET = XET2[ex % 2]
                    f5_gather(ex)
                    f5_xet(ex)
                    for g in range(4):
                        wp = loaded.pop(pi)
                        issue_piece(pi + 2)
                        pi += 1
                        for f in range(4):
                            fc = g * 4 + f
                            pa = PA.next(); pu = PUu.next()
                            for (wsel, pp) in ((0, pa), (8, pu)):
                                for kc in range(8):
                                    P(lambda e: e.matmul(pp[:, 0:512], lhsT=wp[:, wsel + kc, f * 128:(f + 1) * 128], rhs=XET[:, kc, 0:512], start=(kc == 0), stop=(kc == 7)), r=[wp, XET], w=[pp])
                            sa = SA.next()
                            A(lambda e: e.activation(out=sa[:, 0:512], in_=pa[:, 0:512], func=AF.Silu), r=[pa], w=[sa])
                            V(lambda e: e.tensor_tensor(out=HMT[:, fc, 0:512], in0=sa[:, 0:512], in1=pu[:, 0:512], op=ALU.mult), r=[sa, pu], w=[HMT])
                            if need_ctx:
                                for (wsel, c0_) in ((0, 0), (8, 32)):
                                    for kc in range(8):
                                        P(lambda e: e.matmul(PB[7][:, c0_:c0_ + 32], lhsT=wp[:, wsel + kc, f * 128:(f + 1) * 128], rhs=XET[:, kc, 512:544], start=(kc == 0), stop=(kc == 7)), r=[wp, XET], w=[PB[7]])
                                A(lambda e: e.activation(out=sa[:, 512:544], in_=PB[7][:, 0:32], func=AF.Silu), r=[PB[7]], w=[sa])
                                V(lambda e: e.tensor_tensor(out=HMT[:, fc, 512:544], in0=sa[:, 512:544], in1=PB[7][:, 32:64], op=ALU.mult), r=[sa, PB[7]], w=[HMT])
                    wpd = []
                    wpd.append(loaded.pop(pi))
                    issue_piece(pi + 2)
                    pi += 1
                    wpd.append(loaded.pop(pi))
                    pi += 1
                    cur_tokens = {}
                    for (s_, mw, off) in slot_tiles:
                        for hf_ in range(2):
                            py = PYr.next()
                            for fc in range(16):
                                P(lambda e: e.matmul(py[0:mw, 0:512], lhsT=HMT[:, fc, off:off + mw], rhs=wpd[hf_][:, fc, :], start=(fc == 0), stop=(fc == 15)), r=[HMT, wpd[hf_]], w=[py])
                            A(lambda e: e.activation(out=YS[s_][0:mw, hf_ * 512:(hf_ + 1) * 512], in_=py[0:mw, 0:512], func=AF.Identity, scale=GS[0:mw, ex, s_:s_ + 1]), r=[py, GS], w=[YS[s_]])
                        MACC.writes = dict(prev_tokens)
                        MACC.readers = {}
                        k.dma("pool", lambda e: e.indirect_dma_start(out=MACC[:, :], out_offset=bass.IndirectOffsetOnAxis(ap=TOKI[0:mw, ex, s_:s_ + 1], axis=0), in_=YS[s_][0:mw, :], in_offset=None, compute_op=ALU.add), reads=[TOKI, YS[s_]], writes=[MACC], sembuf=YS[s_])
                        _merge(cur_tokens, MACC.writes)
                    prev_tokens = cur_tokens
                    MACC.writes = dict(prev_tokens)
                    issue_piece(pi + 1)
            k.barrier()

            with ExitStack() as es4:
                es4.enter_context(nc.named_scope('F6_%d' % l))
                G2R = [k.sbuf("fg2r%d" % j, [128, D], F32, es4) for j in range(len(sets))]
                for j in range(len(sets)):
                    bcast_row(G2R[j], MODd[j, 5 * D:6 * D])
                XL = Ring([k.sbuf("gxt%d" % i, [128, D], F32, es4) for i in range(3)])
                ML = Ring([k.sbuf("gml%d" % i, [128, D], F32, es4) for i in range(3)])
                XO = Ring([k.sbuf("gxo%d" % i, [128, D], F32, es4) for i in range(3)])
                junk = k.sbuf("gjunk", [128, D], BF16, es4)
                SSQR = Ring([k.sbuf("gssq%d" % i, [128, 1], F32, es4) for i in range(2)])
                RTR = Ring([k.sbuf("grt%d" % i, [128, 1], F32, es4) for i in range(2)])
                if last:
                    FNW = k.sbuf("gfnw", [128, D], F32, es4)
                    LD(FNW[:], fnw_d.t.partition_broadcast(128), FNW)
                for ti in tiles_out:
                    j = 1 if ti < 2 else 0
                    xt = XL.next(); ml = ML.next(); xo = XO.next()
                    LD(xt[:], xs_oth[ti * 128:(ti + 1) * 128, :], xt)
                    LD(ml[:], MACC[ti * 128:(ti + 1) * 128, :], ml)
                    V(lambda e: e.tensor_tensor(out=ml[:], in0=ml[:], in1=G2R[j][:], op=ALU.mult), r=[ml, G2R[j]], w=[ml])
                    if not last:
                        V(lambda e: e.tensor_tensor(out=xo[:], in0=ml[:], in1=xt[:], op=ALU.add), r=[ml, xt], w=[xo])
                        ST(xs_cur[ti * 128:(ti + 1) * 128, :], xo[:], xo)
                    else:
                        V(lambda e: e.tensor_tensor(out=xt[:], in0=ml[:], in1=xt[:], op=ALU.add), r=[ml, xt], w=[xt])
                        rms_to_xn(xt, ml, junk, SSQR.next(), RTR.next())
                        V(lambda e: e.tensor_tensor(out=xo[:], in0=ml[:], in1=FNW[:], op=ALU.mult), r=[ml, FNW], w=[xo])
                        ST(out_d[(ti - 2) * 128:(ti - 1) * 128, :], xo[:], xo)
        k.barrier()
    k.finish()
    return nc, k


_CACHE = {}


def kernel(**inputs):
    n = 8
    if "nc" not in _CACHE:
        _CACHE["nc"] = build_program()[0]
    nc = _CACHE["nc"]
    shared = {kk: np.ascontiguousarray(v, dtype=np.float32) for kk, v in inputs.items() if kk not in ("x", "c", "ctx")}
    in_maps = []
    for b in range(n):
        m = dict(shared)
        m["x"] = np.ascontiguousarray(inputs["x"][b], dtype=np.float32)
        m["c"] = np.ascontiguousarray(inputs["c"][b], dtype=np.float32)
        m["ctx"] = np.ascontiguousarray(inputs["ctx"][b], dtype=np.float32)
        in_maps.append(m)
    res = run_bass_kernel_spmd(nc, in_maps, core_ids=list(range(n)))
    return np.stack([np.asarray(r["out"], dtype=np.float32) for r in res.results], axis=0)
```
